# Optimizing a Trainium2 kernel written in Bass

```python
import math
import jax, jax.numpy as jnp
from jax import lax
import numpy as np

D_MODEL = 1024
BATCH = 2
SEQ = 8192
DEPTH = 2

N_MIXERS = 2
N_HEADS = 16
HEAD_DIM = 64
WIDTH = N_HEADS * HEAD_DIM
MOBA_BLOCK = 256
MOBA_TOPK = 3
Q_CHUNK = 64
DECAY_LORA = 64
AAA_LORA = 64
N_SHIFT_STREAMS = 6
NORM_EPS = 1e-6
LNX_EPS = 64e-5

kernel_name = "hybrid_moba_rwkv7_trunk"


def rms_norm(x, g):
    xf = x.astype(jnp.float32)
    y = xf * lax.rsqrt(jnp.mean(xf * xf, axis=-1, keepdims=True) + NORM_EPS)
    return (y * g.astype(jnp.float32)).astype(x.dtype)


def alibi_slopes(n):
    return jnp.asarray([2.0 ** (-8.0 * (i + 1) / n) for i in range(n)], dtype=jnp.float32)


def to_heads(t):
    b, s, _ = t.shape
    return t.reshape(b, s, N_HEADS, HEAD_DIM).transpose(0, 2, 1, 3)


def moba_attention(q, k, v):
    B, H, S, dh = q.shape
    nb = -(-S // MOBA_BLOCK)
    s_pad = nb * MOBA_BLOCK
    pad = ((0, 0), (0, 0), (0, s_pad - S), (0, 0))
    kp = jnp.pad(k, pad)
    vp = jnp.pad(v, pad)
    k_blocks = kp.reshape(B, H, nb, MOBA_BLOCK, dh)
    v_blocks = vp.reshape(B, H, nb, MOBA_BLOCK, dh)
    k_mean = jnp.mean(k_blocks.astype(jnp.float32), axis=3).astype(q.dtype)
    topk = min(MOBA_TOPK, nb)
    slopes = alibi_slopes(H)[None, :, None, None]
    scale = dh ** -0.5
    b_idx = jnp.arange(B)[:, None, None, None]
    h_idx = jnp.arange(H)[None, :, None, None]
    blk_pos = jnp.arange(MOBA_BLOCK)

    def chunk(c):
        t0 = c * Q_CHUNK
        own = t0 // MOBA_BLOCK
        q_c = lax.dynamic_slice_in_dim(q, t0, Q_CHUNK, axis=2)
        t_int = t0 + jnp.arange(Q_CHUNK)
        t_f = t_int.astype(jnp.float32)
        gate = jnp.einsum('bhqd,bhnd->bhqn', q_c, k_mean).astype(jnp.float32)
        gate = jnp.where(jnp.arange(nb) < own, gate, -jnp.inf)
        _, sel = lax.top_k(gate, topk)
        sel_valid = jnp.arange(topk) < own
        k_sel = k_blocks[b_idx, h_idx, sel]
        v_sel = v_blocks[b_idx, h_idx, sel]
        s_sel = jnp.einsum('bhqd,bhqjld->bhqjl', q_c, k_sel).astype(jnp.float32) * scale
        pos_sel = (sel[..., None] * MOBA_BLOCK + blk_pos).astype(jnp.float32)
        s_sel = s_sel - slopes[..., None] * (t_f[:, None, None] - pos_sel)
        s_sel = jnp.where(sel_valid[:, None], s_sel, -jnp.inf)
        s_sel = s_sel.reshape(B, H, Q_CHUNK, topk * MOBA_BLOCK)
        k_own = lax.dynamic_slice_in_dim(kp, own * MOBA_BLOCK, MOBA_BLOCK, axis=2)
        v_own = lax.dynamic_slice_in_dim(vp, own * MOBA_BLOCK, MOBA_BLOCK, axis=2)
        pos_own = own * MOBA_BLOCK + blk_pos
        s_own = jnp.einsum('bhqd,bhld->bhql', q_c, k_own).astype(jnp.float32) * scale
        s_own = s_own - slopes * (t_f[:, None] - pos_own.astype(jnp.float32)[None, :])
        s_own = jnp.where(pos_own[None, :] <= t_int[:, None], s_own, -jnp.inf)
        p = jax.nn.softmax(jnp.concatenate([s_sel, s_own], axis=-1), axis=-1)
        p_sel = p[..., :topk * MOBA_BLOCK].reshape(B, H, Q_CHUNK, topk, MOBA_BLOCK).astype(v.dtype)
        p_own = p[..., topk * MOBA_BLOCK:].astype(v.dtype)
        return (jnp.einsum('bhqjl,bhqjld->bhqd', p_sel, v_sel)
                + jnp.einsum('bhql,bhld->bhqd', p_own, v_own))

    out = lax.map(chunk, jnp.arange(S // Q_CHUNK))
    return out.transpose(1, 2, 0, 3, 4).reshape(B, H, S, dh)


def moba_layer(x, norm_g, w_in, w_out):
    B, S, _ = x.shape
    h = rms_norm(x, norm_g)
    proj = h @ w_in
    q, k, v, gate = jnp.split(proj, 4, axis=-1)
    o = moba_attention(to_heads(q), to_heads(k), to_heads(v))
    o = o.transpose(0, 2, 1, 3).reshape(B, S, WIDTH)
    return x + (o * jax.nn.silu(gate)) @ w_out


def rwkv7_scan(r, w, k, v, kk, a):
    B, S, H, dh = r.shape
    seq_first = lambda t: jnp.moveaxis(t, 1, 0)

    def step(state, inp):
        r_t, w_t, k_t, v_t, kk_t, a_t = inp
        sa = jnp.einsum('bhvk,bhk->bhv', state, -kk_t)
        state = (state * w_t[:, :, None, :]
                 + sa[..., None] * (kk_t * a_t)[:, :, None, :]
                 + v_t[..., None] * k_t[:, :, None, :])
        return state, jnp.einsum('bhvk,bhk->bhv', state, r_t)

    s0 = jnp.zeros((B, H, dh, dh), jnp.float32)
    _, y = lax.scan(step, s0, tuple(seq_first(t) for t in (r, w, k, v, kk, a)))
    return jnp.moveaxis(y, 0, 1)


def rwkv7_layer(x, norm_g, mix, w_in, w0, w1, w2, a0, a1, a2, k_k, k_a, r_k, lnx_g, lnx_b, w_out):
    B, S, _ = x.shape
    f32 = jnp.float32
    h = rms_norm(x, norm_g)
    h_prev = jnp.pad(h, ((0, 0), (1, 0), (0, 0)))[:, :-1]
    xx = h_prev - h
    xs = h[:, :, None, :] + xx[:, :, None, :] * mix
    proj = jnp.einsum('bsnd,ndw->bsnw', xs[:, :, :4], w_in)
    r, k, v, g = proj[:, :, 0], proj[:, :, 1], proj[:, :, 2], proj[:, :, 3]
    xw, xa = xs[:, :, 4], xs[:, :, 5]
    w_log = -jax.nn.softplus(-(w0 + jnp.tanh(xw @ w1) @ w2).astype(f32)) - 0.5
    decay = jnp.exp(-jnp.exp(w_log))
    a = jax.nn.sigmoid((a0 + (xa @ a1) @ a2).astype(f32))
    hd = lambda t: t.astype(f32).reshape(B, S, N_HEADS, HEAD_DIM)
    hp = lambda p: p.astype(f32).reshape(N_HEADS, HEAD_DIM)
    kk = hd(k * k_k)
    kk = kk / jnp.maximum(jnp.sqrt(jnp.sum(kk * kk, axis=-1, keepdims=True)), 1e-12)
    k_mod = hd(k) * (1.0 + (hd(a) - 1.0) * hp(k_a))
    r_h, v_h, a_h = hd(r), hd(v), hd(a)
    y = rwkv7_scan(r_h, hd(decay), k_mod, v_h, kk, a_h)
    mu = jnp.mean(y, axis=-1, keepdims=True)
    var = jnp.mean(jnp.square(y - mu), axis=-1, keepdims=True)
    y = (y - mu) * lax.rsqrt(var + LNX_EPS)
    y = y * hp(lnx_g) + hp(lnx_b)
    bonus = jnp.sum(r_h * k_mod * r_k.astype(f32), axis=-1, keepdims=True) * v_h
    o = (y + bonus).reshape(B, S, WIDTH).astype(x.dtype)
    return x + (o * jax.nn.silu(g)) @ w_out


def setup_inputs(seed: int = 0) -> dict:
    key = jax.random.key(seed)
    ks = jax.random.split(key, 24)
    nrm = lambda k, shape, s: jax.random.normal(k, shape, jnp.float32) * s
    D, W = D_MODEL, WIDTH
    return {
        "x": nrm(ks[0], (BATCH, SEQ, D), 1.0),
        "moba_norm_g": 1.0 + nrm(ks[1], (D,), 0.05),
        "moba_w_in": nrm(ks[2], (D, 4 * W), D ** -0.5),
        "moba_w_out": nrm(ks[3], (W, D), W ** -0.5),
        "rwkv_norm_g": 1.0 + nrm(ks[4], (D,), 0.05),
        "rwkv_mix": jax.random.uniform(ks[5], (N_SHIFT_STREAMS, D), jnp.float32),
        "rwkv_w_in": nrm(ks[6], (4, D, W), D ** -0.5),
        "rwkv_w0": jax.random.uniform(ks[7], (W,), jnp.float32, -4.0, 1.0),
        "rwkv_w1": nrm(ks[8], (D, DECAY_LORA), D ** -0.5),
        "rwkv_w2": nrm(ks[9], (DECAY_LORA, W), 0.3 * DECAY_LORA ** -0.5),
        "rwkv_a0": nrm(ks[10], (W,), 0.1),
        "rwkv_a1": nrm(ks[11], (D, AAA_LORA), D ** -0.5),
        "rwkv_a2": nrm(ks[12], (AAA_LORA, W), 0.3 * AAA_LORA ** -0.5),
        "rwkv_k_k": 0.85 + nrm(ks[13], (W,), 0.05),
        "rwkv_k_a": 1.0 + nrm(ks[14], (W,), 0.05),
        "rwkv_r_k": nrm(ks[15], (N_HEADS, HEAD_DIM), 0.1),
        "rwkv_lnx_g": 1.0 + nrm(ks[16], (W,), 0.05),
        "rwkv_lnx_b": nrm(ks[17], (W,), 0.02),
        "rwkv_w_out": nrm(ks[18], (W, D), W ** -0.5),
        "final_norm_g": 1.0 + nrm(ks[19], (D,), 0.05),
    }


def reference(x, moba_norm_g, moba_w_in, moba_w_out, rwkv_norm_g, rwkv_mix, rwkv_w_in,
              rwkv_w0, rwkv_w1, rwkv_w2, rwkv_a0, rwkv_a1, rwkv_a2, rwkv_k_k, rwkv_k_a,
              rwkv_r_k, rwkv_lnx_g, rwkv_lnx_b, rwkv_w_out, final_norm_g):
    moba_params = (moba_norm_g, moba_w_in, moba_w_out)
    rwkv_params = (rwkv_norm_g, rwkv_mix, rwkv_w_in, rwkv_w0, rwkv_w1, rwkv_w2, rwkv_a0,
                   rwkv_a1, rwkv_a2, rwkv_k_k, rwkv_k_a, rwkv_r_k, rwkv_lnx_g, rwkv_lnx_b,
                   rwkv_w_out)
    for i in range(DEPTH):
        if i % N_MIXERS == 0:
            x = moba_layer(x, *moba_params)
        else:
            x = rwkv7_layer(x, *rwkv_params)
    return rms_norm(x, final_norm_g)
```

```python
import numpy as np
import ml_dtypes
from concourse.bass_utils import run_bass_kernel_spmd
import concourse.bass as bass
import concourse.mybir as mybir
from contextlib import ExitStack

F32 = mybir.dt.float32
BF16 = mybir.dt.bfloat16
AF = mybir.ActivationFunctionType
ALU = mybir.AluOpType
AX = mybir.AxisListType

ENGS = ("pe", "act", "dve", "pool", "sp")
DMA_POOLS = {"sp": (0, 8), "pool": (8, 6)}
N_DMA_SEMS = 14


class Op:
    __slots__ = ("eng", "fn", "deps", "signal", "sig_idx", "is_dma", "dma_sem", "dma_val", "idx", "cc")

    def __init__(self, eng, fn, is_dma):
        self.eng = eng
        self.fn = fn
        self.deps = []
        self.signal = False
        self.sig_idx = 0
        self.is_dma = is_dma
        self.dma_sem = -1
        self.dma_val = 0
        self.cc = False


class Sched:
    def __init__(self):
        self.ops = []
        self.last_w = {}
        self.readers = {}
        self.n_dma_q = {}
        self.n_cc = 0

    def op(self, eng, fn, reads=(), writes=(), dma=False):
        o = Op(eng, fn, dma)
        o.idx = len(self.ops)
        _isps = lambda b: (isinstance(b, str) and b.startswith("ps")) or (isinstance(b, tuple) and isinstance(b[0], str) and b[0].startswith("ps"))
        writes = list(writes) + [b for b in reads if _isps(b)]
        reads = [b for b in reads if not _isps(b)]
        deps = {}
        for b in reads:
            w = self.last_w.get(b)
            if w is not None:
                deps[w.idx] = w
        for b in writes:
            w = self.last_w.get(b)
            if w is not None:
                deps[w.idx] = w
            for r in self.readers.get(b, ()):
                deps[r.idx] = r
        best = {}
        for d in deps.values():
            if d is o:
                continue
            if d.is_dma:
                o.deps.append(d)
                continue
            if d.eng == eng and eng == "pe" and not dma:
                continue
            if d.eng not in best or best[d.eng].idx < d.idx:
                best[d.eng] = d
        for d in best.values():
            o.deps.append(d)
            d.signal = True
        for b in reads:
            self.readers.setdefault(b, []).append(o)
        for b in writes:
            self.last_w[b] = o
            self.readers[b] = []
        if dma:
            base, n = DMA_POOLS[eng]
            k = self.n_dma_q.get(eng, 0)
            self.n_dma_q[eng] = k + 1
            o.dma_sem = base + k % n
            o.dma_val = 16 * (k // n + 1)
        self.ops.append(o)
        return o

    def cc(self, fn, reads=(), writes=()):
        o = self.op("pool", fn, reads, writes, dma=True)
        self.n_dma_q["pool"] -= 1
        o.cc = True
        o.dma_sem = N_DMA_SEMS + self.n_cc
        o.dma_val = 1
        self.n_cc += 1
        return o

    def mm(self, out, lhsT, rhs, start=True, stop=True, reads=(), writes=()):
        return self.op("pe", lambda e: e.matmul(out, lhsT=lhsT, rhs=rhs, start=start, stop=stop), reads, writes)

    def tr(self, out, in_, ident, reads=(), writes=()):
        return self.op("pe", lambda e: e.transpose(out, in_, ident), reads, writes)

    def act(self, out, in_, func, scale=None, bias=None, accum_out=None, reads=(), writes=()):
        kw = {}
        if scale is not None:
            kw["scale"] = scale
        if bias is not None:
            kw["bias"] = bias
        if accum_out is not None:
            kw["accum_out"] = accum_out
        return self.op("act", lambda e: e.activation(out=out, in_=in_, func=func, **kw), reads, writes)

    def dma(self, q, out, in_, reads=(), writes=()):
        return self.op(q, lambda e: e.dma_start(out=out, in_=in_), reads, writes, dma=True)

    def copy(self, eng, out, in_, reads=(), writes=()):
        if eng == "act":
            return self.op("act", lambda e: e.activation(out=out, in_=in_, func=AF.Copy), reads, writes)
        return self.op(eng, lambda e: e.tensor_copy(out=out, in_=in_), reads, writes)

    def memset(self, eng, ap, val, writes=()):
        return self.op(eng, lambda e: e.memset(ap, val), (), writes)

    def tt(self, eng, out, in0, in1, op, reads=(), writes=()):
        return self.op(eng, lambda e: e.tensor_tensor(out=out, in0=in0, in1=in1, op=op), reads, writes)

    def ts(self, eng, out, in0, s1, s2, op0, op1=None, reads=(), writes=(), accum_out=None):
        kw = {}
        if op1 is not None:
            kw["op1"] = op1
        if accum_out is not None:
            kw["accum_out"] = accum_out
        return self.op(eng, lambda e: e.tensor_scalar(out=out, in0=in0, scalar1=s1, scalar2=s2, op0=op0, **kw), reads, writes)

    def stt(self, out, in0, scalar, in1, op0, op1, reads=(), writes=(), eng="dve"):
        return self.op(eng, lambda e: e.scalar_tensor_tensor(out=out, in0=in0, scalar=scalar, in1=in1, op0=op0, op1=op1), reads, writes)

    def emit(self, nc, final_waits=True, limit=None, semstack=None, tag=''):
        if limit is not None:
            self.ops = self.ops[:limit]
            for o in self.ops:
                o.signal = False
            for o in self.ops:
                for d in o.deps:
                    if not d.is_dma:
                        d.signal = True
        cnt = {e: 0 for e in ENGS}
        for o in self.ops:
            if o.is_dma:
                continue
            if o.signal:
                cnt[o.eng] += 1
                o.sig_idx = cnt[o.eng]
        per_eng = {e: [o for o in self.ops if o.eng == e] for e in ENGS}
        with ExitStack() as st:
            sst = semstack if semstack is not None else st
            sems = {e: sst.enter_context(nc.semaphore(tag + "s_" + e)) for e in ENGS}
            dsems = [sst.enter_context(nc.semaphore(tag + "d%d" % i)) for i in range(N_DMA_SEMS + self.n_cc)]
            block = st.enter_context(nc.Block())
            all_dma = [o for o in self.ops if o.is_dma]

            def body(ename):
                def f(engine):
                    waited = {}

                    def wait(sem_key, sem, val):
                        if waited.get(sem_key, 0) >= val:
                            return
                        waited[sem_key] = val
                        engine.wait_ge(sem, val)

                    for o in per_eng[ename]:
                        for d in o.deps:
                            if d.is_dma:
                                wait(("d", d.dma_sem), dsems[d.dma_sem], d.dma_val)
                            else:
                                wait(("e", d.eng), sems[d.eng], d.sig_idx)
                        if o.is_dma:
                            if o.dma_val > 16:
                                wait(("d", o.dma_sem), dsems[o.dma_sem], o.dma_val - 16)
                            ins = o.fn(engine)
                            if o.cc:
                                ins.then_inc(dsems[o.dma_sem])
                            else:
                                ins.then_inc(dsems[o.dma_sem], 16)
                        else:
                            ins = o.fn(engine)
                            if o.signal:
                                ins.then_inc(sems[ename], 1)
                    if final_waits and ename == "sp":
                        last = {}
                        for o in all_dma:
                            last[o.dma_sem] = max(last.get(o.dma_sem, 0), o.dma_val)
                        for s, v in last.items():
                            wait(("d", s), dsems[s], v)
                        for e in ENGS:
                            if cnt[e] > 0:
                                wait(("e", e), sems[e], cnt[e])
                return f

            block.tensor(body("pe"))
            block.scalar(body("act"))
            block.vector(body("dve"))
            block.gpsimd(body("pool"))
            block.sync(body("sp"))


S = 8192
D = 1024
NG_A = S // 512
bf = ml_dtypes.bfloat16
NEG = -1.0e30


def phase_A(nc, semstack, x, ogbuf, ogall, n_groups=NG_A, limit=None):
    dram = lambda n, sh, dt, kind="ExternalInput": nc.dram_tensor("a_" + n, sh, dt, kind=kind).ap()
    gbc = dram("gbc", [128, D], F32)
    w4 = dram("w4", [D, 1024], F32)
    qc = dram("qc", [4, 8, S], BF16)
    kc = dram("kc", [8, S], BF16)
    oh = dram("oh", [32, S], BF16)
    caus = dram("caus", [128, 2, 256], BF16)
    ident = dram("ident", [128, 128], BF16)
    sel = dram("sel", [128, 4], F32)
    s = Sched()
    with ExitStack() as st:
        sb = lambda name, shape, dt: st.enter_context(nc.sbuf_tensor("A_" + name, shape, dt))
        NX = 3
        xin = [sb("xin%d" % i, [128, D], F32) for i in range(NX)]
        gbc_sb = sb("gbc_sb", [128, D], F32)
        junk = sb("junk", [128, D], BF16)
        hn = [sb("hn%d" % i, [128, D], BF16) for i in range(2)]
        hT = sb("hT", [128, 8, 512], BF16)
        w_sb = sb("w_sb", [128, 8, 1024], BF16)
        kaug = sb("kaug", [104, 4, S], BF16)
        vaug = sb("vaug", [128, 64, 4, 65], BF16)
        qaug = [sb("qaug%d" % i, [104, 4, 512], BF16) for i in range(2)]
        sg = [sb("sg%d" % i, [64, 4, 512], BF16) for i in range(2)]
        gs = sb("gs", [128, 16, 32], F32)
        m8 = sb("m8", [128, 16, 8], F32)
        mk01 = sb("mk01", [128, 16, 32], F32)
        mkb = sb("mkb", [128, 16, 32], BF16)
        NP = 4
        pT = [sb("pT%d" % i, [128, 512], BF16) for i in range(NP)]
        kmean = sb("kmean", [64, 4, 32], BF16)
        ksum = sb("ksum", [64, 4, 2], F32)
        ss = sb("ss", [128, 4], F32)
        rstd = sb("rstd", [128, 4], F32)
        ones32 = sb("ones32", [128, 64], F32)
        rden = sb("rden", [128, 512], F32)
        t1 = [sb("t1_%d" % i, [64, 512], F32) for i in range(2)]
        og = [sb("og%d" % i, [64, 512], F32) for i in range(2)]
        og4 = [sb("og4_%d" % i, [64, 4, 512], F32) for i in range(2)]
        sel_sb = sb("sel_sb", [128, 4], F32)
        id_sb = sb("id_sb", [128, 128], BF16)
        caus_sb = sb("caus_sb", [128, 2, 256], BF16)
        psb = [st.enter_context(nc.psum_tensor("A_ps%d" % i, [128, 512], F32)) for i in range(8)]
        psS = psb[0:3]
        psO = psb[3:5]
        psP = psb[5:7]
        psM = psb[7]
        psM_bf = psM[:].bitcast(BF16)

        s.dma("sp", gbc_sb[:], gbc[:], writes=["gbc"])
        s.dma("sp", sel_sb[:], sel[:], writes=["sel"])
        s.dma("sp", id_sb[:], ident[:], writes=["ident"])
        s.dma("sp", caus_sb[:], caus[:], writes=["caus"])
        for c in range(8):
            slot = c % NX
            s.dma("sp", xin[slot][:], w4[c * 128:(c + 1) * 128, :], writes=[("xin", slot)])
            s.copy("pool", w_sb[:, c, :], xin[slot][:], reads=[("xin", slot)], writes=["w_sb"])
        for h in range(4):
            s.dma("pool", kaug[64:96, h, :], oh[:, :], writes=[("kaug_c", h, 0)])
            s.dma("pool", kaug[96:104, h, :], kc[:, :], writes=[("kaug_c", h, 1)])
        s.memset("pool", vaug[:, :, :, 64:65], 1.0, writes=["vaug_ones"])
        s.memset("pool", gs[:], NEG, writes=["gs"])
        s.memset("pool", ones32[:], 1.0, writes=["ones32"])
        for h in range(4):
            s.memset("pool", kmean[:, h, :], 0.0, writes=[("kmean", h)])

        xcount = [0]
        pcount = [0]
        scount = [0]
        ocount = [0]

        def stage_P(G):
            qs = G % 2
            t0 = G * 512
            s.dma("sp", qaug[qs][96:104, :, :], qc[:, :, t0:t0 + 512].rearrange("h r t -> r h t"),
                  writes=[("qaug_c", qs)])
            for tt in range(4):
                xs = xcount[0] % NX
                xcount[0] += 1
                hs = tt % 2
                tok = t0 + tt * 128
                s.dma("sp", xin[xs][:], x[tok:tok + 128, :], writes=[("xin", xs)])
                s.op("dve", lambda e, xs=xs, tt=tt: e.scalar_tensor_tensor(
                    out=junk[:], in0=xin[xs][:], scalar=1.0, in1=xin[xs][:],
                    op0=ALU.mult, op1=ALU.mult, accum_out=ss[:, tt:tt + 1]),
                    reads=[("xin", xs)], writes=["junk", ("ss", tt)])
                s.act(rstd[:, tt:tt + 1], ss[:, tt:tt + 1], AF.Ln, scale=1.0 / D, bias=eps_t[:, 0:1],
                      reads=[("ss", tt), "eps"], writes=[("rstd", tt)])
                s.act(rstd[:, tt:tt + 1], rstd[:, tt:tt + 1], AF.Exp, scale=-0.5,
                      reads=[("rstd", tt)], writes=[("rstd", tt)])
                s.stt(hn[hs][:], xin[xs][:], rstd[:, tt:tt + 1], gbc_sb[:], ALU.mult, ALU.mult,
                      reads=[("xin", xs), ("rstd", tt), "gbc"], writes=[("hn", hs)])
                for c in range(8):
                    s.tr(psM_bf[:, c * 128:(c + 1) * 128], hn[hs][:, c * 128:(c + 1) * 128], id_sb[:],
                         reads=[("hn", hs), "ident"], writes=["psM"])
                s.copy("dve", hT[:, :, tt * 128:(tt + 1) * 128],
                       psM_bf.rearrange("p (c t) -> p c t", c=8), reads=["psM"], writes=[("hT", tt)])
            hT_keys = [("hT", tt) for tt in range(4)]

            def proj(col0):
                ps = pcount[0] % 2
                pcount[0] += 1
                for c in range(8):
                    s.mm(psP[ps][:, :], w_sb[:, c, col0:col0 + 128], hT[:, c, :], start=(c == 0), stop=(c == 7),
                         reads=hT_keys + ["w_sb"], writes=[("psP", ps)])
                return ps

            for hp in range(2):
                ps = proj(hp * 128)
                for hh in range(2):
                    h = 2 * hp + hh
                    s.copy("dve", qaug[qs][0:64, h, :], psP[ps][hh * 64:(hh + 1) * 64, :], reads=[("psP", ps)],
                           writes=[("qaug_q", qs, h)])
                ps = proj(256 + hp * 128)
                for hh in range(2):
                    h = 2 * hp + hh
                    for b2 in range(2):
                        s.act(kaug[0:64, h, t0 + b2 * 256:t0 + (b2 + 1) * 256],
                              psP[ps][hh * 64:(hh + 1) * 64, b2 * 256:(b2 + 1) * 256],
                              AF.Copy, accum_out=ksum[:, h, b2:b2 + 1], reads=[("psP", ps)],
                              writes=[("kaug_k", h, G), ("ksum", h)])
                    s.ts("dve", kmean[:, h, 2 * G:2 * G + 2], ksum[:, h, :], 1.0 / 256.0, None, ALU.mult, None,
                         reads=[("ksum", h)], writes=[("kmean", h)])
                ps = proj(768 + hp * 128)
                for hh in range(2):
                    h = 2 * hp + hh
                    s.act(sg[qs][:, h, :], psP[ps][hh * 64:(hh + 1) * 64, :], AF.Silu, reads=[("psP", ps)],
                          writes=[("sg", qs, h)])
            for tt in range(4):
                ps = pcount[0] % 2
                pcount[0] += 1
                for c in range(8):
                    s.mm(psP[ps][:, 0:256], hT[:, c, tt * 128:(tt + 1) * 128], w_sb[:, c, 512:768],
                         start=(c == 0), stop=(c == 7), reads=[("hT", tt), "w_sb"], writes=[("psP", ps)])
                s.copy("dve", vaug[:, 4 * G + tt, :, 0:64], psP[ps][:, 0:256].rearrange("p (h d) -> p h d", h=4),
                       reads=[("psP", ps)], writes=[("vaug", 4 * G + tt)])
            psM3 = psM[:].rearrange("p (i n) -> p i n", n=32)
            for tt in range(4):
                for h in range(4):
                    s.mm(psM3[:, tt * 4 + h, :], qaug[qs][0:64, h, tt * 128:(tt + 1) * 128], kmean[:, h, :],
                         reads=[("qaug_q", qs, h), ("kmean", h)], writes=["psM"])
            own0, own1 = 2 * G, 2 * G + 1
            if own0 > 0:
                s.copy("dve", gs[:, 0:8, 0:own0], psM3[:, 0:8, 0:own0], reads=["psM"], writes=["gs"])
            s.copy("dve", gs[:, 8:16, 0:own1], psM3[:, 8:16, 0:own1], reads=["psM"], writes=["gs"])
            for i in range(16):
                s.op("dve", lambda e, i=i: e.max(out=m8[:, i, :], in_=gs[:, i, :]), reads=["gs"], writes=["m8"])
            s.tt("dve", mk01[:], gs[:], m8[:, :, 2:3].to_broadcast([128, 16, 32]), ALU.is_ge,
                 reads=["gs", "m8"], writes=["mk01"])
            s.ts("dve", mkb[:], mk01[:], -1.0, 30000.0, ALU.add, ALU.mult, reads=["mk01"], writes=["mkb"])
            s.memset("dve", mkb[:, 0:8, own0:own0 + 1], 0.0, writes=["mkb"])
            s.memset("dve", mkb[:, 8:16, own1:own1 + 1], 0.0, writes=["mkb"])
            psM4 = psM_bf.rearrange("p (h t) -> p h t", h=2)
            for hp in range(2):
                for hh in range(2):
                    h = hp * 2 + hh
                    for tt in range(4):
                        s.tr(psM4[64:96, hh, tt * 128:(tt + 1) * 128], mkb[:, tt * 4 + h, :], id_sb[:],
                             reads=["mkb", "ident"], writes=["psM"])
                s.copy("dve", qaug[qs][64:96, hp * 2:hp * 2 + 2, :], psM4[64:96, :, :], reads=["psM"],
                       writes=[("qaug_m", qs, hp)])

        def stage_A(G):
            qs = G % 2
            t0 = G * 512
            for h in range(4):
                os_ = ocount[0] % 2
                ocount[0] += 1
                chunks = []
                for c in range(4 * G):
                    chunks.append((c, 0, 512, None))
                for cc in range(2):
                    chunks.append((4 * G + cc, 0, 512, cc))
                for cc in range(2):
                    chunks.append((4 * G + 2 + cc, 256, 512, cc))
                qreads = [("qaug_q", qs, h), ("qaug_m", qs, h // 2), ("qaug_c", qs)]
                pend = []
                DLY = 2

                def emit_pv(item):
                    ci_, c_, c0_, c1_, pl_ = item
                    s.mm(psO[os_][0:65, c0_:c1_], vaug[:, c_, h, 0:65], pT[pl_][:, c0_:c1_],
                         start=(ci_ == 0), stop=(ci_ == len(chunks) - 1),
                         reads=[("pT", pl_), ("vaug", c_), "vaug_ones"], writes=[("psO", os_)])

                for ci, (c, c0, c1, diag) in enumerate(chunks):
                    sl = scount[0] % 3
                    pl = scount[0] % NP
                    scount[0] += 1
                    s.mm(psS[sl][:, c0:c1], kaug[0:104, h, c * 128:(c + 1) * 128], qaug[qs][0:104, h, c0:c1],
                         start=True, stop=(diag is None),
                         reads=qreads + [("kaug_k", h, c // 4), ("kaug_c", h, 0), ("kaug_c", h, 1)], writes=[("psS", sl)])
                    if diag is not None:
                        d0 = 0 if c0 == 0 else 256
                        s.mm(psS[sl][:, d0:d0 + 256], id_sb[:], caus_sb[:, diag, :], start=False, stop=True,
                             reads=["ident", "caus"], writes=[("psS", sl)])
                    s.act(pT[pl][:, c0:c1], psS[sl][:, c0:c1], AF.Exp, scale=0.125,
                          reads=[("psS", sl)], writes=[("pT", pl)])
                    pend.append((ci, c, c0, c1, pl))
                    if len(pend) > DLY:
                        emit_pv(pend.pop(0))
                while pend:
                    emit_pv(pend.pop(0))
                ts_ = os_
                s.op("dve", lambda e, os_=os_: e.reciprocal(out=rden[64:65, :], in_=psO[os_][64:65, :]),
                     reads=[("psO", os_)], writes=["rden"])
                s.mm(psM[0:64, :], ones32[64:65, 0:64], rden[64:65, :], reads=["ones32", "rden"], writes=["psM"])
                s.tt("dve", t1[ts_][:], psO[os_][0:64, :], sg[qs][:, h, :], ALU.mult,
                     reads=[("psO", os_), ("sg", qs, h)], writes=[("t1", ts_)])
                s.tt("dve", og[ts_][:], t1[ts_][:], psM[0:64, :], ALU.mult,
                     reads=[("t1", ts_), "psM"], writes=[("og", ts_)])
                for jj in range(4):
                    s.ts("pool", og4[ts_][:, jj, :], og[ts_][:], sel_sb[0:64, jj:jj + 1], 1.0, ALU.mult, ALU.mult,
                         reads=[("og", ts_), "sel"], writes=[("og4", ts_)])
                ck, c0 = t0 // 1024, t0 % 1024
                s.dma("pool", ogbuf[ck, :, c0:c0 + 512].rearrange("(j r) t -> r j t", j=4)[h * 64:(h + 1) * 64, :, :], og4[ts_][:],
                      reads=[("og4", ts_)], writes=[("ogbuf", ck)])

        eps_t = sb("eps_t", [128, 1], F32)
        s.memset("pool", eps_t[:], 1e-6, writes=["eps"])
        for G in range(n_groups + 1):
            if G < n_groups:
                stage_P(G)
            if G >= 1:
                stage_A(G - 1)
                if (G - 1) % 2 == 1:
                    ck_ = (G - 1) // 2
                    s.cc(lambda e, ck_=ck_: e.collective_compute("AllReduce", ALU.add, replica_groups=GROUPS,
                                                                 ins=[ogbuf[ck_].opt()], outs=[ogall[ck_].opt()]),
                         reads=[("ogbuf", ck_)])
        s.emit(nc, limit=limit, semstack=semstack, tag='A')


def consts_A(g):
    pos = np.arange(S)
    qc = np.zeros((4, 8, S), dtype=bf)
    for hl in range(4):
        hg = 4 * g + hl
        slope = 2.0 ** (-8.0 * (hg + 1) / 16)
        s8 = np.float64(8.0 * slope)
        p1 = np.float64(bf(s8)); p2 = np.float64(bf(s8 - p1)); p3 = np.float64(bf(s8 - p1 - p2))
        s8p = p1 + p2 + p3
        T = -s8p * pos.astype(np.float64)
        Thi = T.astype(bf); Tlo = (T - Thi.astype(np.float64)).astype(bf)
        qc[hl, 0] = p1; qc[hl, 1] = p2; qc[hl, 2] = p3
        qc[hl, 3] = p1; qc[hl, 4] = p2; qc[hl, 5] = p3
        qc[hl, 6] = Thi; qc[hl, 7] = Tlo
    kc = np.zeros((8, S), dtype=bf)
    kc[0:3] = (pos % 256).astype(bf)
    kc[3:6] = (256 * (pos // 256)).astype(bf)
    kc[6:8] = 1.0
    oh = np.zeros((32, S), dtype=bf)
    oh[pos // 256, pos] = 1.0
    caus = np.zeros((128, 2, 256), dtype=bf)
    j = np.arange(128)[:, None]
    i = np.arange(256)[None, :]
    for c in range(2):
        caus[:, c, :] = np.where(c * 128 + j <= i, 0.0, -30000.0).astype(bf)
    ident = np.eye(128).astype(bf)
    return dict(qc=qc, kc=kc, oh=oh, caus=caus, ident=ident)


F32R = mybir.dt.float32r
S = 8192
D = 1024
bf = ml_dtypes.bfloat16
CDEC = 0.6065306597126334
L = 128


GT = 256
NCH = GT // L


def phase_B(nc, semstack, x, ogall, og1buf, og1all, n_groups=S // GT, limit=None):
    dram = lambda n, sh, dt, kind="ExternalInput": nc.dram_tensor("b_" + n, sh, dt, kind=kind).ap()
    wout0 = dram("wout0", [D, D], F32)
    gbc = dram("gbc", [128, D], F32)
    mixT = dram("mixT", [128, 8, 6], F32)
    w4 = dram("w4", [D, 1024], F32)
    wl = dram("wl", [D, 128], F32)
    w2a2 = dram("w2a2", [64, 2, 256], F32)
    cpar = dram("cpar", [64, 5, 4], F32)
    lnx = dram("lnx", [128, 2, 256], F32)
    masks = dram("masks", [128, 3, 128], F32)
    identf = dram("identf", [128, 128], F32)
    identb = dram("identb", [128, 128], BF16)
    sel = dram("sel", [128, 4], F32)
    s = Sched()
    with ExitStack() as st:
        sb = lambda name, shape, dt: st.enter_context(nc.sbuf_tensor("B_" + name, shape, dt))
        NX = 2
        xin = [sb("xin%d" % i, [128, D], F32) for i in range(NX)]
        ogin = [sb("ogin%d" % i, [128, 8, 128], BF16) for i in range(1)] * 2
        ogf = [sb("ogf%d" % i, [128, 8, 128], F32) for i in range(1)] * 2
        sel_sb = sb("sel_sb", [128, 4], F32)
        ogo4 = [sb("ogo4_%d" % i, [128, 4, 256], F32) for i in range(1)] * 2
        x1t = sb("x1t", [128, D], F32)
        hn = sb("hn", [128, D], BF16)
        junk = hn
        hx = sb("hx", [128, 8, GT + 4], BF16)
        lastcol = sb("lastcol", [128, 8, 2], BF16)
        gbc_sb = sb("gbc_sb", [128, D], F32)
        wo_sb = sb("wo_sb", [128, 8, 1024], BF16)
        wst = x1t
        wa_sb = sb("wa_sb", [128, 8, 1024], BF16)
        wb_sb = sb("wb_sb", [128, 8, 1024], BF16)
        wla_sb = sb("wla_sb", [128, 8, 128], BF16)
        wlb_sb = sb("wlb_sb", [128, 8, 128], BF16)
        mix_sb = sb("mix_sb", [128, 8, 6], F32)
        onem_sb = sb("onem_sb", [128, 8, 6], F32)
        w2a2_sb = sb("w2a2_sb", [64, 2, 256], BF16)
        w2st = sb("w2st", [64, 2, 256], F32)
        cp = sb("cp", [64, 5, 4], F32)
        onemka = sb("onemka", [64, 4], F32)
        rk2 = sb("rk2", [64, 4, 2], F32R)
        lnx_sb = sb("lnx_sb", [128, 2, 256], F32)
        mk = sb("mk", [128, 3, 128], F32)
        idf = sb("idf", [128, 128], F32)
        idb = sb("idb", [128, 128], BF16)
        ones_r = sb("ones_r", [64, 64], F32R)
        ones_f = sb("ones_f", [64, 128], F32)
        eps_t = sb("eps_t", [128, 3], F32)
        ss = sb("ss", [128, 2], F32)
        rstd = sb("rstd", [128, 2], F32)
        Gt = lambda name, dt=F32: sb(name, [64, 4, GT], dt)
        r_c2 = [Gt("r_c%d" % i) for i in range(2)]; k_c2 = [Gt("k_c%d" % i) for i in range(2)]
        sgw2 = [Gt("sgw%d" % i) for i in range(2)]; a_c2 = [Gt("a_c%d" % i) for i in range(2)]
        lhid2 = [sb("lhid%d" % i, [64, 2, GT], BF16) for i in range(2)]
        v_r2 = [sb("v_r%d" % i, [128, NCH, 256], F32R) for i in range(2)]
        sg_t2 = [sb("sg_t%d" % i, [128, NCH, 256], F32) for i in range(2)]
        Ct = lambda name, dt=F32: sb(name, [64, 4, L], dt)
        kk = Ct("kk"); tmp1 = Ct("tmp1"); tmp2 = Ct("tmp2", F32R); cum = Ct("cum"); ex = Ct("ex")
        g_incl = Ct("g_incl"); g_inv = Ct("g_inv"); g_excl = Ct("g_excl"); g_rem = Ct("g_rem")
        b_c = Ct("b_c"); km = Ct("km")
        AR = sb("AR", [64, 4, 2, L], F32R)
        bh = Ct("bh", F32R); kh = Ct("kh", F32R); rkp = Ct("rkp", F32R)
        Bt = Ct("Bt"); Kt = Ct("Kt")
        gLt = sb("gLt", [64, 4, 2], F32)
        NU = 4
        A1sb = [sb("A1sb%d" % i, [128, 256], F32R) for i in range(NU)]
        A2sb = [sb("A2sb%d" % i, [128, 256], F32R) for i in range(NU)]
        QMP = [[sb("QMP%d_%d" % (i, j), [128, 384], F32R) for j in range(2)] for i in range(NU)]
        tok3 = [sb("tok3_%d" % i, [128, 3, 64], F32R) for i in range(NU)]
        G1sb = [sb("G1sb%d" % i, [64, 128], F32R) for i in range(NU)]
        AVsb = [sb("AVsb%d" % i, [128, 64], F32R) for i in range(NU)]
        P2sb = [sb("P2sb%d" % i, [128, 64], F32) for i in range(NU)]
        Usb = [sb("Usb%d" % i, [128, 64], F32R) for i in range(NU)]
        Ssb = [[sb("Ssb%d_%d" % (h, j), [64, 64], F32R) for j in range(2)] for h in range(4)]
        zer = sb("zer", [64, 64], F32)
        ysb = sb("ysb", [128, 4, 64], F32)
        bst = sb("bst", [128, 4, 6], F32)
        mv = sb("mv", [128, 4, 2], F32)
        rs4 = sb("rs4", [128, 4], F32)
        yn = sb("yn", [128, 256], F32)
        rks = sb("rks", [128, 4], F32)
        ogo = [sb("ogo%d" % i, [128, 256], F32) for i in range(1)] * 2
        psb = [st.enter_context(nc.psum_tensor("B_ps%d" % i, [128, 512], F32)) for i in range(8)]
        psP = psb[0:2]
        psT = psb[1]
        psT_bf = psT[:].bitcast(BF16)
        psN = psb[2:6]
        psAs = psb[6:8]

        s.dma("sp", gbc_sb[:], gbc[:], writes=["gbc"])
        s.dma("sp", sel_sb[:], sel[:], writes=["sel"])
        s.dma("sp", idb[:], identb[:], writes=["idb"])
        s.dma("sp", idf[:], identf[:], writes=["idf"])
        s.dma("sp", mk[:], masks[:], writes=["mk"])
        s.dma("sp", mix_sb[:], mixT[:], writes=["mix"])
        s.dma("sp", cp[:], cpar[:], writes=["cp"])
        s.dma("sp", lnx_sb[:], lnx[:], writes=["lnx"])
        s.dma("sp", w2st[:], w2a2[:], writes=["w2st"])
        s.copy("pool", w2a2_sb[:], w2st[:], reads=["w2st"], writes=["w2a2"])
        s.ts("dve", onem_sb[:], mix_sb[:], -1.0, 1.0, ALU.mult, ALU.add, reads=["mix"], writes=["onem"])
        s.ts("dve", onemka[:], cp[:, 3, :], -1.0, 1.0, ALU.mult, ALU.add, reads=["cp"], writes=["onemka"])
        s.copy("dve", rk2[:], cp[:, 4, :].unsqueeze(2).to_broadcast([64, 4, 2]), reads=["cp"], writes=["rk2"])
        s.memset("pool", ones_f[:], 1.0, writes=["ones_f"])
        s.copy("dve", ones_r[:], ones_f[:, 0:64], reads=["ones_f"], writes=["ones_r"])
        s.memset("pool", eps_t[:, 0:1], 1e-6, writes=["eps"])
        s.memset("pool", eps_t[:, 1:2], 64e-5, writes=["eps"])
        s.memset("pool", eps_t[:, 2:3], 1e-30, writes=["eps"])
        s.memset("pool", hx[:], 0.0, writes=[("hx", 0), ("hx", 1), "hx0"])
        s.memset("pool", zer[:], 0.0, writes=["zer"])
        stg = [(x1t, "wst"), (xin[0], ("xin", 0)), (xin[1], ("xin", 1))]
        sk = [0]

        def nstg():
            t_, k_ = stg[sk[0] % 3]
            sk[0] += 1
            return t_, k_

        for c in range(8):
            w_, k_ = nstg()
            s.dma("sp", w_[:], wout0[c * 128:(c + 1) * 128, :], writes=[k_])
            s.copy("pool", wo_sb[:, c, :], w_[:], reads=[k_], writes=["wo_sb"])
        for c in range(8):
            w_, k_ = nstg()
            s.dma("sp", w_[:], w4[c * 128:(c + 1) * 128, :], writes=[k_])
            for n in range(4):
                s.ts("dve", wa_sb[:, c, n * 256:(n + 1) * 256], w_[:, n * 256:(n + 1) * 256], onem_sb[:, c, n:n + 1], None,
                     ALU.mult, reads=[k_, "onem"], writes=["wa_sb"])
                s.ts("pool", wb_sb[:, c, n * 256:(n + 1) * 256], w_[:, n * 256:(n + 1) * 256], mix_sb[:, c, n:n + 1], 1.0,
                     ALU.mult, ALU.mult, reads=[k_, "mix"], writes=["wb_sb"])
        for c in range(8):
            w_, k_ = nstg()
            s.dma("sp", w_[:, 0:128], wl[c * 128:(c + 1) * 128, :], writes=[k_])
            for n in range(2):
                s.ts("dve", wla_sb[:, c, n * 64:(n + 1) * 64], w_[:, n * 64:(n + 1) * 64], onem_sb[:, c, 4 + n:5 + n], None,
                     ALU.mult, reads=[k_, "onem"], writes=["wla_sb"])
                s.ts("pool", wlb_sb[:, c, n * 64:(n + 1) * 64], w_[:, n * 64:(n + 1) * 64], mix_sb[:, c, 4 + n:5 + n], 1.0,
                     ALU.mult, ALU.mult, reads=[k_, "mix"], writes=["wlb_sb"])
        for h in range(4):
            s.copy("dve", Ssb[h][0][:], zer[:], reads=["zer"], writes=[("S", h, 0)])

        cnt = {"x": 0, "p": 0, "u": 0, "o": 0}
        allh = lambda n, gp=None: [((n, h) if gp is None else (n, h, gp)) for h in range(4)]

        def nextp():
            p = cnt["p"] % 2
            cnt["p"] += 1
            return p

        def stage_Pa(Gi):
            t0 = Gi * GT
            if Gi > 0:
                s.copy("pool", hx[:, :, 3:4], lastcol[:, :, 0:1], reads=["lastcol"], writes=["hx0"])
            for tt in range(NCH):
                xs = cnt["x"] % NX
                cnt["x"] += 1
                tok = t0 + tt * 128
                s.dma("sp", xin[xs][:], x[tok:tok + 128, :], writes=[("xin", xs)])
                s.dma("pool", ogf[xs][:], ogall[tok // 1024, :, tok % 1024:tok % 1024 + 128].rearrange("(c p) t -> p c t", p=128), writes=["ogf"])
                s.copy("pool", ogin[xs][:], ogf[xs][:], reads=["ogf"], writes=["ogin"])
                yield
                for half in range(2):
                    ps = nextp()
                    for c in range(8):
                        s.mm(psP[ps][:, :], ogin[xs][:, c, :], wo_sb[:, c, half * 512:(half + 1) * 512], start=(c == 0), stop=(c == 7),
                             reads=["ogin", "wo_sb"], writes=[("psP", ps)])
                    yield
                    s.tt("dve", x1t[:, half * 512:(half + 1) * 512], psP[ps][:, :], xin[xs][:, half * 512:(half + 1) * 512], ALU.add,
                         reads=[("psP", ps), ("xin", xs)], writes=[("x1t", half), "wst"])
                    yield
                s.op("dve", lambda e, tt=tt: e.scalar_tensor_tensor(
                    out=junk[:], in0=x1t[:], scalar=1.0, in1=x1t[:],
                    op0=ALU.mult, op1=ALU.mult, accum_out=ss[:, tt:tt + 1]),
                    reads=[("x1t", 0), ("x1t", 1)], writes=["hn", ("ss", tt)])
                s.act(rstd[:, tt:tt + 1], ss[:, tt:tt + 1], AF.Ln, scale=1.0 / D, bias=eps_t[:, 0:1],
                      reads=[("ss", tt), "eps"], writes=[("rstd", tt)])
                s.act(rstd[:, tt:tt + 1], rstd[:, tt:tt + 1], AF.Exp, scale=-0.5,
                      reads=[("rstd", tt)], writes=[("rstd", tt)])
                s.stt(hn[:], x1t[:], rstd[:, tt:tt + 1], gbc_sb[:], ALU.mult, ALU.mult,
                      reads=[("x1t", 0), ("x1t", 1), ("rstd", tt), "gbc"], writes=["hn"])
                yield
                for c in range(8):
                    s.tr(psT_bf[:, c * 128:(c + 1) * 128], hn[:, c * 128:(c + 1) * 128], idb[:],
                         reads=["hn", "idb"], writes=[("psP", 1)])
                yield
                s.copy("act", hx[:, :, 4 + tt * 128:4 + (tt + 1) * 128],
                       psT_bf.rearrange("p (c t) -> p c t", c=8), reads=[("psP", 1)], writes=[("hx", tt)])
                yield
            s.copy("pool", lastcol[:, :, 0:1], hx[:, :, GT + 3:GT + 4], reads=[("hx", NCH - 1)], writes=["lastcol"])
            yield

        def stage_Pb(Gi):
            gp = Gi % 2
            r_c, k_c, sgw, a_c, lhid, v_r, sg_t = r_c2[gp], k_c2[gp], sgw2[gp], a_c2[gp], lhid2[gp], v_r2[gp], sg_t2[gp]
            hkeys = [("hx", tt) for tt in range(NCH)] + ["hx0"]

            def proj_cm(wa, wb, col0, ncols, ps_):
                out_ps = psP[ps_][0:ncols, 0:GT]
                for c in range(8):
                    s.mm(out_ps, wa[:, c, col0:col0 + ncols], hx[:, c, 4:GT + 4], start=(c == 0), stop=False,
                         reads=hkeys + ["wa_sb", "wla_sb"], writes=[("psP", ps_)])
                for c in range(8):
                    s.mm(out_ps, wb[:, c, col0:col0 + ncols], hx[:, c, 3:GT + 3], start=False, stop=(c == 7),
                         reads=hkeys + ["wb_sb", "wlb_sb"], writes=[("psP", ps_)])
                return out_ps

            ps_ = nextp()
            proj_cm(wla_sb, wlb_sb, 0, 128, ps_)
            s.act(lhid[:, 0, :], psP[ps_][0:64, 0:GT], AF.Tanh, reads=[("psP", ps_)], writes=[("lhid", 0, gp)])
            s.copy("act", lhid[:, 1, :], psP[ps_][64:128, 0:GT], reads=[("psP", ps_)], writes=[("lhid", 1, gp)])
            yield
            for h in range(4):
                ps_ = nextp()
                o_ = psP[ps_][0:64, 0:GT]
                s.mm(o_, w2a2_sb[:, 0, h * 64:(h + 1) * 64], lhid[:, 0, :], reads=["w2a2", ("lhid", 0, gp)], writes=[("psP", ps_)])
                s.act(sgw[:, h, :], o_, AF.Sigmoid, bias=cp[:, 0, h:h + 1], reads=[("psP", ps_), "cp"], writes=[("sgw", h, gp)])
                yield
                ps_ = nextp()
                o_ = psP[ps_][0:64, 0:GT]
                s.mm(o_, w2a2_sb[:, 1, h * 64:(h + 1) * 64], lhid[:, 1, :], reads=["w2a2", ("lhid", 1, gp)], writes=[("psP", ps_)])
                s.act(a_c[:, h, :], o_, AF.Sigmoid, bias=cp[:, 1, h:h + 1], reads=[("psP", ps_), "cp"], writes=[("a_c", h, gp)])
                yield
            for hp in range(2):
                ps_ = nextp()
                proj_cm(wa_sb, wb_sb, hp * 128, 128, ps_)
                s.copy("dve", r_c[:, 2 * hp, :], psP[ps_][0:64, 0:GT], reads=[("psP", ps_)], writes=[("r_c", 2 * hp, gp)])
                s.copy("dve", r_c[:, 2 * hp + 1, :], psP[ps_][64:128, 0:GT], reads=[("psP", ps_)], writes=[("r_c", 2 * hp + 1, gp)])
                yield
                ps_ = nextp()
                proj_cm(wa_sb, wb_sb, 256 + hp * 128, 128, ps_)
                s.copy("act", k_c[:, 2 * hp, :], psP[ps_][0:64, 0:GT], reads=[("psP", ps_)], writes=[("k_c", 2 * hp, gp)])
                s.copy("act", k_c[:, 2 * hp + 1, :], psP[ps_][64:128, 0:GT], reads=[("psP", ps_)], writes=[("k_c", 2 * hp + 1, gp)])
                yield
            for tt in range(NCH):
                ps_ = nextp()
                for c in range(8):
                    s.mm(psP[ps_][:, :], hx[:, c, 4 + tt * 128:4 + (tt + 1) * 128], wa_sb[:, c, 512:1024], start=(c == 0), stop=False,
                         reads=hkeys + ["wa_sb"], writes=[("psP", ps_)])
                for c in range(8):
                    s.mm(psP[ps_][:, :], hx[:, c, 3 + tt * 128:3 + (tt + 1) * 128], wb_sb[:, c, 512:1024], start=False, stop=(c == 7),
                         reads=hkeys + ["wb_sb"], writes=[("psP", ps_)])
                s.copy("dve", v_r[:, tt, :], psP[ps_][:, 0:256], reads=[("psP", ps_)], writes=[("v_r", tt, gp)])
                s.act(sg_t[:, tt, :], psP[ps_][:, 256:512], AF.Sigmoid, reads=[("psP", ps_)], writes=[("sg_t", tt, gp)])
                s.tt("dve", sg_t[:, tt, :], sg_t[:, tt, :], psP[ps_][:, 256:512], ALU.mult, reads=[("psP", ps_), ("sg_t", tt, gp)], writes=[("sg_t", tt, gp)])
                yield

        def chunk_elem(j, gp):
            r_c, k_c, sgw, a_c, lhid, v_r, sg_t = r_c2[gp], k_c2[gp], sgw2[gp], a_c2[gp], lhid2[gp], v_r2[gp], sg_t2[gp]
            cs = slice(j * L, (j + 1) * L)
            bc = lambda i: cp[:, i, :].unsqueeze(2).to_broadcast([64, 4, L])
            for h in range(4):
                s.op("dve", lambda e, h=h: e.tensor_tensor_scan(
                    out=cum[:, h, :], data0=ones_f[:, 0:L], data1=sgw[:, h, cs],
                    initial=0.0, op0=ALU.mult, op1=ALU.add), reads=[("sgw", h, gp), "ones_f"], writes=["cum"])
                yield
            s.tt("pool", ex[:], cum[:], sgw[:, :, cs], ALU.subtract, reads=["cum"] + allh("sgw", gp), writes=["ex"])
            yield
            s.tt("dve", g_rem[:], cum[:], cum[:, :, L - 1:L].to_broadcast([64, 4, L]), ALU.subtract, reads=["cum"], writes=["g_rem"])
            yield
            s.tt("pool", km[:], a_c[:, :, cs], bc(3), ALU.mult, reads=allh("a_c", gp) + ["cp"], writes=["km"])
            yield
            s.act(g_rem[:], g_rem[:], AF.Exp, scale=CDEC, reads=["g_rem"], writes=["g_rem"])
            yield
            s.act(g_incl[:], cum[:], AF.Exp, scale=-CDEC, reads=["cum"], writes=["g_incl"])
            yield
            s.tt("dve", kk[:], k_c[:, :, cs], bc(2), ALU.mult, reads=allh("k_c", gp) + ["cp"], writes=["kk"])
            yield
            s.tt("dve", tmp2[:], kk[:], kk[:], ALU.mult, reads=["kk"], writes=["tmp2"])
            yield
            s.tt("pool", km[:], km[:], onemka[:].unsqueeze(2).to_broadcast([64, 4, L]), ALU.add, reads=["km", "onemka"], writes=["km"])
            yield
            s.act(g_inv[:], cum[:], AF.Exp, scale=CDEC, reads=["cum"], writes=["g_inv"])
            yield
            s.act(g_excl[:], ex[:], AF.Exp, scale=-CDEC, reads=["ex"], writes=["g_excl"])
            yield
            for h in range(4):
                ps_ = nextp()
                o_ = psP[ps_][0:64, 0:L]
                s.mm(o_, ones_r[:, :], tmp2[:, h, :], reads=["ones_r", "tmp2"], writes=[("psP", ps_)])
                yield
                s.act(tmp1[:, h, :], o_, AF.Ln, bias=eps_t[0:64, 2:3], reads=[("psP", ps_), "eps"], writes=["tmp1"])
                yield
            s.tt("pool", km[:], k_c[:, :, cs], km[:], ALU.mult, reads=allh("k_c", gp) + ["km"], writes=["km"])
            yield
            s.act(tmp1[:], tmp1[:], AF.Exp, scale=-0.5, reads=["tmp1"], writes=["tmp1"])
            yield
            s.tt("dve", AR[:, :, 1, :], r_c[:, :, cs], g_incl[:], ALU.mult, reads=allh("r_c", gp) + ["g_incl"], writes=["AR_rt"])
            yield
            s.tt("dve", kk[:], kk[:], tmp1[:], ALU.mult, reads=["kk", "tmp1"], writes=["kk"])
            yield
            s.stt(AR[:, :, 0, :], kk[:], -1.0, g_excl[:], ALU.mult, ALU.mult, reads=["kk", "g_excl"], writes=["AR_at"])
            yield
            s.tt("dve", b_c[:], kk[:], a_c[:, :, cs], ALU.mult, reads=["kk"] + allh("a_c", gp), writes=["b_c"])
            yield
            s.tt("dve", bh[:], b_c[:], g_inv[:], ALU.mult, reads=["b_c", "g_inv"], writes=["bh"])
            yield
            s.tt("dve", kh[:], km[:], g_inv[:], ALU.mult, reads=["km", "g_inv"], writes=["kh"])
            yield
            s.tt("pool", Bt[:], b_c[:], g_rem[:], ALU.mult, reads=["b_c", "g_rem"], writes=["Bt"])
            yield
            s.tt("dve", rkp[:], r_c[:, :, cs], km[:], ALU.mult, reads=allh("r_c", gp) + ["km"], writes=["rkp"])
            yield
            s.tt("pool", Kt[:], km[:], g_rem[:], ALU.mult, reads=["km", "g_rem"], writes=["Kt"])
            yield
            s.copy("pool", gLt[:, :, 0:1], g_incl[:, :, L - 1:L], reads=["g_incl"], writes=["gLt"])
            yield

        def unit_pre(j, h, u, gp):
            r_c, k_c, sgw, a_c, lhid, v_r, sg_t = r_c2[gp], k_c2[gp], sgw2[gp], a_c2[gp], lhid2[gp], v_r2[gp], sg_t2[gp]
            AR2 = AR[:, h, :, :].rearrange("c a t -> c (a t)")
            pa = u % 2
            psA_ = psAs[pa]
            ka = ("psA", pa)
            kn = ("psN", u)
            s.mm(psA_[:, 0:256], bh[:, h, :], AR2, reads=["bh", "AR_at", "AR_rt"], writes=[ka])
            s.mm(psA_[:, 256:512], kh[:, h, :], AR2, reads=["kh", "AR_at", "AR_rt"], writes=[ka])
            s.mm(psN[u][:, 256:384], AR[:, h, 0, :], bh[:, h, :], reads=["bh", "AR_at"], writes=[kn])
            yield
            mUI = mk[:, 0:2, :].rearrange("p a t -> p (a t)")
            s.tt("dve", A1sb[u][:], psA_[:, 0:256], mUI, ALU.mult, reads=[ka, "mk"], writes=[("A1", u)])
            s.tt("dve", QMP[u][0][:, 0:128], psA_[:, 0:128], mk[:, 0, :], ALU.mult, reads=[ka, "mk"], writes=[("QMP", u, 0)])
            s.tt("dve", QMP[u][0][:, 256:384], psN[u][:, 256:384], mk[:, 2, :], ALU.mult, reads=[kn, "mk"], writes=[("QMP", u, 0)])
            s.tt("dve", A2sb[u][:], psA_[:, 256:512], mUI, ALU.mult, reads=[ka, "mk"], writes=[("A2", u)])
            s.tt("dve", QMP[u][0][:, 128:256], A1sb[u][:, 0:128].bitcast(F32), idf[:], ALU.add, reads=[("A1", u), "idf"], writes=[("QMP", u, 0)])
            s.tt("dve", QMP[u][1][:, 128:256], A1sb[u][:, 0:128].bitcast(F32), idf[:], ALU.add, reads=[("A1", u), "idf"], writes=[("QMP", u, 1)])
            yield
            cur = 0
            for it in range(7):
                nxt = 1 - cur
                last = (it == 6)
                T_ = QMP[u][cur]
                Qc = T_[:, 0:128]
                Pc = T_[:, 256:384]
                kc_ = ("QMP", u, cur)
                if it == 0:
                    s.mm(psN[u][:, 0:128], Pc, Qc, reads=[kc_], writes=[kn])
                elif not last:
                    s.mm(psN[u][:, 0:256], Pc, T_[:, 0:256], reads=[kc_], writes=[kn])
                else:
                    s.mm(psN[u][:, 128:256], Pc, T_[:, 128:256], reads=[kc_], writes=[kn])
                if not last:
                    s.mm(psN[u][:, 256:384], Qc, Pc, reads=[kc_], writes=[kn])
                yield
                if not last:
                    s.copy("act", QMP[u][nxt][:].rearrange("p (a t) -> p a t", a=3)[:, 0:3:2, :],
                           psN[u][:, 0:384].rearrange("p (a t) -> p a t", a=3)[:, 0:3:2, :], reads=[kn], writes=[("QMP", u, nxt)])
                if it >= 1:
                    s.tt("dve", QMP[u][nxt][:, 128:256], psN[u][:, 128:256], T_[:, 128:256].bitcast(F32), ALU.add,
                         reads=[kn, kc_], writes=[("QMP", u, nxt)])
                yield
                cur = nxt
            M = QMP[u][cur][:, 128:256]
            Mkey = ("QMP", u, cur)
            psX = psN[u]
            s.tr(psX[:, 0:64], AR[:, h, 0, :].bitcast(F32), idf[0:64, 0:64], reads=["AR_at", "idf"], writes=[kn])
            s.tr(psX[:, 64:128], Bt[:, h, :], idf[0:64, 0:64], reads=["Bt", "idf"], writes=[kn])
            s.tr(psX[:, 128:192], Kt[:, h, :], idf[0:64, 0:64], reads=["Kt", "idf"], writes=[kn])
            s.mm(psX[:, 320:384], A2sb[u][:, 0:128], v_r[:, j, h * 64:(h + 1) * 64], reads=[("A2", u), ("v_r", j, gp)], writes=[kn])
            yield
            s.copy("act", tok3[u][:].rearrange("p a k -> p (a k)"), psX[:, 0:192], reads=[kn], writes=[("tok3", u)])
            s.copy("act", AVsb[u][:], psX[:, 320:384], reads=[kn], writes=[("AV", u)])
            yield
            s.mm(psX[0:64, 192:320], tok3[u][:, 0, :], M, reads=[("tok3", u), Mkey], writes=[kn])
            s.mm(psX[:, 384:448], M, AVsb[u][:], reads=[Mkey, ("AV", u)], writes=[kn])
            yield
            s.copy("act", G1sb[u][:], psX[0:64, 192:320], reads=[kn], writes=[("G1", u)])
            s.copy("act", P2sb[u][:], psX[:, 384:448], reads=[kn], writes=[("P2", u)])
            yield

        def unit_chain(Gi, j, h, u):
            gp = Gi % 2
            r_c, k_c, sgw, a_c, lhid, v_r, sg_t = r_c2[gp], k_c2[gp], sgw2[gp], a_c2[gp], lhid2[gp], v_r2[gp], sg_t2[gp]
            cidx = Gi * NCH + j
            so = cidx % 2
            sn = 1 - so
            vv = v_r[:, j, h * 64:(h + 1) * 64]
            psC = psN[u]
            Ups = psC[:, 448:512]
            Sps = psC[0:64, 0:64]
            Yps = psC[:, 64:128]
            Rps = psC[:, 128:130]
            key = ("psN", u)
            s.mm(Ups, G1sb[u][:], Ssb[h][so][:], reads=[("G1", u), ("S", h, so)], writes=[key])
            yield
            s.tt("dve", Usb[u][:], Ups, P2sb[u][:], ALU.add, reads=[key, ("P2", u)], writes=[("U", u)])
            yield
            s.mm(Sps, tok3[u][:, 2, :], vv, start=True, stop=False, reads=[("tok3", u), ("v_r", j, gp)], writes=[key])
            s.mm(Sps, tok3[u][:, 1, :], Usb[u][:], start=False, stop=True, reads=[("tok3", u), ("U", u)], writes=[key])
            s.mm(Yps, AR[:, h, 1, :], Ssb[h][so][:], start=True, stop=False, reads=["AR_rt", ("S", h, so)], writes=[key])
            s.mm(Yps, A1sb[u][:, 128:256], Usb[u][:], start=False, stop=False, reads=[("A1", u), ("U", u)], writes=[key])
            s.mm(Yps, A2sb[u][:, 128:256], vv, start=False, stop=True, reads=[("A2", u), ("v_r", j, gp)], writes=[key])
            s.mm(Rps, rkp[:, h, :], rk2[:, h, :], reads=["rkp", "rk2"], writes=[key])
            yield
            s.stt(Ssb[h][sn][:], Ssb[h][so][:].bitcast(F32), gLt[:, h, 0:1], Sps, ALU.mult, ALU.add,
                  reads=[("S", h, so), "gLt", key], writes=[("S", h, sn)])
            s.copy("act", ysb[:, h, :], Yps, reads=[key], writes=[("ysb", h)])
            s.copy("act", rks[:, h:h + 1], Rps[:, 0:1], reads=[key], writes=[("rks", h)])
            yield
            s.op("dve", lambda e, h=h: e.bn_stats(out=bst[:, h, :], in_=ysb[:, h, :]), reads=[("ysb", h)], writes=[("bst", h)])
            s.op("dve", lambda e, h=h: e.bn_aggr(out=mv[:, h, :], in_=bst[:, h, :]), reads=[("bst", h)], writes=[("mv", h)])
            yield

        def rr(gens):
            gens = list(gens)
            while gens:
                for g_ in list(gens):
                    try:
                        next(g_)
                    except StopIteration:
                        gens.remove(g_)

        def chunk_out(Gi, j):
            gp = Gi % 2
            r_c, k_c, sgw, a_c, lhid, v_r, sg_t = r_c2[gp], k_c2[gp], sgw2[gp], a_c2[gp], lhid2[gp], v_r2[gp], sg_t2[gp]
            t0 = Gi * GT + j * L
            oo = cnt["o"] % 2
            cnt["o"] += 1
            s.act(rs4[:], mv[:, :, 1], AF.Ln, bias=eps_t[:, 1:2], reads=allh("mv") + ["eps"], writes=["rs4"])
            yield
            s.act(rs4[:], rs4[:], AF.Exp, scale=-0.5, reads=["rs4"], writes=["rs4"])
            yield
            for h in range(4):
                s.ts("dve", yn[:, h * 64:(h + 1) * 64], ysb[:, h, :], mv[:, h, 0:1], rs4[:, h:h + 1], ALU.subtract, ALU.mult,
                     reads=[("ysb", h), ("mv", h), "rs4"], writes=["yn"])
                yield
            s.tt("pool", yn[:], yn[:], lnx_sb[:, 0, :], ALU.mult, reads=["yn", "lnx"], writes=["yn"])
            yield
            s.tt("pool", yn[:], yn[:], lnx_sb[:, 1, :], ALU.add, reads=["yn", "lnx"], writes=["yn"])
            yield
            for h in range(4):
                s.stt(ogo[oo][:, h * 64:(h + 1) * 64], v_r[:, j, h * 64:(h + 1) * 64].bitcast(F32), rks[:, h:h + 1], yn[:, h * 64:(h + 1) * 64],
                      ALU.mult, ALU.add, reads=[("v_r", j, gp), ("rks", h), "yn"], writes=["ogo"])
                yield
            s.tt("dve", ogo[oo][:], ogo[oo][:], sg_t[:, j, :], ALU.mult, reads=["ogo", ("sg_t", j, gp)], writes=["ogo"])
            yield
            for jj in range(4):
                s.ts("pool", ogo4[oo][:, jj, :], ogo[oo][:], sel_sb[:, jj:jj + 1], 1.0, ALU.mult, ALU.mult,
                     reads=["ogo", "sel"], writes=["ogo4"])
                yield
            s.dma("sp", og1buf[t0 // 1024, t0 % 1024:t0 % 1024 + 128, :].rearrange("t (j f) -> t j f", j=4), ogo4[oo][:], reads=["ogo4"], writes=[("og1buf", t0 // 1024)])
            yield
            if (t0 + 128) % 1024 == 0:
                ck_ = t0 // 1024
                s.cc(lambda e, ck_=ck_: e.collective_compute("AllReduce", ALU.add, replica_groups=GROUPS,
                                                             ins=[og1buf[ck_].opt()], outs=[og1all[ck_].opt()]),
                     reads=[("og1buf", ck_)])
                yield

        def chain_gens(gs):
            for g_ in gs:
                yield from g_

        def rr2(gens, bg):
            gens = list(gens)
            while gens:
                for g_ in list(gens):
                    try:
                        next(g_)
                    except StopIteration:
                        gens.remove(g_)
                if bg[0] is not None:
                    try:
                        next(bg[0])
                    except StopIteration:
                        bg[0] = None

        rr([stage_Pa(0)])
        rr([stage_Pb(0)])
        prev_out = []
        for Gi in range(n_groups):
            gp = Gi % 2
            bg = [chain_gens([stage_Pa(Gi + 1), stage_Pb(Gi + 1)])] if Gi + 1 < n_groups else [None]
            for j in range(NCH):
                rr([chunk_elem(j, gp)] + prev_out)
                prev_out = []
                gens = [unit_pre(j, h, h, gp) for h in range(4)]
                for pair in ((0, 1), (2, 3)):
                    for _ in range(2):
                        for u_ in pair:
                            next(gens[u_])
                rr2(gens, bg)
                rr2([unit_chain(Gi, j, h, h) for h in range(4)], bg)
                prev_out = [chunk_out(Gi, j)]
            if bg[0] is not None:
                rr([bg[0]])
        rr(prev_out)
        s.emit(nc, limit=limit, semstack=semstack, tag='B')


def consts_B():
    r = np.arange(128)[:, None]
    c = np.arange(128)[None, :]
    masks = np.stack([(r < c), (r <= c), (r > c)], axis=1).astype(np.float32)
    return dict(masks=np.ascontiguousarray(masks), identf=np.eye(128, dtype=np.float32), identb=np.eye(128).astype(bf))


def inputs_B(inp, b, g):
    cs = slice(g * 256, (g + 1) * 256)
    w_in = inp["rwkv_w_in"]
    w4 = np.concatenate([w_in[n][:, cs] for n in range(4)], axis=1)
    mixT = np.ascontiguousarray(inp["rwkv_mix"].reshape(6, 8, 128).transpose(2, 1, 0))
    hd = lambda p: np.asarray(p).reshape(-1)[cs].reshape(4, 64).T
    cpar = np.stack([hd(inp["rwkv_w0"]), hd(inp["rwkv_a0"]), hd(inp["rwkv_k_k"]), hd(inp["rwkv_k_a"]), hd(inp["rwkv_r_k"])], axis=1)
    lnx = np.stack([np.broadcast_to(inp["rwkv_lnx_g"][cs][None, :], (128, 256)),
                    np.broadcast_to(inp["rwkv_lnx_b"][cs][None, :], (128, 256))], axis=1)
    m = dict(wout0=np.ascontiguousarray(inp["moba_w_out"]),
             gbc=np.ascontiguousarray(np.broadcast_to(inp["rwkv_norm_g"][None, :], (128, 1024))),
             mixT=mixT, w4=np.ascontiguousarray(w4),
             wl=np.ascontiguousarray(np.concatenate([inp["rwkv_w1"], inp["rwkv_a1"]], axis=1)),
             w2a2=np.ascontiguousarray(np.stack([inp["rwkv_w2"][:, cs], inp["rwkv_a2"][:, cs]], axis=1)),
             cpar=np.ascontiguousarray(cpar.astype(np.float32)), lnx=np.ascontiguousarray(lnx.astype(np.float32)))
    m.update(consts_B())
    return m


D = 1024
NT = 64


def phase_C1(nc, semstack, ogall, og1all, ssbuf, x2dram):
    dram = lambda n, sh, dt, kind="ExternalInput": nc.dram_tensor("c_" + n, sh, dt, kind=kind).ap()
    xc = dram("xc", [S, 256], F32)
    w0c = dram("w0c", [D, 256], F32)
    w1c = dram("w1c", [D, 256], F32)
    identb = dram("identb", [128, 128], BF16)
    s = Sched()
    with ExitStack() as st:
        sb = lambda name, shape, dt: st.enter_context(nc.sbuf_tensor("C_" + name, shape, dt))
        xin = [sb("xin%d" % i, [128, 256], F32) for i in range(3)]
        o1in = [sb("o1in%d" % i, [128, D], F32) for i in range(3)]
        ogf = [sb("ogf%d" % i, [128, 8, 128], F32) for i in range(3)]
        ogin = [sb("ogin%d" % i, [128, 8, 128], BF16) for i in range(2)]
        o1b = sb("o1b", [128, D], BF16)
        o1T = [sb("o1T%d" % i, [128, 8, 128], BF16) for i in range(2)]
        x2t = [sb("x2t%d" % i, [128, 256], F32) for i in range(2)]
        junk = sb("junk", [128, 256], BF16)
        wo0 = sb("wo0", [128, 8, 256], BF16)
        wo1 = sb("wo1", [128, 8, 256], BF16)
        wst = sb("wst", [128, 256], F32)
        idb = sb("idb", [128, 128], BF16)
        ssq = sb("ssq", [128, NT], F32)
        psb = [st.enter_context(nc.psum_tensor("C_ps%d" % i, [128, 512], F32)) for i in range(4)]
        psP = psb[0:2]
        psT = psb[2]
        psT_bf = psT[:].bitcast(BF16)
        s.dma("sp", idb[:], identb[:], writes=["idb"])
        cstg = [(wst, "wst"), (xin[0], ("xin", 0)), (xin[1], ("xin", 1)), (xin[2], ("xin", 2))]
        for c in range(8):
            w_, k_ = cstg[c % 4]
            s.dma("sp", w_[:], w0c[c * 128:(c + 1) * 128, :], writes=[k_])
            s.copy("act", wo0[:, c, :], w_[:], reads=[k_], writes=["wo0"])
        for c in range(8):
            w_, k_ = cstg[c % 4]
            s.dma("sp", w_[:], w1c[c * 128:(c + 1) * 128, :], writes=[k_])
            s.copy("dve", wo1[:, c, :], w_[:], reads=[k_], writes=["wo1"])
        def c_stage1(ti):
            xs = ti % 3
            tok = ti * 128
            ck, c0 = tok // 1024, tok % 1024
            s.dma("sp", xin[xs][:], xc[tok:tok + 128, :], writes=[("xin", xs)])
            s.dma("sp", o1in[xs][:], og1all[ck, c0:c0 + 128, :], writes=[("o1in", xs)])
            s.dma("pool", ogf[xs][:], ogall[ck, :, c0:c0 + 128].rearrange("(c p) t -> p c t", p=128), writes=[("ogf", xs)])

        def c_stage2(ti):
            xs = ti % 3
            b2 = ti % 2
            s.copy("pool", ogin[b2][:], ogf[xs][:], reads=[("ogf", xs)], writes=[("ogin", b2)])
            s.copy("act", o1b[:], o1in[xs][:], reads=[("o1in", xs)], writes=["o1b"])
            for c in range(8):
                s.tr(psT_bf[:, c * 128:(c + 1) * 128], o1b[:, c * 128:(c + 1) * 128], idb[:], reads=["o1b", "idb"], writes=["psT"])
            s.copy("act", o1T[b2][:], psT_bf.rearrange("p (c t) -> p c t", c=8), reads=["psT"], writes=[("o1T", b2)])

        def c_stage3(ti):
            xs = ti % 3
            b2 = ti % 2
            tok = ti * 128
            ps = ti % 2
            for c in range(8):
                s.mm(psP[ps][:, 0:256], ogin[b2][:, c, :], wo0[:, c, :], start=(c == 0), stop=False,
                     reads=[("ogin", b2), "wo0"], writes=[("psP", ps)])
            for c in range(8):
                s.mm(psP[ps][:, 0:256], o1T[b2][:, c, :], wo1[:, c, :], start=False, stop=(c == 7),
                     reads=[("o1T", b2), "wo1"], writes=[("psP", ps)])
            s.tt("dve", x2t[b2][:], psP[ps][:, 0:256], xin[xs][:], ALU.add,
                 reads=[("psP", ps), ("xin", xs)], writes=[("x2t", b2)])
            s.op("dve", lambda e, b2=b2, ti=ti: e.scalar_tensor_tensor(
                out=junk[:], in0=x2t[b2][:], scalar=1.0, in1=x2t[b2][:], op0=ALU.mult, op1=ALU.mult, accum_out=ssq[:, ti:ti + 1]),
                reads=[("x2t", b2)], writes=["junk", "ssq"])
            s.dma("sp", x2dram[tok:tok + 128, :], x2t[b2][:], reads=[("x2t", b2)])

        for ti in range(NT + 2):
            if ti < NT:
                c_stage1(ti)
            if 1 <= ti <= NT:
                c_stage2(ti - 1)
            if ti >= 2:
                c_stage3(ti - 2)
        s.dma("sp", ssbuf[:, :], ssq[:], reads=["ssq"])
        s.emit(nc, semstack=semstack, tag='C')


def phase_C2(nc, semstack, ssall, x2dram):
    dram = lambda n, sh, dt, kind="ExternalInput": nc.dram_tensor("c_" + n, sh, dt, kind=kind).ap()
    gbc = dram("gbc", [128, 256], F32)
    out = dram("out", [S, 256], F32, kind="ExternalOutput")
    s = Sched()
    with ExitStack() as st:
        sb = lambda name, shape, dt: st.enter_context(nc.sbuf_tensor("E_" + name, shape, dt))
        x2t = [sb("x2t%d" % i, [128, 256], F32) for i in range(3)]
        res = [sb("res%d" % i, [128, 256], F32) for i in range(3)]
        gbc_sb = sb("gbc_sb", [128, 256], F32)
        ssq = sb("ssq", [128, NT], F32)
        rstd = sb("rstd", [128, NT], F32)
        eps_t = sb("eps_t", [128, 1], F32)
        s.dma("sp", gbc_sb[:], gbc[:], writes=["gbc"])
        s.dma("sp", ssq[:], ssall[:, :], writes=["ssq"])
        s.memset("pool", eps_t[:], 1e-6, writes=["eps"])
        s.act(rstd[:], ssq[:], AF.Ln, scale=1.0 / D, bias=eps_t[:, 0:1], reads=["ssq", "eps"], writes=["rstd"])
        s.act(rstd[:], rstd[:], AF.Exp, scale=-0.5, reads=["rstd"], writes=["rstd"])
        for ti in range(NT):
            k = ti % 3
            tok = ti * 128
            s.dma("sp", x2t[k][:], x2dram[tok:tok + 128, :], writes=[("x2t", k)])
            s.stt(res[k][:], x2t[k][:], rstd[:, ti:ti + 1], gbc_sb[:], ALU.mult, ALU.mult,
                  reads=[("x2t", k), "rstd", "gbc"], writes=[("res", k)])
            s.dma("pool", out[tok:tok + 128, :], res[k][:], reads=[("res", k)])
        s.emit(nc, semstack=semstack, tag='E')


GROUPS = [[0, 1, 2, 3], [4, 5, 6, 7]]


def allreduce_block(nc, semstack, pairs, tag):
    sem = semstack.enter_context(nc.semaphore(tag + "_cc"))
    with nc.Block() as block:
        @block.gpsimd
        def _(g):
            for i, (src, dst) in enumerate(pairs):
                g.collective_compute("AllReduce", ALU.add, replica_groups=GROUPS,
                                     ins=[src.opt()], outs=[dst.opt()]).then_inc(sem)
                g.wait_ge(sem, i + 1)


def build_fused():
    nc = bass.Bass("TRN2", target_bir_lowering=False)
    x = nc.dram_tensor("x", [S, D], F32, kind="ExternalInput").ap()
    ogbuf = nc.dram_tensor("ogbuf", [8, 1024, 1024], F32).ap()
    ogall = nc.dram_tensor("ogall", [8, 1024, 1024], F32).ap()
    og1buf = nc.dram_tensor("og1buf", [8, 1024, 1024], F32).ap()
    og1all = nc.dram_tensor("og1all", [8, 1024, 1024], F32).ap()
    ssbuf = nc.dram_tensor("ssbuf", [128, 64], F32).ap()
    ssall = nc.dram_tensor("ssall", [128, 64], F32).ap()
    x2dram = nc.dram_tensor("x2dram", [S, 256], F32).ap()
    with ExitStack() as semstack:
        phase_A(nc, semstack, x, ogbuf, ogall)
        phase_B(nc, semstack, x, ogall, og1buf, og1all)
        phase_C1(nc, semstack, ogall, og1all, ssbuf, x2dram)
        allreduce_block(nc, semstack, [(ssbuf, ssall)], "x3")
        phase_C2(nc, semstack, ssall, x2dram)
    return nc


def kernel(**inp):
    inp = {k: np.asarray(v) for k, v in inp.items()}
    x = inp["x"]
    w_in = inp["moba_w_in"]
    nc = build_fused()
    gbcA = np.ascontiguousarray(np.broadcast_to(inp["moba_norm_g"][None, :], (128, 1024)))
    identb = np.eye(128).astype(bf)
    maps = []
    for c in range(8):
        b, g = c // 4, c % 4
        cs = slice(g * 256, (g + 1) * 256)
        m = dict(x=np.ascontiguousarray(x[b]))
        w4 = np.concatenate([w_in[:, k * 1024 + g * 256: k * 1024 + (g + 1) * 256] for k in range(4)], axis=1)
        sel = np.zeros((128, 4), np.float32)
        sel[:, g] = 1.0
        am = dict(gbc=gbcA, w4=np.ascontiguousarray(w4), sel=sel)
        am.update(consts_A(g))
        for k, v in am.items():
            m["a_" + k] = v
        bm = inputs_B(inp, b, g)
        bm["sel"] = sel
        for k, v in bm.items():
            m["b_" + k] = v
        m["c_xc"] = np.ascontiguousarray(x[b][:, cs])
        m["c_w0c"] = np.ascontiguousarray(inp["moba_w_out"][:, cs])
        m["c_w1c"] = np.ascontiguousarray(inp["rwkv_w_out"][:, cs])
        m["c_identb"] = identb
        m["c_gbc"] = np.ascontiguousarray(np.broadcast_to(inp["final_norm_g"][cs][None, :], (128, 256)))
        maps.append(m)
    res = run_bass_kernel_spmd(nc, maps, core_ids=list(range(8))).results
    out = np.zeros((2, 8192, 1024), np.float32)
    for c in range(8):
        b, g = c // 4, c % 4
        out[b, :, g * 256:(g + 1) * 256] = np.asarray(res[c]["c_out"])
    return out
```

```python
import numpy as np
import ml_dtypes
from concourse.bass_utils import run_bass_kernel_spmd
import concourse.bass as bass
import concourse.mybir as mybir
from contextlib import ExitStack

F32 = mybir.dt.float32
BF16 = mybir.dt.bfloat16
AF = mybir.ActivationFunctionType
ALU = mybir.AluOpType
AX = mybir.AxisListType

ENGS = ("pe", "act", "dve", "pool", "sp")
DMA_POOLS = {"sp": (0, 8), "pool": (8, 6)}
N_DMA_SEMS = 14


class Op:
    __slots__ = ("eng", "fn", "deps", "signal", "sig_idx", "is_dma", "dma_sem", "dma_val", "idx", "cc")

    def __init__(self, eng, fn, is_dma):
        self.eng = eng
        self.fn = fn
        self.deps = []
        self.signal = False
        self.sig_idx = 0
        self.is_dma = is_dma
        self.dma_sem = -1
        self.dma_val = 0
        self.cc = False


class Sched:
    def __init__(self):
        self.ops = []
        self.last_w = {}
        self.readers = {}
        self.n_dma_q = {}
        self.n_cc = 0

    def op(self, eng, fn, reads=(), writes=(), dma=False):
        o = Op(eng, fn, dma)
        o.idx = len(self.ops)
        _isps = lambda b: (isinstance(b, str) and b.startswith("ps")) or (isinstance(b, tuple) and isinstance(b[0], str) and b[0].startswith("ps"))
        writes = list(writes) + [b for b in reads if _isps(b)]
        reads = [b for b in reads if not _isps(b)]
        deps = {}
        for b in reads:
            w = self.last_w.get(b)
            if w is not None:
                deps[w.idx] = w
        for b in writes:
            w = self.last_w.get(b)
            if w is not None:
                deps[w.idx] = w
            for r in self.readers.get(b, ()):
                deps[r.idx] = r
        best = {}
        for d in deps.values():
            if d is o:
                continue
            if d.is_dma:
                o.deps.append(d)
                continue
            if d.eng == eng and eng == "pe" and not dma:
                continue
            if d.eng not in best or best[d.eng].idx < d.idx:
                best[d.eng] = d
        for d in best.values():
            o.deps.append(d)
            d.signal = True
        for b in reads:
            self.readers.setdefault(b, []).append(o)
        for b in writes:
            self.last_w[b] = o
            self.readers[b] = []
        if dma:
            base, n = DMA_POOLS[eng]
            k = self.n_dma_q.get(eng, 0)
            self.n_dma_q[eng] = k + 1
            o.dma_sem = base + k % n
            o.dma_val = 16 * (k // n + 1)
        self.ops.append(o)
        return o

    def cc(self, fn, reads=(), writes=()):
        o = self.op("pool", fn, reads, writes, dma=True)
        self.n_dma_q["pool"] -= 1
        o.cc = True
        o.dma_sem = N_DMA_SEMS + self.n_cc
        o.dma_val = 1
        self.n_cc += 1
        return o

    def mm(self, out, lhsT, rhs, start=True, stop=True, reads=(), writes=()):
        return self.op("pe", lambda e: e.matmul(out, lhsT=lhsT, rhs=rhs, start=start, stop=stop), reads, writes)

    def tr(self, out, in_, ident, reads=(), writes=()):
        return self.op("pe", lambda e: e.transpose(out, in_, ident), reads, writes)

    def act(self, out, in_, func, scale=None, bias=None, accum_out=None, reads=(), writes=()):
        kw = {}
        if scale is not None:
            kw["scale"] = scale
        if bias is not None:
            kw["bias"] = bias
        if accum_out is not None:
            kw["accum_out"] = accum_out
        return self.op("act", lambda e: e.activation(out=out, in_=in_, func=func, **kw), reads, writes)

    def dma(self, q, out, in_, reads=(), writes=()):
        return self.op(q, lambda e: e.dma_start(out=out, in_=in_), reads, writes, dma=True)

    def copy(self, eng, out, in_, reads=(), writes=()):
        if eng == "act":
            return self.op("act", lambda e: e.activation(out=out, in_=in_, func=AF.Copy), reads, writes)
        return self.op(eng, lambda e: e.tensor_copy(out=out, in_=in_), reads, writes)

    def memset(self, eng, ap, val, writes=()):
        return self.op(eng, lambda e: e.memset(ap, val), (), writes)

    def tt(self, eng, out, in0, in1, op, reads=(), writes=()):
        return self.op(eng, lambda e: e.tensor_tensor(out=out, in0=in0, in1=in1, op=op), reads, writes)

    def ts(self, eng, out, in0, s1, s2, op0, op1=None, reads=(), writes=(), accum_out=None):
        kw = {}
        if op1 is not None:
            kw["op1"] = op1
        if accum_out is not None:
            kw["accum_out"] = accum_out
        return self.op(eng, lambda e: e.tensor_scalar(out=out, in0=in0, scalar1=s1, scalar2=s2, op0=op0, **kw), reads, writes)

    def stt(self, out, in0, scalar, in1, op0, op1, reads=(), writes=(), eng="dve"):
        return self.op(eng, lambda e: e.scalar_tensor_tensor(out=out, in0=in0, scalar=scalar, in1=in1, op0=op0, op1=op1), reads, writes)

    def emit(self, nc, final_waits=True, limit=None, semstack=None, tag=''):
        if limit is not None:
            self.ops = self.ops[:limit]
            for o in self.ops:
                o.signal = False
            for o in self.ops:
                for d in o.deps:
                    if not d.is_dma:
                        d.signal = True
        cnt = {e: 0 for e in ENGS}
        for o in self.ops:
            if o.is_dma:
                continue
            if o.signal:
                cnt[o.eng] += 1
                o.sig_idx = cnt[o.eng]
        per_eng = {e: [o for o in self.ops if o.eng == e] for e in ENGS}
        with ExitStack() as st:
            sst = semstack if semstack is not None else st
            sems = {e: sst.enter_context(nc.semaphore(tag + "s_" + e)) for e in ENGS}
            dsems = [sst.enter_context(nc.semaphore(tag + "d%d" % i)) for i in range(N_DMA_SEMS + self.n_cc)]
            block = st.enter_context(nc.Block())
            all_dma = [o for o in self.ops if o.is_dma]

            def body(ename):
                def f(engine):
                    waited = {}

                    def wait(sem_key, sem, val):
                        if waited.get(sem_key, 0) >= val:
                            return
                        waited[sem_key] = val
                        engine.wait_ge(sem, val)

                    for o in per_eng[ename]:
                        for d in o.deps:
                            if d.is_dma:
                                wait(("d", d.dma_sem), dsems[d.dma_sem], d.dma_val)
                            else:
                                wait(("e", d.eng), sems[d.eng], d.sig_idx)
                        if o.is_dma:
                            if o.dma_val > 16:
                                wait(("d", o.dma_sem), dsems[o.dma_sem], o.dma_val - 16)
                            ins = o.fn(engine)
                            if o.cc:
                                ins.then_inc(dsems[o.dma_sem])
                            else:
                                ins.then_inc(dsems[o.dma_sem], 16)
                        else:
                            ins = o.fn(engine)
                            if o.signal:
                                ins.then_inc(sems[ename], 1)
                    if final_waits and ename == "sp":
                        last = {}
                        for o in all_dma:
                            last[o.dma_sem] = max(last.get(o.dma_sem, 0), o.dma_val)
                        for s, v in last.items():
                            wait(("d", s), dsems[s], v)
                        for e in ENGS:
                            if cnt[e] > 0:
                                wait(("e", e), sems[e], cnt[e])
                return f

            block.tensor(body("pe"))
            block.scalar(body("act"))
            block.vector(body("dve"))
            block.gpsimd(body("pool"))
            block.sync(body("sp"))


S = 8192
D = 1024
NG_A = S // 512
bf = ml_dtypes.bfloat16
NEG = -1.0e30


def phase_A(nc, semstack, x, ogbuf, ogall, n_groups=NG_A, limit=None):
    dram = lambda n, sh, dt, kind="ExternalInput": nc.dram_tensor("a_" + n, sh, dt, kind=kind).ap()
    gbc = dram("gbc", [128, D], F32)
    w4 = dram("w4", [D, 1024], F32)
    qc = dram("qc", [4, 8, S], BF16)
    kc = dram("kc", [8, S], BF16)
    oh = dram("oh", [32, S], BF16)
    caus = dram("caus", [128, 2, 256], BF16)
    ident = dram("ident", [128, 128], BF16)
    sel = dram("sel", [128, 4], F32)
    s = Sched()
    with ExitStack() as st:
        sb = lambda name, shape, dt: st.enter_context(nc.sbuf_tensor("A_" + name, shape, dt))
        NX = 3
        xin = [sb("xin%d" % i, [128, D], F32) for i in range(NX)]
        gbc_sb = sb("gbc_sb", [128, D], F32)
        junk = sb("junk", [128, D], BF16)
        hn = [sb("hn%d" % i, [128, D], BF16) for i in range(2)]
        hT = sb("hT", [128, 8, 512], BF16)
        w_sb = sb("w_sb", [128, 8, 1024], BF16)
        kaug = sb("kaug", [104, 4, S], BF16)
        vaug = sb("vaug", [128, 64, 4, 65], BF16)
        qaug = [sb("qaug%d" % i, [104, 4, 512], BF16) for i in range(2)]
        sg = [sb("sg%d" % i, [64, 4, 512], BF16) for i in range(2)]
        gs = sb("gs", [128, 16, 32], F32)
        m8 = sb("m8", [128, 16, 8], F32)
        mk01 = sb("mk01", [128, 16, 32], F32)
        mkb = sb("mkb", [128, 16, 32], BF16)
        NP = 4
        pT = [sb("pT%d" % i, [128, 512], BF16) for i in range(NP)]
        kmean = sb("kmean", [64, 4, 32], BF16)
        ksum = sb("ksum", [64, 4, 2], F32)
        ss = sb("ss", [128, 4], F32)
        rstd = sb("rstd", [128, 4], F32)
        ones32 = sb("ones32", [128, 64], F32)
        rden = sb("rden", [128, 512], F32)
        t1 = [sb("t1_%d" % i, [64, 512], F32) for i in range(2)]
        og = [sb("og%d" % i, [64, 512], F32) for i in range(2)]
        og4 = [sb("og4_%d" % i, [64, 4, 512], F32) for i in range(2)]
        sel_sb = sb("sel_sb", [128, 4], F32)
        id_sb = sb("id_sb", [128, 128], BF16)
        caus_sb = sb("caus_sb", [128, 2, 256], BF16)
        psb = [st.enter_context(nc.psum_tensor("A_ps%d" % i, [128, 512], F32)) for i in range(8)]
        psS = psb[0:3]
        psO = psb[3:5]
        psP = psb[5:7]
        psM = psb[7]
        psM_bf = psM[:].bitcast(BF16)

        s.dma("sp", gbc_sb[:], gbc[:], writes=["gbc"])
        s.dma("sp", sel_sb[:], sel[:], writes=["sel"])
        s.dma("sp", id_sb[:], ident[:], writes=["ident"])
        s.dma("sp", caus_sb[:], caus[:], writes=["caus"])
        for c in range(8):
            slot = c % NX
            s.dma("sp", xin[slot][:], w4[c * 128:(c + 1) * 128, :], writes=[("xin", slot)])
            s.copy("pool", w_sb[:, c, :], xin[slot][:], reads=[("xin", slot)], writes=["w_sb"])
        for h in range(4):
            s.dma("pool", kaug[64:96, h, :], oh[:, :], writes=[("kaug_c", h, 0)])
            s.dma("pool", kaug[96:104, h, :], kc[:, :], writes=[("kaug_c", h, 1)])
        s.memset("pool", vaug[:, :, :, 64:65], 1.0, writes=["vaug_ones"])
        s.memset("pool", gs[:], NEG, writes=["gs"])
        s.memset("pool", ones32[:], 1.0, writes=["ones32"])
        for h in range(4):
            s.memset("pool", kmean[:, h, :], 0.0, writes=[("kmean", h)])

        xcount = [0]
        pcount = [0]
        scount = [0]
        ocount = [0]

        def stage_P(G):
            qs = G % 2
            t0 = G * 512
            s.dma("sp", qaug[qs][96:104, :, :], qc[:, :, t0:t0 + 512].rearrange("h r t -> r h t"),
                  writes=[("qaug_c", qs)])
            yield
            for tt in range(4):
                xs = xcount[0] % NX
                xcount[0] += 1
                hs = tt % 2
                tok = t0 + tt * 128
                s.dma("sp", xin[xs][:], x[tok:tok + 128, :], writes=[("xin", xs)])
                s.op("dve", lambda e, xs=xs, tt=tt: e.scalar_tensor_tensor(
                    out=junk[:], in0=xin[xs][:], scalar=1.0, in1=xin[xs][:],
                    op0=ALU.mult, op1=ALU.mult, accum_out=ss[:, tt:tt + 1]),
                    reads=[("xin", xs)], writes=["junk", ("ss", tt)])
                s.act(rstd[:, tt:tt + 1], ss[:, tt:tt + 1], AF.Ln, scale=1.0 / D, bias=eps_t[:, 0:1],
                      reads=[("ss", tt), "eps"], writes=[("rstd", tt)])
                s.act(rstd[:, tt:tt + 1], rstd[:, tt:tt + 1], AF.Exp, scale=-0.5,
                      reads=[("rstd", tt)], writes=[("rstd", tt)])
                s.stt(hn[hs][:], xin[xs][:], rstd[:, tt:tt + 1], gbc_sb[:], ALU.mult, ALU.mult,
                      reads=[("xin", xs), ("rstd", tt), "gbc"], writes=[("hn", hs)])
                yield
                for c in range(8):
                    s.tr(psM_bf[:, c * 128:(c + 1) * 128], hn[hs][:, c * 128:(c + 1) * 128], id_sb[:],
                         reads=[("hn", hs), "ident"], writes=["psM"])
                s.copy("dve", hT[:, :, tt * 128:(tt + 1) * 128],
                       psM_bf.rearrange("p (c t) -> p c t", c=8), reads=["psM"], writes=[("hT", tt)])
                yield
            hT_keys = [("hT", tt) for tt in range(4)]

            def proj(col0):
                ps = pcount[0] % 2
                pcount[0] += 1
                for c in range(8):
                    s.mm(psP[ps][:, :], w_sb[:, c, col0:col0 + 128], hT[:, c, :], start=(c == 0), stop=(c == 7),
                         reads=hT_keys + ["w_sb"], writes=[("psP", ps)])
                return ps

            for hp in range(2):
                ps = proj(hp * 128)
                for hh in range(2):
                    h = 2 * hp + hh
                    s.copy("dve", qaug[qs][0:64, h, :], psP[ps][hh * 64:(hh + 1) * 64, :], reads=[("psP", ps)],
                           writes=[("qaug_q", qs, h)])
                    yield
                ps = proj(256 + hp * 128)
                for hh in range(2):
                    h = 2 * hp + hh
                    for b2 in range(2):
                        s.act(kaug[0:64, h, t0 + b2 * 256:t0 + (b2 + 1) * 256],
                              psP[ps][hh * 64:(hh + 1) * 64, b2 * 256:(b2 + 1) * 256],
                              AF.Copy, accum_out=ksum[:, h, b2:b2 + 1], reads=[("psP", ps)],
                              writes=[("kaug_k", h, G), ("ksum", h)])
                    s.ts("dve", kmean[:, h, 2 * G:2 * G + 2], ksum[:, h, :], 1.0 / 256.0, None, ALU.mult, None,
                         reads=[("ksum", h)], writes=[("kmean", h)])
                    yield
            for hp in range(2):
                ps = proj(768 + hp * 128)
                for hh in range(2):
                    h = 2 * hp + hh
                    s.act(sg[qs][:, h, :], psP[ps][hh * 64:(hh + 1) * 64, :], AF.Silu, reads=[("psP", ps)],
                          writes=[("sg", qs, h)])
            yield
            for tt in range(4):
                ps = pcount[0] % 2
                pcount[0] += 1
                for c in range(8):
                    s.mm(psP[ps][:, 0:256], hT[:, c, tt * 128:(tt + 1) * 128], w_sb[:, c, 512:768],
                         start=(c == 0), stop=(c == 7), reads=[("hT", tt), "w_sb"], writes=[("psP", ps)])
                s.copy("dve", vaug[:, 4 * G + tt, :, 0:64], psP[ps][:, 0:256].rearrange("p (h d) -> p h d", h=4),
                       reads=[("psP", ps)], writes=[("vaug", 4 * G + tt)])
                yield
            psM3 = psM[:].rearrange("p (i n) -> p i n", n=32)
            for tt in range(4):
                for h in range(4):
                    s.mm(psM3[:, tt * 4 + h, :], qaug[qs][0:64, h, tt * 128:(tt + 1) * 128], kmean[:, h, :],
                         reads=[("qaug_q", qs, h), ("kmean", h)], writes=["psM"])
            own0, own1 = 2 * G, 2 * G + 1
            if own0 > 0:
                s.copy("dve", gs[:, 0:8, 0:own0], psM3[:, 0:8, 0:own0], reads=["psM"], writes=["gs"])
            s.copy("dve", gs[:, 8:16, 0:own1], psM3[:, 8:16, 0:own1], reads=["psM"], writes=["gs"])
            yield
            for i in range(16):
                s.op("dve", lambda e, i=i: e.max(out=m8[:, i, :], in_=gs[:, i, :]), reads=["gs"], writes=["m8"])
                if i % 4 == 3:
                    yield
            s.tt("dve", mk01[:], gs[:], m8[:, :, 2:3].to_broadcast([128, 16, 32]), ALU.is_ge,
                 reads=["gs", "m8"], writes=["mk01"])
            s.ts("dve", mkb[:], mk01[:], -1.0, 30000.0, ALU.add, ALU.mult, reads=["mk01"], writes=["mkb"])
            s.memset("dve", mkb[:, 0:8, own0:own0 + 1], 0.0, writes=["mkb"])
            s.memset("dve", mkb[:, 8:16, own1:own1 + 1], 0.0, writes=["mkb"])
            yield
            psM4 = psM_bf.rearrange("p (h t) -> p h t", h=2)
            for hp in range(2):
                for hh in range(2):
                    h = hp * 2 + hh
                    for tt in range(4):
                        s.tr(psM4[64:96, hh, tt * 128:(tt + 1) * 128], mkb[:, tt * 4 + h, :], id_sb[:],
                             reads=["mkb", "ident"], writes=["psM"])
                s.copy("dve", qaug[qs][64:96, hp * 2:hp * 2 + 2, :], psM4[64:96, :, :], reads=["psM"],
                       writes=[("qaug_m", qs, hp)])
                yield

        def stage_A(G, bg):
            qs = G % 2
            t0 = G * 512
            for h in range(4):
                os_ = ocount[0] % 2
                ocount[0] += 1
                chunks = []
                for c in range(4 * G):
                    chunks.append((c, 0, 512, None))
                for cc in range(2):
                    chunks.append((4 * G + cc, 0, 512, cc))
                for cc in range(2):
                    chunks.append((4 * G + 2 + cc, 256, 512, cc))
                qreads = [("qaug_q", qs, h), ("qaug_m", qs, h // 2), ("qaug_c", qs)]
                pend = []
                DLY = 2

                def emit_pv(item):
                    ci_, c_, c0_, c1_, pl_ = item
                    s.mm(psO[os_][0:65, c0_:c1_], vaug[:, c_, h, 0:65], pT[pl_][:, c0_:c1_],
                         start=(ci_ == 0), stop=(ci_ == len(chunks) - 1),
                         reads=[("pT", pl_), ("vaug", c_), "vaug_ones"], writes=[("psO", os_)])

                for ci, (c, c0, c1, diag) in enumerate(chunks):
                    sl = scount[0] % 3
                    pl = scount[0] % NP
                    scount[0] += 1
                    s.mm(psS[sl][:, c0:c1], kaug[0:104, h, c * 128:(c + 1) * 128], qaug[qs][0:104, h, c0:c1],
                         start=True, stop=(diag is None),
                         reads=qreads + [("kaug_k", h, c // 4), ("kaug_c", h, 0), ("kaug_c", h, 1)], writes=[("psS", sl)])
                    if diag is not None:
                        d0 = 0 if c0 == 0 else 256
                        s.mm(psS[sl][:, d0:d0 + 256], id_sb[:], caus_sb[:, diag, :], start=False, stop=True,
                             reads=["ident", "caus"], writes=[("psS", sl)])
                    s.act(pT[pl][:, c0:c1], psS[sl][:, c0:c1], AF.Exp, scale=0.125,
                          reads=[("psS", sl)], writes=[("pT", pl)])
                    pend.append((ci, c, c0, c1, pl))
                    if len(pend) > DLY:
                        emit_pv(pend.pop(0))
                    if bg[0] is not None:
                        try:
                            next(bg[0])
                        except StopIteration:
                            bg[0] = None
                while pend:
                    emit_pv(pend.pop(0))
                ts_ = os_
                s.op("dve", lambda e, os_=os_: e.reciprocal(out=rden[64:65, :], in_=psO[os_][64:65, :]),
                     reads=[("psO", os_)], writes=["rden"])
                s.mm(psM[0:64, :], ones32[64:65, 0:64], rden[64:65, :], reads=["ones32", "rden"], writes=["psM"])
                s.tt("dve", t1[ts_][:], psO[os_][0:64, :], sg[qs][:, h, :], ALU.mult,
                     reads=[("psO", os_), ("sg", qs, h)], writes=[("t1", ts_)])
                s.tt("dve", og[ts_][:], t1[ts_][:], psM[0:64, :], ALU.mult,
                     reads=[("t1", ts_), "psM"], writes=[("og", ts_)])
                for jj in range(4):
                    s.ts("pool", og4[ts_][:, jj, :], og[ts_][:], sel_sb[0:64, jj:jj + 1], 1.0, ALU.mult, ALU.mult,
                         reads=[("og", ts_), "sel"], writes=[("og4", ts_)])
                ck, c0 = t0 // 1024, t0 % 1024
                s.dma("pool", ogbuf[ck, :, c0:c0 + 512].rearrange("(j r) t -> r j t", j=4)[h * 64:(h + 1) * 64, :, :], og4[ts_][:],
                      reads=[("og4", ts_)], writes=[("ogbuf", ck)])

        eps_t = sb("eps_t", [128, 1], F32)
        s.memset("pool", eps_t[:], 1e-6, writes=["eps"])
        for _ in stage_P(0):
            pass
        for G in range(1, n_groups + 1):
            bg = [stage_P(G)] if G < n_groups else [None]
            stage_A(G - 1, bg)
            if bg[0] is not None:
                for _ in bg[0]:
                    pass
            if True:
                if (G - 1) % 2 == 1:
                    ck_ = (G - 1) // 2
                    s.cc(lambda e, ck_=ck_: e.collective_compute("AllReduce", ALU.add, replica_groups=GROUPS,
                                                                 ins=[ogbuf[ck_].opt()], outs=[ogall[ck_].opt()]),
                         reads=[("ogbuf", ck_)])
        s.emit(nc, limit=limit, semstack=semstack, tag='A')


def consts_A(g):
    pos = np.arange(S)
    qc = np.zeros((4, 8, S), dtype=bf)
    for hl in range(4):
        hg = 4 * g + hl
        slope = 2.0 ** (-8.0 * (hg + 1) / 16)
        s8 = np.float64(8.0 * slope)
        p1 = np.float64(bf(s8)); p2 = np.float64(bf(s8 - p1)); p3 = np.float64(bf(s8 - p1 - p2))
        s8p = p1 + p2 + p3
        T = -s8p * pos.astype(np.float64)
        Thi = T.astype(bf); Tlo = (T - Thi.astype(np.float64)).astype(bf)
        qc[hl, 0] = p1; qc[hl, 1] = p2; qc[hl, 2] = p3
        qc[hl, 3] = p1; qc[hl, 4] = p2; qc[hl, 5] = p3
        qc[hl, 6] = Thi; qc[hl, 7] = Tlo
    kc = np.zeros((8, S), dtype=bf)
    kc[0:3] = (pos % 256).astype(bf)
    kc[3:6] = (256 * (pos // 256)).astype(bf)
    kc[6:8] = 1.0
    oh = np.zeros((32, S), dtype=bf)
    oh[pos // 256, pos] = 1.0
    caus = np.zeros((128, 2, 256), dtype=bf)
    j = np.arange(128)[:, None]
    i = np.arange(256)[None, :]
    for c in range(2):
        caus[:, c, :] = np.where(c * 128 + j <= i, 0.0, -30000.0).astype(bf)
    ident = np.eye(128).astype(bf)
    return dict(qc=qc, kc=kc, oh=oh, caus=caus, ident=ident)


F32R = mybir.dt.float32r
S = 8192
D = 1024
bf = ml_dtypes.bfloat16
CDEC = 0.6065306597126334
L = 128


GT = 256
NCH = GT // L


def phase_B(nc, semstack, x, ogall, og1buf, og1all, n_groups=S // GT, limit=None):
    dram = lambda n, sh, dt, kind="ExternalInput": nc.dram_tensor("b_" + n, sh, dt, kind=kind).ap()
    wout0 = dram("wout0", [D, D], F32)
    gbc = dram("gbc", [128, D], F32)
    mixT = dram("mixT", [128, 8, 6], F32)
    w4 = dram("w4", [D, 1024], F32)
    wl = dram("wl", [D, 128], F32)
    w2a2 = dram("w2a2", [64, 2, 256], F32)
    cpar = dram("cpar", [64, 5, 4], F32)
    lnx = dram("lnx", [128, 2, 256], F32)
    masks = dram("masks", [128, 3, 128], F32)
    identf = dram("identf", [128, 128], F32)
    identb = dram("identb", [128, 128], BF16)
    sel = dram("sel", [128, 4], F32)
    s = Sched()
    with ExitStack() as st:
        sb = lambda name, shape, dt: st.enter_context(nc.sbuf_tensor("B_" + name, shape, dt))
        NX = 2
        xin = [sb("xin%d" % i, [128, D], F32) for i in range(NX)]
        ogin = [sb("ogin%d" % i, [128, 8, 128], BF16) for i in range(1)] * 2
        ogf = [sb("ogf%d" % i, [128, 8, 128], F32) for i in range(1)] * 2
        sel_sb = sb("sel_sb", [128, 4], F32)
        ogo4 = [sb("ogo4_%d" % i, [128, 4, 256], F32) for i in range(1)] * 2
        x1t = sb("x1t", [128, D], F32)
        hn = sb("hn", [128, D], BF16)
        junk = hn
        hx = sb("hx", [128, 8, GT + 4], BF16)
        lastcol = sb("lastcol", [128, 8, 2], BF16)
        gbc_sb = sb("gbc_sb", [128, D], F32)
        wo_sb = sb("wo_sb", [128, 8, 1024], BF16)
        wst = x1t
        wa_sb = sb("wa_sb", [128, 8, 1024], BF16)
        wb_sb = sb("wb_sb", [128, 8, 1024], BF16)
        wla_sb = sb("wla_sb", [128, 8, 128], BF16)
        wlb_sb = sb("wlb_sb", [128, 8, 128], BF16)
        mix_sb = sb("mix_sb", [128, 8, 6], F32)
        onem_sb = sb("onem_sb", [128, 8, 6], F32)
        w2a2_sb = sb("w2a2_sb", [64, 2, 256], BF16)
        w2st = sb("w2st", [64, 2, 256], F32)
        cp = sb("cp", [64, 5, 4], F32)
        onemka = sb("onemka", [64, 4], F32)
        rk2 = sb("rk2", [64, 4, 2], F32R)
        lnx_sb = sb("lnx_sb", [128, 2, 256], F32)
        mk = sb("mk", [128, 3, 128], F32)
        idf = sb("idf", [128, 128], F32)
        idb = sb("idb", [128, 128], BF16)
        ones_r = sb("ones_r", [64, 64], F32R)
        ones_f = sb("ones_f", [64, 128], F32)
        eps_t = sb("eps_t", [128, 3], F32)
        ss = sb("ss", [128, 2], F32)
        rstd = sb("rstd", [128, 2], F32)
        Gt = lambda name, dt=F32: sb(name, [64, 4, GT], dt)
        r_c2 = [Gt("r_c%d" % i) for i in range(2)]; k_c2 = [Gt("k_c%d" % i) for i in range(2)]
        sgw2 = [Gt("sgw%d" % i) for i in range(2)]; a_c2 = [Gt("a_c%d" % i) for i in range(2)]
        lhid2 = [sb("lhid%d" % i, [64, 2, GT], BF16) for i in range(2)]
        v_r2 = [sb("v_r%d" % i, [128, NCH, 256], F32R) for i in range(2)]
        sg_t2 = [sb("sg_t%d" % i, [128, NCH, 256], F32) for i in range(2)]
        Ct = lambda name, dt=F32: sb(name, [64, 4, L], dt)
        kk = Ct("kk"); tmp1 = Ct("tmp1"); tmp2 = Ct("tmp2", F32R); cum = Ct("cum"); ex = Ct("ex")
        g_incl = Ct("g_incl"); g_inv = Ct("g_inv"); g_excl = Ct("g_excl"); g_rem = Ct("g_rem")
        b_c = Ct("b_c"); km = Ct("km")
        AR = sb("AR", [64, 4, 2, L], F32R)
        bh = Ct("bh", F32R); kh = Ct("kh", F32R); rkp = Ct("rkp", F32R)
        Bt = Ct("Bt"); Kt = Ct("Kt")
        gLt = sb("gLt", [64, 4, 2], F32)
        NU = 4
        A1sb = [sb("A1sb%d" % i, [128, 256], F32R) for i in range(NU)]
        A2sb = [sb("A2sb%d" % i, [128, 256], F32R) for i in range(NU)]
        QMP = [[sb("QMP%d_%d" % (i, j), [128, 384], F32R) for j in range(2)] for i in range(NU)]
        tok3 = [sb("tok3_%d" % i, [128, 3, 64], F32R) for i in range(NU)]
        G1sb = [sb("G1sb%d" % i, [64, 128], F32R) for i in range(NU)]
        AVsb = [sb("AVsb%d" % i, [128, 64], F32R) for i in range(NU)]
        P2sb = [sb("P2sb%d" % i, [128, 64], F32) for i in range(NU)]
        Usb = [sb("Usb%d" % i, [128, 64], F32R) for i in range(NU)]
        Ssb = [[sb("Ssb%d_%d" % (h, j), [64, 64], F32R) for j in range(2)] for h in range(4)]
        zer = sb("zer", [64, 64], F32)
        ysb = sb("ysb", [128, 4, 64], F32)
        bst = sb("bst", [128, 4, 6], F32)
        mv = sb("mv", [128, 4, 2], F32)
        rs4 = sb("rs4", [128, 4], F32)
        yn = sb("yn", [128, 256], F32)
        rks = sb("rks", [128, 4], F32)
        ogo = [sb("ogo%d" % i, [128, 256], F32) for i in range(1)] * 2
        psb = [st.enter_context(nc.psum_tensor("B_ps%d" % i, [128, 512], F32)) for i in range(8)]
        psP = psb[0:2]
        psT = psb[1]
        psT_bf = psT[:].bitcast(BF16)
        psN = psb[2:6]
        psAs = psb[6:8]

        s.dma("sp", gbc_sb[:], gbc[:], writes=["gbc"])
        s.dma("sp", sel_sb[:], sel[:], writes=["sel"])
        s.dma("sp", idb[:], identb[:], writes=["idb"])
        s.dma("sp", idf[:], identf[:], writes=["idf"])
        s.dma("sp", mk[:], masks[:], writes=["mk"])
        s.dma("sp", mix_sb[:], mixT[:], writes=["mix"])
        s.dma("sp", cp[:], cpar[:], writes=["cp"])
        s.dma("sp", lnx_sb[:], lnx[:], writes=["lnx"])
        s.dma("sp", w2st[:], w2a2[:], writes=["w2st"])
        s.copy("pool", w2a2_sb[:], w2st[:], reads=["w2st"], writes=["w2a2"])
        s.ts("dve", onem_sb[:], mix_sb[:], -1.0, 1.0, ALU.mult, ALU.add, reads=["mix"], writes=["onem"])
        s.ts("dve", onemka[:], cp[:, 3, :], -1.0, 1.0, ALU.mult, ALU.add, reads=["cp"], writes=["onemka"])
        s.copy("dve", rk2[:], cp[:, 4, :].unsqueeze(2).to_broadcast([64, 4, 2]), reads=["cp"], writes=["rk2"])
        s.memset("pool", ones_f[:], 1.0, writes=["ones_f"])
        s.copy("dve", ones_r[:], ones_f[:, 0:64], reads=["ones_f"], writes=["ones_r"])
        s.memset("pool", eps_t[:, 0:1], 1e-6, writes=["eps"])
        s.memset("pool", eps_t[:, 1:2], 64e-5, writes=["eps"])
        s.memset("pool", eps_t[:, 2:3], 1e-30, writes=["eps"])
        s.memset("pool", hx[:], 0.0, writes=[("hx", 0), ("hx", 1), "hx0"])
        s.memset("pool", zer[:], 0.0, writes=["zer"])
        stg = [(x1t, "wst"), (xin[0], ("xin", 0)), (xin[1], ("xin", 1))]
        sk = [0]

        def nstg():
            t_, k_ = stg[sk[0] % 3]
            sk[0] += 1
            return t_, k_

        for c in range(8):
            w_, k_ = nstg()
            s.dma("sp", w_[:], wout0[c * 128:(c + 1) * 128, :], writes=[k_])
            s.copy("pool", wo_sb[:, c, :], w_[:], reads=[k_], writes=["wo_sb"])
        for c in range(8):
            w_, k_ = nstg()
            s.dma("sp", w_[:], w4[c * 128:(c + 1) * 128, :], writes=[k_])
            for n in range(4):
                s.ts("dve", wa_sb[:, c, n * 256:(n + 1) * 256], w_[:, n * 256:(n + 1) * 256], onem_sb[:, c, n:n + 1], None,
                     ALU.mult, reads=[k_, "onem"], writes=["wa_sb"])
                s.ts("pool", wb_sb[:, c, n * 256:(n + 1) * 256], w_[:, n * 256:(n + 1) * 256], mix_sb[:, c, n:n + 1], 1.0,
                     ALU.mult, ALU.mult, reads=[k_, "mix"], writes=["wb_sb"])
        for c in range(8):
            w_, k_ = nstg()
            s.dma("sp", w_[:, 0:128], wl[c * 128:(c + 1) * 128, :], writes=[k_])
            for n in range(2):
                s.ts("dve", wla_sb[:, c, n * 64:(n + 1) * 64], w_[:, n * 64:(n + 1) * 64], onem_sb[:, c, 4 + n:5 + n], None,
                     ALU.mult, reads=[k_, "onem"], writes=["wla_sb"])
                s.ts("pool", wlb_sb[:, c, n * 64:(n + 1) * 64], w_[:, n * 64:(n + 1) * 64], mix_sb[:, c, 4 + n:5 + n], 1.0,
                     ALU.mult, ALU.mult, reads=[k_, "mix"], writes=["wlb_sb"])
        for h in range(4):
            s.copy("dve", Ssb[h][0][:], zer[:], reads=["zer"], writes=[("S", h, 0)])

        cnt = {"x": 0, "p": 0, "u": 0, "o": 0}
        allh = lambda n, gp=None: [((n, h) if gp is None else (n, h, gp)) for h in range(4)]

        def nextp():
            p = cnt["p"] % 2
            cnt["p"] += 1
            return p

        def stage_Pa(Gi):
            t0 = Gi * GT
            if Gi > 0:
                s.copy("pool", hx[:, :, 3:4], lastcol[:, :, 0:1], reads=["lastcol"], writes=["hx0"])
            for tt in range(NCH):
                xs = cnt["x"] % NX
                cnt["x"] += 1
                tok = t0 + tt * 128
                s.dma("sp", xin[xs][:], x[tok:tok + 128, :], writes=[("xin", xs)])
                s.dma("pool", ogf[xs][:], ogall[tok // 1024, :, tok % 1024:tok % 1024 + 128].rearrange("(c p) t -> p c t", p=128), writes=["ogf"])
                s.copy("pool", ogin[xs][:], ogf[xs][:], reads=["ogf"], writes=["ogin"])
                yield
                for half in range(2):
                    ps = nextp()
                    for c in range(8):
                        s.mm(psP[ps][:, :], ogin[xs][:, c, :], wo_sb[:, c, half * 512:(half + 1) * 512], start=(c == 0), stop=(c == 7),
                             reads=["ogin", "wo_sb"], writes=[("psP", ps)])
                    yield
                    s.tt("dve", x1t[:, half * 512:(half + 1) * 512], psP[ps][:, :], xin[xs][:, half * 512:(half + 1) * 512], ALU.add,
                         reads=[("psP", ps), ("xin", xs)], writes=[("x1t", half), "wst"])
                    yield
                s.op("dve", lambda e, tt=tt: e.scalar_tensor_tensor(
                    out=junk[:], in0=x1t[:], scalar=1.0, in1=x1t[:],
                    op0=ALU.mult, op1=ALU.mult, accum_out=ss[:, tt:tt + 1]),
                    reads=[("x1t", 0), ("x1t", 1)], writes=["hn", ("ss", tt)])
                s.act(rstd[:, tt:tt + 1], ss[:, tt:tt + 1], AF.Ln, scale=1.0 / D, bias=eps_t[:, 0:1],
                      reads=[("ss", tt), "eps"], writes=[("rstd", tt)])
                s.act(rstd[:, tt:tt + 1], rstd[:, tt:tt + 1], AF.Exp, scale=-0.5,
                      reads=[("rstd", tt)], writes=[("rstd", tt)])
                s.stt(hn[:], x1t[:], rstd[:, tt:tt + 1], gbc_sb[:], ALU.mult, ALU.mult,
                      reads=[("x1t", 0), ("x1t", 1), ("rstd", tt), "gbc"], writes=["hn"])
                yield
                for c in range(8):
                    s.tr(psT_bf[:, c * 128:(c + 1) * 128], hn[:, c * 128:(c + 1) * 128], idb[:],
                         reads=["hn", "idb"], writes=[("psP", 1)])
                yield
                s.copy("act", hx[:, :, 4 + tt * 128:4 + (tt + 1) * 128],
                       psT_bf.rearrange("p (c t) -> p c t", c=8), reads=[("psP", 1)], writes=[("hx", tt)])
                yield
            s.copy("pool", lastcol[:, :, 0:1], hx[:, :, GT + 3:GT + 4], reads=[("hx", NCH - 1)], writes=["lastcol"])
            yield

        def stage_Pb(Gi):
            gp = Gi % 2
            r_c, k_c, sgw, a_c, lhid, v_r, sg_t = r_c2[gp], k_c2[gp], sgw2[gp], a_c2[gp], lhid2[gp], v_r2[gp], sg_t2[gp]
            hkeys = [("hx", tt) for tt in range(NCH)] + ["hx0"]

            def proj_cm(wa, wb, col0, ncols, ps_):
                out_ps = psP[ps_][0:ncols, 0:GT]
                for c in range(8):
                    s.mm(out_ps, wa[:, c, col0:col0 + ncols], hx[:, c, 4:GT + 4], start=(c == 0), stop=False,
                         reads=hkeys + ["wa_sb", "wla_sb"], writes=[("psP", ps_)])
                for c in range(8):
                    s.mm(out_ps, wb[:, c, col0:col0 + ncols], hx[:, c, 3:GT + 3], start=False, stop=(c == 7),
                         reads=hkeys + ["wb_sb", "wlb_sb"], writes=[("psP", ps_)])
                return out_ps

            ps_ = nextp()
            proj_cm(wla_sb, wlb_sb, 0, 128, ps_)
            s.act(lhid[:, 0, :], psP[ps_][0:64, 0:GT], AF.Tanh, reads=[("psP", ps_)], writes=[("lhid", 0, gp)])
            s.copy("act", lhid[:, 1, :], psP[ps_][64:128, 0:GT], reads=[("psP", ps_)], writes=[("lhid", 1, gp)])
            yield
            for h in range(4):
                ps_ = nextp()
                o_ = psP[ps_][0:64, 0:GT]
                s.mm(o_, w2a2_sb[:, 0, h * 64:(h + 1) * 64], lhid[:, 0, :], reads=["w2a2", ("lhid", 0, gp)], writes=[("psP", ps_)])
                s.act(sgw[:, h, :], o_, AF.Sigmoid, bias=cp[:, 0, h:h + 1], reads=[("psP", ps_), "cp"], writes=[("sgw", h, gp)])
                yield
                ps_ = nextp()
                o_ = psP[ps_][0:64, 0:GT]
                s.mm(o_, w2a2_sb[:, 1, h * 64:(h + 1) * 64], lhid[:, 1, :], reads=["w2a2", ("lhid", 1, gp)], writes=[("psP", ps_)])
                s.act(a_c[:, h, :], o_, AF.Sigmoid, bias=cp[:, 1, h:h + 1], reads=[("psP", ps_), "cp"], writes=[("a_c", h, gp)])
                yield
            for hp in range(2):
                ps_ = nextp()
                proj_cm(wa_sb, wb_sb, hp * 128, 128, ps_)
                s.copy("dve", r_c[:, 2 * hp, :], psP[ps_][0:64, 0:GT], reads=[("psP", ps_)], writes=[("r_c", 2 * hp, gp)])
                s.copy("dve", r_c[:, 2 * hp + 1, :], psP[ps_][64:128, 0:GT], reads=[("psP", ps_)], writes=[("r_c", 2 * hp + 1, gp)])
                yield
                ps_ = nextp()
                proj_cm(wa_sb, wb_sb, 256 + hp * 128, 128, ps_)
                s.copy("act", k_c[:, 2 * hp, :], psP[ps_][0:64, 0:GT], reads=[("psP", ps_)], writes=[("k_c", 2 * hp, gp)])
                s.copy("act", k_c[:, 2 * hp + 1, :], psP[ps_][64:128, 0:GT], reads=[("psP", ps_)], writes=[("k_c", 2 * hp + 1, gp)])
                yield
            for tt in range(NCH):
                ps_ = nextp()
                for c in range(8):
                    s.mm(psP[ps_][:, :], hx[:, c, 4 + tt * 128:4 + (tt + 1) * 128], wa_sb[:, c, 512:1024], start=(c == 0), stop=False,
                         reads=hkeys + ["wa_sb"], writes=[("psP", ps_)])
                for c in range(8):
                    s.mm(psP[ps_][:, :], hx[:, c, 3 + tt * 128:3 + (tt + 1) * 128], wb_sb[:, c, 512:1024], start=False, stop=(c == 7),
                         reads=hkeys + ["wb_sb"], writes=[("psP", ps_)])
                s.copy("dve", v_r[:, tt, :], psP[ps_][:, 0:256], reads=[("psP", ps_)], writes=[("v_r", tt, gp)])
                s.act(sg_t[:, tt, :], psP[ps_][:, 256:512], AF.Sigmoid, reads=[("psP", ps_)], writes=[("sg_t", tt, gp)])
                s.tt("dve", sg_t[:, tt, :], sg_t[:, tt, :], psP[ps_][:, 256:512], ALU.mult, reads=[("psP", ps_), ("sg_t", tt, gp)], writes=[("sg_t", tt, gp)])
                yield

        def chunk_elem(j, gp):
            r_c, k_c, sgw, a_c, lhid, v_r, sg_t = r_c2[gp], k_c2[gp], sgw2[gp], a_c2[gp], lhid2[gp], v_r2[gp], sg_t2[gp]
            cs = slice(j * L, (j + 1) * L)
            bc = lambda i: cp[:, i, :].unsqueeze(2).to_broadcast([64, 4, L])
            s.tt("dve", kk[:], k_c[:, :, cs], bc(2), ALU.mult, reads=allh("k_c", gp) + ["cp"], writes=["kk"])
            yield
            s.tt("dve", tmp2[:], kk[:], kk[:], ALU.mult, reads=["kk"], writes=["tmp2"])
            yield
            for h in range(4):
                ps_ = nextp()
                o_ = psP[ps_][0:64, 0:L]
                s.mm(o_, ones_r[:, :], tmp2[:, h, :], reads=["ones_r", "tmp2"], writes=[("psP", ps_)])
                yield
                s.act(tmp1[:, h, :], o_, AF.Ln, bias=eps_t[0:64, 2:3], reads=[("psP", ps_), "eps"], writes=["tmp1"])
                yield
            s.act(tmp1[:], tmp1[:], AF.Exp, scale=-0.5, reads=["tmp1"], writes=["tmp1"])
            yield
            s.tt("dve", kk[:], kk[:], tmp1[:], ALU.mult, reads=["kk", "tmp1"], writes=["kk"])
            yield
            for h in range(4):
                s.op("dve", lambda e, h=h: e.tensor_tensor_scan(
                    out=cum[:, h, :], data0=ones_f[:, 0:L], data1=sgw[:, h, cs],
                    initial=0.0, op0=ALU.mult, op1=ALU.add), reads=[("sgw", h, gp), "ones_f"], writes=["cum"])
                yield
            s.tt("pool", ex[:], cum[:], sgw[:, :, cs], ALU.subtract, reads=["cum"] + allh("sgw", gp), writes=["ex"])
            yield
            s.tt("dve", tmp1[:], cum[:], cum[:, :, L - 1:L].to_broadcast([64, 4, L]), ALU.subtract, reads=["cum", "tmp1"], writes=["tmp1"])
            yield
            s.act(g_rem[:], tmp1[:], AF.Exp, scale=CDEC, reads=["tmp1"], writes=["g_rem"])
            yield
            s.act(g_incl[:], cum[:], AF.Exp, scale=-CDEC, reads=["cum"], writes=["g_incl"])
            yield
            s.act(g_inv[:], cum[:], AF.Exp, scale=CDEC, reads=["cum"], writes=["g_inv"])
            yield
            s.act(g_excl[:], ex[:], AF.Exp, scale=-CDEC, reads=["ex"], writes=["g_excl"])
            yield
            s.copy("pool", gLt[:, :, 0:1], g_incl[:, :, L - 1:L], reads=["g_incl"], writes=["gLt"])
            yield
            s.stt(AR[:, :, 0, :], kk[:], -1.0, g_excl[:], ALU.mult, ALU.mult, reads=["kk", "g_excl"], writes=["AR_at"])
            yield
            s.tt("pool", b_c[:], kk[:], a_c[:, :, cs], ALU.mult, reads=["kk"] + allh("a_c", gp), writes=["b_c"])
            yield
            s.tt("dve", bh[:], b_c[:], g_inv[:], ALU.mult, reads=["b_c", "g_inv"], writes=["bh"])
            yield
            s.tt("pool", Bt[:], b_c[:], g_rem[:], ALU.mult, reads=["b_c", "g_rem"], writes=["Bt"])
            yield
            s.tt("dve", tmp1[:], a_c[:, :, cs], bc(3), ALU.mult, reads=allh("a_c", gp) + ["cp", "tmp1"], writes=["tmp1"])
            yield
            s.tt("pool", tmp1[:], tmp1[:], onemka[:].unsqueeze(2).to_broadcast([64, 4, L]), ALU.add, reads=["tmp1", "onemka"], writes=["tmp1"])
            yield
            s.tt("dve", km[:], k_c[:, :, cs], tmp1[:], ALU.mult, reads=allh("k_c", gp) + ["tmp1"], writes=["km"])
            yield
            s.tt("dve", kh[:], km[:], g_inv[:], ALU.mult, reads=["km", "g_inv"], writes=["kh"])
            yield
            s.tt("pool", Kt[:], km[:], g_rem[:], ALU.mult, reads=["km", "g_rem"], writes=["Kt"])
            yield
            s.tt("dve", AR[:, :, 1, :], r_c[:, :, cs], g_incl[:], ALU.mult, reads=allh("r_c", gp) + ["g_incl"], writes=["AR_rt"])
            yield
            s.tt("dve", rkp[:], r_c[:, :, cs], km[:], ALU.mult, reads=allh("r_c", gp) + ["km"], writes=["rkp"])
            yield

        def unit_pre(j, h, u, gp):
            r_c, k_c, sgw, a_c, lhid, v_r, sg_t = r_c2[gp], k_c2[gp], sgw2[gp], a_c2[gp], lhid2[gp], v_r2[gp], sg_t2[gp]
            AR2 = AR[:, h, :, :].rearrange("c a t -> c (a t)")
            pa = u % 2
            psA_ = psAs[pa]
            ka = ("psA", pa)
            kn = ("psN", u)
            s.mm(psA_[:, 0:256], bh[:, h, :], AR2, reads=["bh", "AR_at", "AR_rt"], writes=[ka])
            s.mm(psA_[:, 256:512], kh[:, h, :], AR2, reads=["kh", "AR_at", "AR_rt"], writes=[ka])
            s.mm(psN[u][:, 256:384], AR[:, h, 0, :], bh[:, h, :], reads=["bh", "AR_at"], writes=[kn])
            yield
            mUI = mk[:, 0:2, :].rearrange("p a t -> p (a t)")
            s.tt("dve", A1sb[u][:], psA_[:, 0:256], mUI, ALU.mult, reads=[ka, "mk"], writes=[("A1", u)])
            s.tt("dve", QMP[u][0][:, 0:128], psA_[:, 0:128], mk[:, 0, :], ALU.mult, reads=[ka, "mk"], writes=[("QMP", u, 0)])
            s.tt("dve", QMP[u][0][:, 256:384], psN[u][:, 256:384], mk[:, 2, :], ALU.mult, reads=[kn, "mk"], writes=[("QMP", u, 0)])
            s.tt("dve", A2sb[u][:], psA_[:, 256:512], mUI, ALU.mult, reads=[ka, "mk"], writes=[("A2", u)])
            s.tt("dve", QMP[u][0][:, 128:256], A1sb[u][:, 0:128].bitcast(F32), idf[:], ALU.add, reads=[("A1", u), "idf"], writes=[("QMP", u, 0)])
            s.tt("dve", QMP[u][1][:, 128:256], A1sb[u][:, 0:128].bitcast(F32), idf[:], ALU.add, reads=[("A1", u), "idf"], writes=[("QMP", u, 1)])
            yield
            cur = 0
            for it in range(7):
                nxt = 1 - cur
                last = (it == 6)
                T_ = QMP[u][cur]
                Qc = T_[:, 0:128]
                Pc = T_[:, 256:384]
                kc_ = ("QMP", u, cur)
                if it == 0:
                    s.mm(psN[u][:, 0:128], Pc, Qc, reads=[kc_], writes=[kn])
                elif not last:
                    s.mm(psN[u][:, 0:256], Pc, T_[:, 0:256], reads=[kc_], writes=[kn])
                else:
                    s.mm(psN[u][:, 128:256], Pc, T_[:, 128:256], reads=[kc_], writes=[kn])
                if not last:
                    s.mm(psN[u][:, 256:384], Qc, Pc, reads=[kc_], writes=[kn])
                yield
                if not last:
                    s.copy("act", QMP[u][nxt][:].rearrange("p (a t) -> p a t", a=3)[:, 0:3:2, :],
                           psN[u][:, 0:384].rearrange("p (a t) -> p a t", a=3)[:, 0:3:2, :], reads=[kn], writes=[("QMP", u, nxt)])
                if it >= 1:
                    s.tt("dve", QMP[u][nxt][:, 128:256], psN[u][:, 128:256], T_[:, 128:256].bitcast(F32), ALU.add,
                         reads=[kn, kc_], writes=[("QMP", u, nxt)])
                yield
                cur = nxt
            M = QMP[u][cur][:, 128:256]
            Mkey = ("QMP", u, cur)
            psX = psN[u]
            s.tr(psX[:, 0:64], AR[:, h, 0, :].bitcast(F32), idf[0:64, 0:64], reads=["AR_at", "idf"], writes=[kn])
            s.tr(psX[:, 64:128], Bt[:, h, :], idf[0:64, 0:64], reads=["Bt", "idf"], writes=[kn])
            s.tr(psX[:, 128:192], Kt[:, h, :], idf[0:64, 0:64], reads=["Kt", "idf"], writes=[kn])
            s.mm(psX[:, 320:384], A2sb[u][:, 0:128], v_r[:, j, h * 64:(h + 1) * 64], reads=[("A2", u), ("v_r", j, gp)], writes=[kn])
            yield
            s.copy("act", tok3[u][:].rearrange("p a k -> p (a k)"), psX[:, 0:192], reads=[kn], writes=[("tok3", u)])
            s.copy("act", AVsb[u][:], psX[:, 320:384], reads=[kn], writes=[("AV", u)])
            yield
            s.mm(psX[0:64, 192:320], tok3[u][:, 0, :], M, reads=[("tok3", u), Mkey], writes=[kn])
            s.mm(psX[:, 384:448], M, AVsb[u][:], reads=[Mkey, ("AV", u)], writes=[kn])
            yield
            s.copy("act", G1sb[u][:], psX[0:64, 192:320], reads=[kn], writes=[("G1", u)])
            s.copy("act", P2sb[u][:], psX[:, 384:448], reads=[kn], writes=[("P2", u)])
            yield

        def unit_chain(Gi, j, h, u):
            gp = Gi % 2
            r_c, k_c, sgw, a_c, lhid, v_r, sg_t = r_c2[gp], k_c2[gp], sgw2[gp], a_c2[gp], lhid2[gp], v_r2[gp], sg_t2[gp]
            cidx = Gi * NCH + j
            so = cidx % 2
            sn = 1 - so
            vv = v_r[:, j, h * 64:(h + 1) * 64]
            psC = psN[u]
            Ups = psC[:, 448:512]
            Sps = psC[0:64, 0:64]
            Yps = psC[:, 64:128]
            Rps = psC[:, 128:130]
            key = ("psN", u)
            s.mm(Ups, G1sb[u][:], Ssb[h][so][:], reads=[("G1", u), ("S", h, so)], writes=[key])
            yield
            s.tt("dve", Usb[u][:], Ups, P2sb[u][:], ALU.add, reads=[key, ("P2", u)], writes=[("U", u)])
            yield
            s.mm(Sps, tok3[u][:, 2, :], vv, start=True, stop=False, reads=[("tok3", u), ("v_r", j, gp)], writes=[key])
            s.mm(Sps, tok3[u][:, 1, :], Usb[u][:], start=False, stop=True, reads=[("tok3", u), ("U", u)], writes=[key])
            s.mm(Yps, AR[:, h, 1, :], Ssb[h][so][:], start=True, stop=False, reads=["AR_rt", ("S", h, so)], writes=[key])
            s.mm(Yps, A1sb[u][:, 128:256], Usb[u][:], start=False, stop=False, reads=[("A1", u), ("U", u)], writes=[key])
            s.mm(Yps, A2sb[u][:, 128:256], vv, start=False, stop=True, reads=[("A2", u), ("v_r", j, gp)], writes=[key])
            s.mm(Rps, rkp[:, h, :], rk2[:, h, :], reads=["rkp", "rk2"], writes=[key])
            yield
            s.stt(Ssb[h][sn][:], Ssb[h][so][:].bitcast(F32), gLt[:, h, 0:1], Sps, ALU.mult, ALU.add,
                  reads=[("S", h, so), "gLt", key], writes=[("S", h, sn)])
            s.copy("act", ysb[:, h, :], Yps, reads=[key], writes=[("ysb", h)])
            s.copy("act", rks[:, h:h + 1], Rps[:, 0:1], reads=[key], writes=[("rks", h)])
            yield
            s.op("dve", lambda e, h=h: e.bn_stats(out=bst[:, h, :], in_=ysb[:, h, :]), reads=[("ysb", h)], writes=[("bst", h)])
            s.op("dve", lambda e, h=h: e.bn_aggr(out=mv[:, h, :], in_=bst[:, h, :]), reads=[("bst", h)], writes=[("mv", h)])
            yield

        def rr(gens):
            gens = list(gens)
            while gens:
                for g_ in list(gens):
                    try:
                        next(g_)
                    except StopIteration:
                        gens.remove(g_)

        def chunk_out(Gi, j):
            gp = Gi % 2
            r_c, k_c, sgw, a_c, lhid, v_r, sg_t = r_c2[gp], k_c2[gp], sgw2[gp], a_c2[gp], lhid2[gp], v_r2[gp], sg_t2[gp]
            t0 = Gi * GT + j * L
            oo = cnt["o"] % 2
            cnt["o"] += 1
            s.act(rs4[:], mv[:, :, 1], AF.Ln, bias=eps_t[:, 1:2], reads=allh("mv") + ["eps"], writes=["rs4"])
            yield
            s.act(rs4[:], rs4[:], AF.Exp, scale=-0.5, reads=["rs4"], writes=["rs4"])
            yield
            for h in range(4):
                s.ts("dve", yn[:, h * 64:(h + 1) * 64], ysb[:, h, :], mv[:, h, 0:1], rs4[:, h:h + 1], ALU.subtract, ALU.mult,
                     reads=[("ysb", h), ("mv", h), "rs4"], writes=["yn"])
                yield
            s.tt("pool", yn[:], yn[:], lnx_sb[:, 0, :], ALU.mult, reads=["yn", "lnx"], writes=["yn"])
            yield
            s.tt("pool", yn[:], yn[:], lnx_sb[:, 1, :], ALU.add, reads=["yn", "lnx"], writes=["yn"])
            yield
            for h in range(4):
                s.stt(ogo[oo][:, h * 64:(h + 1) * 64], v_r[:, j, h * 64:(h + 1) * 64].bitcast(F32), rks[:, h:h + 1], yn[:, h * 64:(h + 1) * 64],
                      ALU.mult, ALU.add, reads=[("v_r", j, gp), ("rks", h), "yn"], writes=["ogo"])
                yield
            s.tt("dve", ogo[oo][:], ogo[oo][:], sg_t[:, j, :], ALU.mult, reads=["ogo", ("sg_t", j, gp)], writes=["ogo"])
            yield
            for jj in range(4):
                s.ts("pool", ogo4[oo][:, jj, :], ogo[oo][:], sel_sb[:, jj:jj + 1], 1.0, ALU.mult, ALU.mult,
                     reads=["ogo", "sel"], writes=["ogo4"])
                yield
            s.dma("sp", og1buf[t0 // 1024, t0 % 1024:t0 % 1024 + 128, :].rearrange("t (j f) -> t j f", j=4), ogo4[oo][:], reads=["ogo4"], writes=[("og1buf", t0 // 1024)])
            yield
            if (t0 + 128) % 1024 == 0:
                ck_ = t0 // 1024
                s.cc(lambda e, ck_=ck_: e.collective_compute("AllReduce", ALU.add, replica_groups=GROUPS,
                                                             ins=[og1buf[ck_].opt()], outs=[og1all[ck_].opt()]),
                     reads=[("og1buf", ck_)])
                yield

        def chain_gens(gs):
            for g_ in gs:
                yield from g_

        def rr2(gens, bg):
            gens = list(gens)
            while gens:
                for g_ in list(gens):
                    try:
                        next(g_)
                    except StopIteration:
                        gens.remove(g_)
                if bg[0] is not None:
                    try:
                        next(bg[0])
                    except StopIteration:
                        bg[0] = None

        rr([stage_Pa(0)])
        rr([stage_Pb(0)])
        prev_out = []
        for Gi in range(n_groups):
            gp = Gi % 2
            bg = [chain_gens([stage_Pa(Gi + 1), stage_Pb(Gi + 1)])] if Gi + 1 < n_groups else [None]
            for j in range(NCH):
                rr([chunk_elem(j, gp)] + prev_out)
                prev_out = []
                gens = [unit_pre(j, h, h, gp) for h in range(4)]
                for pair in ((0, 1), (2, 3)):
                    for _ in range(2):
                        for u_ in pair:
                            next(gens[u_])
                rr2(gens, bg)
                rr2([unit_chain(Gi, j, h, h) for h in range(4)], bg)
                prev_out = [chunk_out(Gi, j)]
            if bg[0] is not None:
                rr([bg[0]])
        rr(prev_out)
        s.emit(nc, limit=limit, semstack=semstack, tag='B')


def consts_B():
    r = np.arange(128)[:, None]
    c = np.arange(128)[None, :]
    masks = np.stack([(r < c), (r <= c), (r > c)], axis=1).astype(np.float32)
    return dict(masks=np.ascontiguousarray(masks), identf=np.eye(128, dtype=np.float32), identb=np.eye(128).astype(bf))


def inputs_B(inp, b, g):
    cs = slice(g * 256, (g + 1) * 256)
    w_in = inp["rwkv_w_in"]
    w4 = np.concatenate([w_in[n][:, cs] for n in range(4)], axis=1)
    mixT = np.ascontiguousarray(inp["rwkv_mix"].reshape(6, 8, 128).transpose(2, 1, 0))
    hd = lambda p: np.asarray(p).reshape(-1)[cs].reshape(4, 64).T
    cpar = np.stack([hd(inp["rwkv_w0"]), hd(inp["rwkv_a0"]), hd(inp["rwkv_k_k"]), hd(inp["rwkv_k_a"]), hd(inp["rwkv_r_k"])], axis=1)
    lnx = np.stack([np.broadcast_to(inp["rwkv_lnx_g"][cs][None, :], (128, 256)),
                    np.broadcast_to(inp["rwkv_lnx_b"][cs][None, :], (128, 256))], axis=1)
    m = dict(wout0=np.ascontiguousarray(inp["moba_w_out"]),
             gbc=np.ascontiguousarray(np.broadcast_to(inp["rwkv_norm_g"][None, :], (128, 1024))),
             mixT=mixT, w4=np.ascontiguousarray(w4),
             wl=np.ascontiguousarray(np.concatenate([inp["rwkv_w1"], inp["rwkv_a1"]], axis=1)),
             w2a2=np.ascontiguousarray(np.stack([inp["rwkv_w2"][:, cs], inp["rwkv_a2"][:, cs]], axis=1)),
             cpar=np.ascontiguousarray(cpar.astype(np.float32)), lnx=np.ascontiguousarray(lnx.astype(np.float32)))
    m.update(consts_B())
    return m


D = 1024
NT = 64


def phase_C1(nc, semstack, ogall, og1all, ssbuf, x2dram):
    dram = lambda n, sh, dt, kind="ExternalInput": nc.dram_tensor("c_" + n, sh, dt, kind=kind).ap()
    xc = dram("xc", [S, 256], F32)
    w0c = dram("w0c", [D, 256], F32)
    w1c = dram("w1c", [D, 256], F32)
    identb = dram("identb", [128, 128], BF16)
    s = Sched()
    with ExitStack() as st:
        sb = lambda name, shape, dt: st.enter_context(nc.sbuf_tensor("C_" + name, shape, dt))
        xin = [sb("xin%d" % i, [128, 256], F32) for i in range(3)]
        o1in = [sb("o1in%d" % i, [128, D], F32) for i in range(3)]
        ogf = [sb("ogf%d" % i, [128, 8, 128], F32) for i in range(3)]
        ogin = [sb("ogin%d" % i, [128, 8, 128], BF16) for i in range(2)]
        o1b = sb("o1b", [128, D], BF16)
        o1T = [sb("o1T%d" % i, [128, 8, 128], BF16) for i in range(2)]
        x2t = [sb("x2t%d" % i, [128, 256], F32) for i in range(2)]
        junk = sb("junk", [128, 256], BF16)
        wo0 = sb("wo0", [128, 8, 256], BF16)
        wo1 = sb("wo1", [128, 8, 256], BF16)
        wst = sb("wst", [128, 256], F32)
        idb = sb("idb", [128, 128], BF16)
        ssq = sb("ssq", [128, NT], F32)
        psb = [st.enter_context(nc.psum_tensor("C_ps%d" % i, [128, 512], F32)) for i in range(4)]
        psP = psb[0:2]
        psT = psb[2]
        psT_bf = psT[:].bitcast(BF16)
        s.dma("sp", idb[:], identb[:], writes=["idb"])
        cstg = [(wst, "wst"), (xin[0], ("xin", 0)), (xin[1], ("xin", 1)), (xin[2], ("xin", 2))]
        for c in range(8):
            w_, k_ = cstg[c % 4]
            s.dma("sp", w_[:], w0c[c * 128:(c + 1) * 128, :], writes=[k_])
            s.copy("act", wo0[:, c, :], w_[:], reads=[k_], writes=["wo0"])
        for c in range(8):
            w_, k_ = cstg[c % 4]
            s.dma("sp", w_[:], w1c[c * 128:(c + 1) * 128, :], writes=[k_])
            s.copy("dve", wo1[:, c, :], w_[:], reads=[k_], writes=["wo1"])
        def c_stage1(ti):
            xs = ti % 3
            tok = ti * 128
            ck, c0 = tok // 1024, tok % 1024
            s.dma("sp", xin[xs][:], xc[tok:tok + 128, :], writes=[("xin", xs)])
            s.dma("sp", o1in[xs][:], og1all[ck, c0:c0 + 128, :], writes=[("o1in", xs)])
            s.dma("pool", ogf[xs][:], ogall[ck, :, c0:c0 + 128].rearrange("(c p) t -> p c t", p=128), writes=[("ogf", xs)])

        def c_stage2(ti):
            xs = ti % 3
            b2 = ti % 2
            s.copy("pool", ogin[b2][:], ogf[xs][:], reads=[("ogf", xs)], writes=[("ogin", b2)])
            s.copy("act", o1b[:], o1in[xs][:], reads=[("o1in", xs)], writes=["o1b"])
            for c in range(8):
                s.tr(psT_bf[:, c * 128:(c + 1) * 128], o1b[:, c * 128:(c + 1) * 128], idb[:], reads=["o1b", "idb"], writes=["psT"])
            s.copy("act", o1T[b2][:], psT_bf.rearrange("p (c t) -> p c t", c=8), reads=["psT"], writes=[("o1T", b2)])

        def c_stage3(ti):
            xs = ti % 3
            b2 = ti % 2
            tok = ti * 128
            ps = ti % 2
            for c in range(8):
                s.mm(psP[ps][:, 0:256], ogin[b2][:, c, :], wo0[:, c, :], start=(c == 0), stop=False,
                     reads=[("ogin", b2), "wo0"], writes=[("psP", ps)])
            for c in range(8):
                s.mm(psP[ps][:, 0:256], o1T[b2][:, c, :], wo1[:, c, :], start=False, stop=(c == 7),
                     reads=[("o1T", b2), "wo1"], writes=[("psP", ps)])
            s.tt("dve", x2t[b2][:], psP[ps][:, 0:256], xin[xs][:], ALU.add,
                 reads=[("psP", ps), ("xin", xs)], writes=[("x2t", b2)])
            s.op("dve", lambda e, b2=b2, ti=ti: e.scalar_tensor_tensor(
                out=junk[:], in0=x2t[b2][:], scalar=1.0, in1=x2t[b2][:], op0=ALU.mult, op1=ALU.mult, accum_out=ssq[:, ti:ti + 1]),
                reads=[("x2t", b2)], writes=["junk", "ssq"])
            s.dma("sp", x2dram[tok:tok + 128, :], x2t[b2][:], reads=[("x2t", b2)])

        for ti in range(NT + 2):
            if ti < NT:
                c_stage1(ti)
            if 1 <= ti <= NT:
                c_stage2(ti - 1)
            if ti >= 2:
                c_stage3(ti - 2)
        s.dma("sp", ssbuf[:, :], ssq[:], reads=["ssq"])
        s.emit(nc, semstack=semstack, tag='C')


def phase_C2(nc, semstack, ssall, x2dram):
    dram = lambda n, sh, dt, kind="ExternalInput": nc.dram_tensor("c_" + n, sh, dt, kind=kind).ap()
    gbc = dram("gbc", [128, 256], F32)
    out = dram("out", [S, 256], F32, kind="ExternalOutput")
    s = Sched()
    with ExitStack() as st:
        sb = lambda name, shape, dt: st.enter_context(nc.sbuf_tensor("E_" + name, shape, dt))
        x2t = [sb("x2t%d" % i, [128, 256], F32) for i in range(3)]
        res = [sb("res%d" % i, [128, 256], F32) for i in range(3)]
        gbc_sb = sb("gbc_sb", [128, 256], F32)
        ssq = sb("ssq", [128, NT], F32)
        rstd = sb("rstd", [128, NT], F32)
        eps_t = sb("eps_t", [128, 1], F32)
        s.dma("sp", gbc_sb[:], gbc[:], writes=["gbc"])
        s.dma("sp", ssq[:], ssall[:, :], writes=["ssq"])
        s.memset("pool", eps_t[:], 1e-6, writes=["eps"])
        s.act(rstd[:], ssq[:], AF.Ln, scale=1.0 / D, bias=eps_t[:, 0:1], reads=["ssq", "eps"], writes=["rstd"])
        s.act(rstd[:], rstd[:], AF.Exp, scale=-0.5, reads=["rstd"], writes=["rstd"])
        for ti in range(NT):
            k = ti % 3
            tok = ti * 128
            s.dma("sp", x2t[k][:], x2dram[tok:tok + 128, :], writes=[("x2t", k)])
            s.stt(res[k][:], x2t[k][:], rstd[:, ti:ti + 1], gbc_sb[:], ALU.mult, ALU.mult,
                  reads=[("x2t", k), "rstd", "gbc"], writes=[("res", k)])
            s.dma("pool", out[tok:tok + 128, :], res[k][:], reads=[("res", k)])
        s.emit(nc, semstack=semstack, tag='E')


GROUPS = [[0, 1, 2, 3], [4, 5, 6, 7]]


def allreduce_block(nc, semstack, pairs, tag):
    sem = semstack.enter_context(nc.semaphore(tag + "_cc"))
    with nc.Block() as block:
        @block.gpsimd
        def _(g):
            for i, (src, dst) in enumerate(pairs):
                g.collective_compute("AllReduce", ALU.add, replica_groups=GROUPS,
                                     ins=[src.opt()], outs=[dst.opt()]).then_inc(sem)
                g.wait_ge(sem, i + 1)


def build_fused():
    nc = bass.Bass("TRN2", target_bir_lowering=False)
    x = nc.dram_tensor("x", [S, D], F32, kind="ExternalInput").ap()
    ogbuf = nc.dram_tensor("ogbuf", [8, 1024, 1024], F32).ap()
    ogall = nc.dram_tensor("ogall", [8, 1024, 1024], F32).ap()
    og1buf = nc.dram_tensor("og1buf", [8, 1024, 1024], F32).ap()
    og1all = nc.dram_tensor("og1all", [8, 1024, 1024], F32).ap()
    ssbuf = nc.dram_tensor("ssbuf", [128, 64], F32).ap()
    ssall = nc.dram_tensor("ssall", [128, 64], F32).ap()
    x2dram = nc.dram_tensor("x2dram", [S, 256], F32).ap()
    with ExitStack() as semstack:
        phase_A(nc, semstack, x, ogbuf, ogall)
        phase_B(nc, semstack, x, ogall, og1buf, og1all)
        phase_C1(nc, semstack, ogall, og1all, ssbuf, x2dram)
        allreduce_block(nc, semstack, [(ssbuf, ssall)], "x3")
        phase_C2(nc, semstack, ssall, x2dram)
    return nc


def kernel(**inp):
    inp = {k: np.asarray(v) for k, v in inp.items()}
    x = inp["x"]
    w_in = inp["moba_w_in"]
    nc = build_fused()
    gbcA = np.ascontiguousarray(np.broadcast_to(inp["moba_norm_g"][None, :], (128, 1024)))
    identb = np.eye(128).astype(bf)
    maps = []
    for c in range(8):
        b, g = c // 4, c % 4
        cs = slice(g * 256, (g + 1) * 256)
        m = dict(x=np.ascontiguousarray(x[b]))
        w4 = np.concatenate([w_in[:, k * 1024 + g * 256: k * 1024 + (g + 1) * 256] for k in range(4)], axis=1)
        sel = np.zeros((128, 4), np.float32)
        sel[:, g] = 1.0
        am = dict(gbc=gbcA, w4=np.ascontiguousarray(w4), sel=sel)
        am.update(consts_A(g))
        for k, v in am.items():
            m["a_" + k] = v
        bm = inputs_B(inp, b, g)
        bm["sel"] = sel
        for k, v in bm.items():
            m["b_" + k] = v
        m["c_xc"] = np.ascontiguousarray(x[b][:, cs])
        m["c_w0c"] = np.ascontiguousarray(inp["moba_w_out"][:, cs])
        m["c_w1c"] = np.ascontiguousarray(inp["rwkv_w_out"][:, cs])
        m["c_identb"] = identb
        m["c_gbc"] = np.ascontiguousarray(np.broadcast_to(inp["final_norm_g"][cs][None, :], (128, 256)))
        maps.append(m)
    res = run_bass_kernel_spmd(nc, maps, core_ids=list(range(8))).results
    out = np.zeros((2, 8192, 1024), np.float32)
    for c in range(8):
        b, g = c // 4, c % 4
        out[b, :, g * 256:(g + 1) * 256] = np.asarray(res[c]["c_out"])
    return out
```

```python
import numpy as np
import ml_dtypes
from concourse.bass_utils import run_bass_kernel_spmd
import concourse.bass as bass
import concourse.mybir as mybir
from contextlib import ExitStack

F32 = mybir.dt.float32
BF16 = mybir.dt.bfloat16
AF = mybir.ActivationFunctionType
ALU = mybir.AluOpType
AX = mybir.AxisListType

ENGS = ("pe", "act", "dve", "pool", "sp")
DMA_POOLS = {"sp": (0, 8), "pool": (8, 6)}
N_DMA_SEMS = 14


class Op:
    __slots__ = ("eng", "fn", "deps", "signal", "sig_idx", "is_dma", "dma_sem", "dma_val", "idx", "cc")

    def __init__(self, eng, fn, is_dma):
        self.eng = eng
        self.fn = fn
        self.deps = []
        self.signal = False
        self.sig_idx = 0
        self.is_dma = is_dma
        self.dma_sem = -1
        self.dma_val = 0
        self.cc = False


class Sched:
    def __init__(self):
        self.ops = []
        self.last_w = {}
        self.readers = {}
        self.n_dma_q = {}
        self.n_cc = 0

    def op(self, eng, fn, reads=(), writes=(), dma=False):
        o = Op(eng, fn, dma)
        o.idx = len(self.ops)
        _isps = lambda b: (isinstance(b, str) and b.startswith("ps")) or (isinstance(b, tuple) and isinstance(b[0], str) and b[0].startswith("ps"))
        writes = list(writes) + [b for b in reads if _isps(b)]
        reads = [b for b in reads if not _isps(b)]
        deps = {}
        for b in reads:
            w = self.last_w.get(b)
            if w is not None:
                deps[w.idx] = w
        for b in writes:
            w = self.last_w.get(b)
            if w is not None:
                deps[w.idx] = w
            for r in self.readers.get(b, ()):
                deps[r.idx] = r
        best = {}
        for d in deps.values():
            if d is o:
                continue
            if d.is_dma:
                o.deps.append(d)
                continue
            if d.eng == eng and eng == "pe" and not dma:
                continue
            if d.eng not in best or best[d.eng].idx < d.idx:
                best[d.eng] = d
        for d in best.values():
            o.deps.append(d)
            d.signal = True
        for b in reads:
            self.readers.setdefault(b, []).append(o)
        for b in writes:
            self.last_w[b] = o
            self.readers[b] = []
        if dma:
            base, n = DMA_POOLS[eng]
            k = self.n_dma_q.get(eng, 0)
            self.n_dma_q[eng] = k + 1
            o.dma_sem = base + k % n
            o.dma_val = 16 * (k // n + 1)
        self.ops.append(o)
        return o

    def cc(self, fn, reads=(), writes=()):
        o = self.op("pool", fn, reads, writes, dma=True)
        self.n_dma_q["pool"] -= 1
        o.cc = True
        o.dma_sem = N_DMA_SEMS + self.n_cc
        o.dma_val = 1
        self.n_cc += 1
        return o

    def mm(self, out, lhsT, rhs, start=True, stop=True, reads=(), writes=()):
        return self.op("pe", lambda e: e.matmul(out, lhsT=lhsT, rhs=rhs, start=start, stop=stop), reads, writes)

    def tr(self, out, in_, ident, reads=(), writes=()):
        return self.op("pe", lambda e: e.transpose(out, in_, ident), reads, writes)

    def act(self, out, in_, func, scale=None, bias=None, accum_out=None, reads=(), writes=()):
        kw = {}
        if scale is not None:
            kw["scale"] = scale
        if bias is not None:
            kw["bias"] = bias
        if accum_out is not None:
            kw["accum_out"] = accum_out
        return self.op("act", lambda e: e.activation(out=out, in_=in_, func=func, **kw), reads, writes)

    def dma(self, q, out, in_, reads=(), writes=()):
        return self.op(q, lambda e: e.dma_start(out=out, in_=in_), reads, writes, dma=True)

    def copy(self, eng, out, in_, reads=(), writes=()):
        if eng == "act":
            return self.op("act", lambda e: e.activation(out=out, in_=in_, func=AF.Copy), reads, writes)
        return self.op(eng, lambda e: e.tensor_copy(out=out, in_=in_), reads, writes)

    def memset(self, eng, ap, val, writes=()):
        return self.op(eng, lambda e: e.memset(ap, val), (), writes)

    def tt(self, eng, out, in0, in1, op, reads=(), writes=()):
        return self.op(eng, lambda e: e.tensor_tensor(out=out, in0=in0, in1=in1, op=op), reads, writes)

    def ts(self, eng, out, in0, s1, s2, op0, op1=None, reads=(), writes=(), accum_out=None):
        kw = {}
        if op1 is not None:
            kw["op1"] = op1
        if accum_out is not None:
            kw["accum_out"] = accum_out
        return self.op(eng, lambda e: e.tensor_scalar(out=out, in0=in0, scalar1=s1, scalar2=s2, op0=op0, **kw), reads, writes)

    def stt(self, out, in0, scalar, in1, op0, op1, reads=(), writes=(), eng="dve"):
        return self.op(eng, lambda e: e.scalar_tensor_tensor(out=out, in0=in0, scalar=scalar, in1=in1, op0=op0, op1=op1), reads, writes)

    def emit(self, nc, final_waits=True, limit=None, semstack=None, tag=''):
        if limit is not None:
            self.ops = self.ops[:limit]
            for o in self.ops:
                o.signal = False
            for o in self.ops:
                for d in o.deps:
                    if not d.is_dma:
                        d.signal = True
        cnt = {e: 0 for e in ENGS}
        for o in self.ops:
            if o.is_dma:
                continue
            if o.signal:
                cnt[o.eng] += 1
                o.sig_idx = cnt[o.eng]
        per_eng = {e: [o for o in self.ops if o.eng == e] for e in ENGS}
        with ExitStack() as st:
            sst = semstack if semstack is not None else st
            sems = {e: sst.enter_context(nc.semaphore(tag + "s_" + e)) for e in ENGS}
            dsems = [sst.enter_context(nc.semaphore(tag + "d%d" % i)) for i in range(N_DMA_SEMS + self.n_cc)]
            block = st.enter_context(nc.Block())
            all_dma = [o for o in self.ops if o.is_dma]

            def body(ename):
                def f(engine):
                    waited = {}

                    def wait(sem_key, sem, val):
                        if waited.get(sem_key, 0) >= val:
                            return
                        waited[sem_key] = val
                        engine.wait_ge(sem, val)

                    for o in per_eng[ename]:
                        for d in o.deps:
                            if d.is_dma:
                                wait(("d", d.dma_sem), dsems[d.dma_sem], d.dma_val)
                            else:
                                wait(("e", d.eng), sems[d.eng], d.sig_idx)
                        if o.is_dma:
                            if o.dma_val > 16:
                                wait(("d", o.dma_sem), dsems[o.dma_sem], o.dma_val - 16)
                            ins = o.fn(engine)
                            if o.cc:
                                ins.then_inc(dsems[o.dma_sem])
                            else:
                                ins.then_inc(dsems[o.dma_sem], 16)
                        else:
                            ins = o.fn(engine)
                            if o.signal:
                                ins.then_inc(sems[ename], 1)
                    if final_waits and ename == "sp":
                        last = {}
                        for o in all_dma:
                            last[o.dma_sem] = max(last.get(o.dma_sem, 0), o.dma_val)
                        for s, v in last.items():
                            wait(("d", s), dsems[s], v)
                        for e in ENGS:
                            if cnt[e] > 0:
                                wait(("e", e), sems[e], cnt[e])
                return f

            block.tensor(body("pe"))
            block.scalar(body("act"))
            block.vector(body("dve"))
            block.gpsimd(body("pool"))
            block.sync(body("sp"))


S = 8192
D = 1024
NG_A = S // 512
bf = ml_dtypes.bfloat16
NEG = -1.0e30


def phase_A(nc, semstack, x, ogbuf, ogall, n_groups=NG_A, limit=None):
    dram = lambda n, sh, dt, kind="ExternalInput": nc.dram_tensor("a_" + n, sh, dt, kind=kind).ap()
    gbc = dram("gbc", [128, D], F32)
    w4 = dram("w4", [D, 1024], F32)
    qc = dram("qc", [4, 8, S], BF16)
    kc = dram("kc", [8, S], BF16)
    oh = dram("oh", [32, S], BF16)
    caus = dram("caus", [128, 2, 256], BF16)
    ident = dram("ident", [128, 128], BF16)
    sel = dram("sel", [128, 4], F32)
    s = Sched()
    with ExitStack() as st:
        sb = lambda name, shape, dt: st.enter_context(nc.sbuf_tensor("A_" + name, shape, dt))
        NX = 3
        xin = [sb("xin%d" % i, [128, D], F32) for i in range(NX)]
        gbc_sb = sb("gbc_sb", [128, D], F32)
        junk = sb("junk", [128, D], BF16)
        hn = [sb("hn%d" % i, [128, D], BF16) for i in range(2)]
        hT = sb("hT", [128, 8, 512], BF16)
        w_sb = sb("w_sb", [128, 8, 1024], BF16)
        kaug = sb("kaug", [104, 4, S], BF16)
        vaug = sb("vaug", [128, 64, 4, 65], BF16)
        qaug = [sb("qaug%d" % i, [104, 4, 512], BF16) for i in range(2)]
        sg = [sb("sg%d" % i, [64, 4, 512], BF16) for i in range(2)]
        gs = sb("gs", [128, 16, 32], F32)
        m8 = sb("m8", [128, 16, 8], F32)
        mk01 = sb("mk01", [128, 16, 32], F32)
        mkb = sb("mkb", [128, 16, 32], BF16)
        NP = 4
        pT = [sb("pT%d" % i, [128, 512], BF16) for i in range(NP)]
        kmean = sb("kmean", [64, 4, 32], BF16)
        ksum = sb("ksum", [64, 4, 2], F32)
        ss = sb("ss", [128, 4], F32)
        rstd = sb("rstd", [128, 4], F32)
        ones32 = sb("ones32", [128, 64], F32)
        rden = sb("rden", [128, 512], F32)
        t1 = [sb("t1_%d" % i, [64, 512], F32) for i in range(2)]
        og = [sb("og%d" % i, [64, 512], F32) for i in range(2)]
        og4 = [sb("og4_%d" % i, [64, 4, 512], F32) for i in range(2)]
        sel_sb = sb("sel_sb", [128, 4], F32)
        id_sb = sb("id_sb", [128, 128], BF16)
        caus_sb = sb("caus_sb", [128, 2, 256], BF16)
        psb = [st.enter_context(nc.psum_tensor("A_ps%d" % i, [128, 512], F32)) for i in range(8)]
        psS = psb[0:3]
        psO = psb[3:5]
        psP = psb[5:7]
        psM = psb[7]
        psM_bf = psM[:].bitcast(BF16)

        s.dma("sp", gbc_sb[:], gbc[:], writes=["gbc"])
        s.dma("sp", sel_sb[:], sel[:], writes=["sel"])
        s.dma("sp", id_sb[:], ident[:], writes=["ident"])
        s.dma("sp", caus_sb[:], caus[:], writes=["caus"])
        for c in range(8):
            slot = c % NX
            s.dma("sp", xin[slot][:], w4[c * 128:(c + 1) * 128, :], writes=[("xin", slot)])
            s.copy("pool", w_sb[:, c, :], xin[slot][:], reads=[("xin", slot)], writes=["w_sb"])
        for h in range(4):
            s.dma("pool", kaug[64:96, h, :], oh[:, :], writes=[("kaug_c", h, 0)])
            s.dma("pool", kaug[96:104, h, :], kc[:, :], writes=[("kaug_c", h, 1)])
        s.memset("pool", vaug[:, :, :, 64:65], 1.0, writes=["vaug_ones"])
        s.memset("pool", gs[:], NEG, writes=["gs"])
        s.memset("pool", ones32[:], 1.0, writes=["ones32"])
        for h in range(4):
            s.memset("pool", kmean[:, h, :], 0.0, writes=[("kmean", h)])

        xcount = [0]
        pcount = [0]
        scount = [0]
        ocount = [0]

        def stage_P(G):
            qs = G % 2
            t0 = G * 512
            s.dma("sp", qaug[qs][96:104, :, :], qc[:, :, t0:t0 + 512].rearrange("h r t -> r h t"),
                  writes=[("qaug_c", qs)])
            yield
            for tt in range(4):
                xs = xcount[0] % NX
                xcount[0] += 1
                hs = tt % 2
                tok = t0 + tt * 128
                s.dma("sp", xin[xs][:], x[tok:tok + 128, :], writes=[("xin", xs)])
                s.op("dve", lambda e, xs=xs, tt=tt: e.scalar_tensor_tensor(
                    out=junk[:], in0=xin[xs][:], scalar=1.0, in1=xin[xs][:],
                    op0=ALU.mult, op1=ALU.mult, accum_out=ss[:, tt:tt + 1]),
                    reads=[("xin", xs)], writes=["junk", ("ss", tt)])
                s.act(rstd[:, tt:tt + 1], ss[:, tt:tt + 1], AF.Ln, scale=1.0 / D, bias=eps_t[:, 0:1],
                      reads=[("ss", tt), "eps"], writes=[("rstd", tt)])
                s.act(rstd[:, tt:tt + 1], rstd[:, tt:tt + 1], AF.Exp, scale=-0.5,
                      reads=[("rstd", tt)], writes=[("rstd", tt)])
                s.stt(hn[hs][:], xin[xs][:], rstd[:, tt:tt + 1], gbc_sb[:], ALU.mult, ALU.mult,
                      reads=[("xin", xs), ("rstd", tt), "gbc"], writes=[("hn", hs)])
                yield
                for c in range(8):
                    s.tr(psM_bf[:, c * 128:(c + 1) * 128], hn[hs][:, c * 128:(c + 1) * 128], id_sb[:],
                         reads=[("hn", hs), "ident"], writes=["psM"])
                s.copy("dve", hT[:, :, tt * 128:(tt + 1) * 128],
                       psM_bf.rearrange("p (c t) -> p c t", c=8), reads=["psM"], writes=[("hT", tt)])
                yield
            hT_keys = [("hT", tt) for tt in range(4)]

            def proj(col0):
                ps = pcount[0] % 2
                pcount[0] += 1
                for c in range(8):
                    s.mm(psP[ps][:, :], w_sb[:, c, col0:col0 + 128], hT[:, c, :], start=(c == 0), stop=(c == 7),
                         reads=hT_keys + ["w_sb"], writes=[("psP", ps)])
                return ps

            for hp in range(2):
                ps = proj(hp * 128)
                for hh in range(2):
                    h = 2 * hp + hh
                    s.copy("dve", qaug[qs][0:64, h, :], psP[ps][hh * 64:(hh + 1) * 64, :], reads=[("psP", ps)],
                           writes=[("qaug_q", qs, h)])
                    yield
                ps = proj(256 + hp * 128)
                for hh in range(2):
                    h = 2 * hp + hh
                    for b2 in range(2):
                        s.act(kaug[0:64, h, t0 + b2 * 256:t0 + (b2 + 1) * 256],
                              psP[ps][hh * 64:(hh + 1) * 64, b2 * 256:(b2 + 1) * 256],
                              AF.Copy, accum_out=ksum[:, h, b2:b2 + 1], reads=[("psP", ps)],
                              writes=[("kaug_k", h, G), ("ksum", h)])
                    s.ts("dve", kmean[:, h, 2 * G:2 * G + 2], ksum[:, h, :], 1.0 / 256.0, None, ALU.mult, None,
                         reads=[("ksum", h)], writes=[("kmean", h)])
                    yield
            for hp in range(2):
                ps = proj(768 + hp * 128)
                for hh in range(2):
                    h = 2 * hp + hh
                    s.act(sg[qs][:, h, :], psP[ps][hh * 64:(hh + 1) * 64, :], AF.Silu, reads=[("psP", ps)],
                          writes=[("sg", qs, h)])
            yield
            for tt in range(4):
                ps = pcount[0] % 2
                pcount[0] += 1
                for c in range(8):
                    s.mm(psP[ps][:, 0:256], hT[:, c, tt * 128:(tt + 1) * 128], w_sb[:, c, 512:768],
                         start=(c == 0), stop=(c == 7), reads=[("hT", tt), "w_sb"], writes=[("psP", ps)])
                s.copy("dve", vaug[:, 4 * G + tt, :, 0:64], psP[ps][:, 0:256].rearrange("p (h d) -> p h d", h=4),
                       reads=[("psP", ps)], writes=[("vaug", 4 * G + tt)])
                yield
            psM3 = psM[:].rearrange("p (i n) -> p i n", n=32)
            for tt in range(4):
                for h in range(4):
                    s.mm(psM3[:, tt * 4 + h, :], qaug[qs][0:64, h, tt * 128:(tt + 1) * 128], kmean[:, h, :],
                         reads=[("qaug_q", qs, h), ("kmean", h)], writes=["psM"])
            own0, own1 = 2 * G, 2 * G + 1
            if own0 > 0:
                s.copy("dve", gs[:, 0:8, 0:own0], psM3[:, 0:8, 0:own0], reads=["psM"], writes=["gs"])
            s.copy("dve", gs[:, 8:16, 0:own1], psM3[:, 8:16, 0:own1], reads=["psM"], writes=["gs"])
            yield
            for i in range(16):
                s.op("dve", lambda e, i=i: e.max(out=m8[:, i, :], in_=gs[:, i, :]), reads=["gs"], writes=["m8"])
                if i % 4 == 3:
                    yield
            s.tt("dve", mk01[:], gs[:], m8[:, :, 2:3].to_broadcast([128, 16, 32]), ALU.is_ge,
                 reads=["gs", "m8"], writes=["mk01"])
            s.ts("dve", mkb[:], mk01[:], -1.0, 30000.0, ALU.add, ALU.mult, reads=["mk01"], writes=["mkb"])
            s.memset("dve", mkb[:, 0:8, own0:own0 + 1], 0.0, writes=["mkb"])
            s.memset("dve", mkb[:, 8:16, own1:own1 + 1], 0.0, writes=["mkb"])
            yield
            psM4 = psM_bf.rearrange("p (h t) -> p h t", h=2)
            for hp in range(2):
                for hh in range(2):
                    h = hp * 2 + hh
                    for tt in range(4):
                        s.tr(psM4[64:96, hh, tt * 128:(tt + 1) * 128], mkb[:, tt * 4 + h, :], id_sb[:],
                             reads=["mkb", "ident"], writes=["psM"])
                s.copy("dve", qaug[qs][64:96, hp * 2:hp * 2 + 2, :], psM4[64:96, :, :], reads=["psM"],
                       writes=[("qaug_m", qs, hp)])
                yield

        def stage_A(G, bg):
            qs = G % 2
            t0 = G * 512
            for h in range(4):
                os_ = ocount[0] % 2
                ocount[0] += 1
                chunks = []
                for c in range(4 * G):
                    chunks.append((c, 0, 512, None))
                for cc in range(2):
                    chunks.append((4 * G + cc, 0, 512, cc))
                for cc in range(2):
                    chunks.append((4 * G + 2 + cc, 256, 512, cc))
                qreads = [("qaug_q", qs, h), ("qaug_m", qs, h // 2), ("qaug_c", qs)]
                pend = []
                DLY = 2

                def emit_pv(item):
                    ci_, c_, c0_, c1_, pl_ = item
                    s.mm(psO[os_][0:65, c0_:c1_], vaug[:, c_, h, 0:65], pT[pl_][:, c0_:c1_],
                         start=(ci_ == 0), stop=(ci_ == len(chunks) - 1),
                         reads=[("pT", pl_), ("vaug", c_), "vaug_ones"], writes=[("psO", os_)])

                for ci, (c, c0, c1, diag) in enumerate(chunks):
                    sl = scount[0] % 3
                    pl = scount[0] % NP
                    scount[0] += 1
                    s.mm(psS[sl][:, c0:c1], kaug[0:104, h, c * 128:(c + 1) * 128], qaug[qs][0:104, h, c0:c1],
                         start=True, stop=(diag is None),
                         reads=qreads + [("kaug_k", h, c // 4), ("kaug_c", h, 0), ("kaug_c", h, 1)], writes=[("psS", sl)])
                    if diag is not None:
                        d0 = 0 if c0 == 0 else 256
                        s.mm(psS[sl][:, d0:d0 + 256], id_sb[:], caus_sb[:, diag, :], start=False, stop=True,
                             reads=["ident", "caus"], writes=[("psS", sl)])
                    s.act(pT[pl][:, c0:c1], psS[sl][:, c0:c1], AF.Exp, scale=0.125,
                          reads=[("psS", sl)], writes=[("pT", pl)])
                    pend.append((ci, c, c0, c1, pl))
                    if len(pend) > DLY:
                        emit_pv(pend.pop(0))
                    if bg[0] is not None:
                        try:
                            next(bg[0])
                        except StopIteration:
                            bg[0] = None
                while pend:
                    emit_pv(pend.pop(0))
                ts_ = os_
                s.op("dve", lambda e, os_=os_: e.reciprocal(out=rden[64:65, :], in_=psO[os_][64:65, :]),
                     reads=[("psO", os_)], writes=["rden"])
                s.mm(psM[0:64, :], ones32[64:65, 0:64], rden[64:65, :], reads=["ones32", "rden"], writes=["psM"])
                s.tt("dve", t1[ts_][:], psO[os_][0:64, :], sg[qs][:, h, :], ALU.mult,
                     reads=[("psO", os_), ("sg", qs, h)], writes=[("t1", ts_)])
                s.tt("dve", og[ts_][:], t1[ts_][:], psM[0:64, :], ALU.mult,
                     reads=[("t1", ts_), "psM"], writes=[("og", ts_)])
                for jj in range(4):
                    s.ts("pool", og4[ts_][:, jj, :], og[ts_][:], sel_sb[0:64, jj:jj + 1], 1.0, ALU.mult, ALU.mult,
                         reads=[("og", ts_), "sel"], writes=[("og4", ts_)])
                ck, c0 = t0 // 1024, t0 % 1024
                s.dma("pool", ogbuf[ck, :, c0:c0 + 512].rearrange("(j r) t -> r j t", j=4)[h * 64:(h + 1) * 64, :, :], og4[ts_][:],
                      reads=[("og4", ts_)], writes=[("ogbuf", ck)])

        eps_t = sb("eps_t", [128, 1], F32)
        s.memset("pool", eps_t[:], 1e-6, writes=["eps"])
        for _ in stage_P(0):
            pass
        for G in range(1, n_groups + 1):
            bg = [stage_P(G)] if G < n_groups else [None]
            stage_A(G - 1, bg)
            if bg[0] is not None:
                for _ in bg[0]:
                    pass
            if True:
                if (G - 1) % 2 == 1:
                    ck_ = (G - 1) // 2
                    s.cc(lambda e, ck_=ck_: e.collective_compute("AllReduce", ALU.add, replica_groups=GROUPS,
                                                                 ins=[ogbuf[ck_].opt()], outs=[ogall[ck_].opt()]),
                         reads=[("ogbuf", ck_)])
        s.emit(nc, limit=limit, semstack=semstack, tag='A')


def consts_A(g):
    pos = np.arange(S)
    qc = np.zeros((4, 8, S), dtype=bf)
    for hl in range(4):
        hg = 4 * g + hl
        slope = 2.0 ** (-8.0 * (hg + 1) / 16)
        s8 = np.float64(8.0 * slope)
        p1 = np.float64(bf(s8)); p2 = np.float64(bf(s8 - p1)); p3 = np.float64(bf(s8 - p1 - p2))
        s8p = p1 + p2 + p3
        T = -s8p * pos.astype(np.float64)
        Thi = T.astype(bf); Tlo = (T - Thi.astype(np.float64)).astype(bf)
        qc[hl, 0] = p1; qc[hl, 1] = p2; qc[hl, 2] = p3
        qc[hl, 3] = p1; qc[hl, 4] = p2; qc[hl, 5] = p3
        qc[hl, 6] = Thi; qc[hl, 7] = Tlo
    kc = np.zeros((8, S), dtype=bf)
    kc[0:3] = (pos % 256).astype(bf)
    kc[3:6] = (256 * (pos // 256)).astype(bf)
    kc[6:8] = 1.0
    oh = np.zeros((32, S), dtype=bf)
    oh[pos // 256, pos] = 1.0
    caus = np.zeros((128, 2, 256), dtype=bf)
    j = np.arange(128)[:, None]
    i = np.arange(256)[None, :]
    for c in range(2):
        caus[:, c, :] = np.where(c * 128 + j <= i, 0.0, -30000.0).astype(bf)
    ident = np.eye(128).astype(bf)
    return dict(qc=qc, kc=kc, oh=oh, caus=caus, ident=ident)


F32R = mybir.dt.float32r
S = 8192
D = 1024
bf = ml_dtypes.bfloat16
CDEC = 0.6065306597126334
L = 128


GT = 256
NCH = GT // L


def phase_B(nc, semstack, x, ogall, og1buf, og1all, n_groups=S // GT, limit=None):
    dram = lambda n, sh, dt, kind="ExternalInput": nc.dram_tensor("b_" + n, sh, dt, kind=kind).ap()
    wout0 = dram("wout0", [D, D], F32)
    gbc = dram("gbc", [128, D], F32)
    mixT = dram("mixT", [128, 8, 6], F32)
    w4 = dram("w4", [D, 1024], F32)
    wl = dram("wl", [D, 128], F32)
    w2a2 = dram("w2a2", [64, 2, 256], F32)
    cpar = dram("cpar", [64, 5, 4], F32)
    lnx = dram("lnx", [128, 2, 256], F32)
    masks = dram("masks", [128, 3, 128], F32)
    identf = dram("identf", [128, 128], F32)
    identb = dram("identb", [128, 128], BF16)
    sel = dram("sel", [128, 4], F32)
    s = Sched()
    with ExitStack() as st:
        sb = lambda name, shape, dt: st.enter_context(nc.sbuf_tensor("B_" + name, shape, dt))
        NX = 2
        xin = [sb("xin%d" % i, [128, D], F32) for i in range(NX)]
        ogin = [sb("ogin%d" % i, [128, 8, 128], BF16) for i in range(1)] * 2
        ogf = [sb("ogf%d" % i, [128, 8, 128], F32) for i in range(1)] * 2
        sel_sb = sb("sel_sb", [128, 4], F32)
        ogo4 = [sb("ogo4_%d" % i, [128, 4, 256], F32) for i in range(1)] * 2
        x1t = sb("x1t", [128, D], F32)
        hn = sb("hn", [128, D], BF16)
        junk = hn
        hx = sb("hx", [128, 8, GT + 4], BF16)
        lastcol = sb("lastcol", [128, 8, 2], BF16)
        gbc_sb = sb("gbc_sb", [128, D], F32)
        wo_sb = sb("wo_sb", [128, 8, 1024], BF16)
        wst = x1t
        wa_sb = sb("wa_sb", [128, 8, 1024], BF16)
        wb_sb = sb("wb_sb", [128, 8, 1024], BF16)
        wla_sb = sb("wla_sb", [128, 8, 128], BF16)
        wlb_sb = sb("wlb_sb", [128, 8, 128], BF16)
        mix_sb = sb("mix_sb", [128, 8, 6], F32)
        onem_sb = sb("onem_sb", [128, 8, 6], F32)
        w2a2_sb = sb("w2a2_sb", [64, 2, 256], BF16)
        w2st = sb("w2st", [64, 2, 256], F32)
        cp = sb("cp", [64, 5, 4], F32)
        onemka = sb("onemka", [64, 4], F32)
        rk2 = sb("rk2", [64, 4, 2], F32R)
        lnx_sb = sb("lnx_sb", [128, 2, 256], F32)
        mk = sb("mk", [128, 3, 128], F32)
        idf = sb("idf", [128, 128], F32)
        idb = sb("idb", [128, 128], BF16)
        ones_r = sb("ones_r", [64, 64], F32R)
        ones_f = sb("ones_f", [64, 128], F32)
        eps_t = sb("eps_t", [128, 3], F32)
        ss = sb("ss", [128, 2], F32)
        rstd = sb("rstd", [128, 2], F32)
        Gt = lambda name, dt=F32: sb(name, [64, 4, GT], dt)
        r_c2 = [Gt("r_c%d" % i) for i in range(2)]; k_c2 = [Gt("k_c%d" % i) for i in range(2)]
        sgw2 = [Gt("sgw%d" % i) for i in range(2)]; a_c2 = [Gt("a_c%d" % i) for i in range(2)]
        lhid2 = [sb("lhid%d" % i, [64, 2, GT], BF16) for i in range(2)]
        v_r2 = [sb("v_r%d" % i, [128, NCH, 256], F32R) for i in range(2)]
        sg_t2 = [sb("sg_t%d" % i, [128, NCH, 256], F32) for i in range(2)]
        Ct = lambda name, dt=F32: sb(name, [64, 4, L], dt)
        kk = Ct("kk"); tmp1 = Ct("tmp1"); tmp2 = Ct("tmp2", F32R); cum = Ct("cum"); ex = Ct("ex")
        g_incl = Ct("g_incl"); g_inv = Ct("g_inv"); g_excl = Ct("g_excl"); g_rem = Ct("g_rem")
        b_c = Ct("b_c"); km = Ct("km")
        AR = sb("AR", [64, 4, 2, L], F32R)
        bh = Ct("bh", F32R); kh = Ct("kh", F32R); rkp = Ct("rkp", F32R)
        Bt = Ct("Bt"); Kt = Ct("Kt")
        gLt = sb("gLt", [64, 4, 2], F32)
        NU = 4
        A1sb = [sb("A1sb%d" % i, [128, 256], F32R) for i in range(NU)]
        A2sb = [sb("A2sb%d" % i, [128, 256], F32R) for i in range(NU)]
        QMP = [[sb("QMP%d_%d" % (i, j), [128, 384], F32R) for j in range(2)] for i in range(NU)]
        tok3 = [sb("tok3_%d" % i, [128, 3, 64], F32R) for i in range(NU)]
        G1sb = [sb("G1sb%d" % i, [64, 128], F32R) for i in range(NU)]
        AVsb = [sb("AVsb%d" % i, [128, 64], F32R) for i in range(NU)]
        P2sb = [sb("P2sb%d" % i, [128, 64], F32) for i in range(NU)]
        Usb = [sb("Usb%d" % i, [128, 64], F32R) for i in range(NU)]
        Ssb = [[sb("Ssb%d_%d" % (h, j), [64, 64], F32R) for j in range(2)] for h in range(4)]
        zer = sb("zer", [64, 64], F32)
        ysb = sb("ysb", [128, 4, 64], F32)
        bst = sb("bst", [128, 4, 6], F32)
        mv = sb("mv", [128, 4, 2], F32)
        rs4 = sb("rs4", [128, 4], F32)
        yn = sb("yn", [128, 256], F32)
        rks = sb("rks", [128, 4], F32)
        ogo = [sb("ogo%d" % i, [128, 256], F32) for i in range(1)] * 2
        psb = [st.enter_context(nc.psum_tensor("B_ps%d" % i, [128, 512], F32)) for i in range(8)]
        psP = psb[0:2]
        psT = psb[1]
        psT_bf = psT[:].bitcast(BF16)
        psN = psb[2:6]
        psAs = psb[6:8]

        s.dma("sp", gbc_sb[:], gbc[:], writes=["gbc"])
        s.dma("sp", sel_sb[:], sel[:], writes=["sel"])
        s.dma("sp", idb[:], identb[:], writes=["idb"])
        s.dma("sp", idf[:], identf[:], writes=["idf"])
        s.dma("sp", mk[:], masks[:], writes=["mk"])
        s.dma("sp", mix_sb[:], mixT[:], writes=["mix"])
        s.dma("sp", cp[:], cpar[:], writes=["cp"])
        s.dma("sp", lnx_sb[:], lnx[:], writes=["lnx"])
        s.dma("sp", w2st[:], w2a2[:], writes=["w2st"])
        s.copy("pool", w2a2_sb[:], w2st[:], reads=["w2st"], writes=["w2a2"])
        s.ts("dve", onem_sb[:], mix_sb[:], -1.0, 1.0, ALU.mult, ALU.add, reads=["mix"], writes=["onem"])
        s.ts("dve", onemka[:], cp[:, 3, :], -1.0, 1.0, ALU.mult, ALU.add, reads=["cp"], writes=["onemka"])
        s.copy("dve", rk2[:], cp[:, 4, :].unsqueeze(2).to_broadcast([64, 4, 2]), reads=["cp"], writes=["rk2"])
        s.memset("pool", ones_f[:], 1.0, writes=["ones_f"])
        s.copy("dve", ones_r[:], ones_f[:, 0:64], reads=["ones_f"], writes=["ones_r"])
        s.memset("pool", eps_t[:, 0:1], 1e-6, writes=["eps"])
        s.memset("pool", eps_t[:, 1:2], 64e-5, writes=["eps"])
        s.memset("pool", eps_t[:, 2:3], 1e-30, writes=["eps"])
        s.memset("pool", hx[:], 0.0, writes=[("hx", 0), ("hx", 1), "hx0"])
        s.memset("pool", zer[:], 0.0, writes=["zer"])
        stg = [(x1t, "wst"), (xin[0], ("xin", 0)), (xin[1], ("xin", 1))]
        sk = [0]

        def nstg():
            t_, k_ = stg[sk[0] % 3]
            sk[0] += 1
            return t_, k_

        for c in range(8):
            w_, k_ = nstg()
            s.dma("sp", w_[:], wout0[c * 128:(c + 1) * 128, :], writes=[k_])
            s.copy("pool", wo_sb[:, c, :], w_[:], reads=[k_], writes=["wo_sb"])
        for c in range(8):
            w_, k_ = nstg()
            s.dma("sp", w_[:], w4[c * 128:(c + 1) * 128, :], writes=[k_])
            for n in range(4):
                s.ts("dve", wa_sb[:, c, n * 256:(n + 1) * 256], w_[:, n * 256:(n + 1) * 256], onem_sb[:, c, n:n + 1], None,
                     ALU.mult, reads=[k_, "onem"], writes=["wa_sb"])
                s.ts("pool", wb_sb[:, c, n * 256:(n + 1) * 256], w_[:, n * 256:(n + 1) * 256], mix_sb[:, c, n:n + 1], 1.0,
                     ALU.mult, ALU.mult, reads=[k_, "mix"], writes=["wb_sb"])
        for c in range(8):
            w_, k_ = nstg()
            s.dma("sp", w_[:, 0:128], wl[c * 128:(c + 1) * 128, :], writes=[k_])
            for n in range(2):
                s.ts("dve", wla_sb[:, c, n * 64:(n + 1) * 64], w_[:, n * 64:(n + 1) * 64], onem_sb[:, c, 4 + n:5 + n], None,
                     ALU.mult, reads=[k_, "onem"], writes=["wla_sb"])
                s.ts("pool", wlb_sb[:, c, n * 64:(n + 1) * 64], w_[:, n * 64:(n + 1) * 64], mix_sb[:, c, 4 + n:5 + n], 1.0,
                     ALU.mult, ALU.mult, reads=[k_, "mix"], writes=["wlb_sb"])
        for h in range(4):
            s.copy("dve", Ssb[h][0][:], zer[:], reads=["zer"], writes=[("S", h, 0)])

        cnt = {"x": 0, "p": 0, "u": 0, "o": 0}
        allh = lambda n, gp=None: [((n, h) if gp is None else (n, h, gp)) for h in range(4)]

        def nextp():
            p = cnt["p"] % 2
            cnt["p"] += 1
            return p

        def stage_Pa(Gi):
            t0 = Gi * GT
            if Gi > 0:
                s.copy("pool", hx[:, :, 3:4], lastcol[:, :, 0:1], reads=["lastcol"], writes=["hx0"])
            for tt in range(NCH):
                xs = cnt["x"] % NX
                cnt["x"] += 1
                tok = t0 + tt * 128
                s.dma("sp", xin[xs][:], x[tok:tok + 128, :], writes=[("xin", xs)])
                s.dma("pool", ogf[xs][:], ogall[tok // 1024, :, tok % 1024:tok % 1024 + 128].rearrange("(c p) t -> p c t", p=128), writes=["ogf"])
                s.copy("act", ogin[xs][:], ogf[xs][:], reads=["ogf"], writes=["ogin"])
                yield
                for half in range(2):
                    ps = nextp()
                    for c in range(8):
                        s.mm(psP[ps][:, :], ogin[xs][:, c, :], wo_sb[:, c, half * 512:(half + 1) * 512], start=(c == 0), stop=(c == 7),
                             reads=["ogin", "wo_sb"], writes=[("psP", ps)])
                    yield
                    s.tt("dve", x1t[:, half * 512:(half + 1) * 512], psP[ps][:, :], xin[xs][:, half * 512:(half + 1) * 512], ALU.add,
                         reads=[("psP", ps), ("xin", xs)], writes=[("x1t", half), "wst"])
                    yield
                s.op("dve", lambda e, tt=tt: e.scalar_tensor_tensor(
                    out=junk[:], in0=x1t[:], scalar=1.0, in1=x1t[:],
                    op0=ALU.mult, op1=ALU.mult, accum_out=ss[:, tt:tt + 1]),
                    reads=[("x1t", 0), ("x1t", 1)], writes=["hn", ("ss", tt)])
                s.act(rstd[:, tt:tt + 1], ss[:, tt:tt + 1], AF.Ln, scale=1.0 / D, bias=eps_t[:, 0:1],
                      reads=[("ss", tt), "eps"], writes=[("rstd", tt)])
                s.act(rstd[:, tt:tt + 1], rstd[:, tt:tt + 1], AF.Exp, scale=-0.5,
                      reads=[("rstd", tt)], writes=[("rstd", tt)])
                s.stt(hn[:], x1t[:], rstd[:, tt:tt + 1], gbc_sb[:], ALU.mult, ALU.mult,
                      reads=[("x1t", 0), ("x1t", 1), ("rstd", tt), "gbc"], writes=["hn"])
                yield
                for c in range(8):
                    s.tr(psT_bf[:, c * 128:(c + 1) * 128], hn[:, c * 128:(c + 1) * 128], idb[:],
                         reads=["hn", "idb"], writes=[("psP", 1)])
                yield
                s.copy("act", hx[:, :, 4 + tt * 128:4 + (tt + 1) * 128],
                       psT_bf.rearrange("p (c t) -> p c t", c=8), reads=[("psP", 1)], writes=[("hx", tt)])
                yield
            s.copy("pool", lastcol[:, :, 0:1], hx[:, :, GT + 3:GT + 4], reads=[("hx", NCH - 1)], writes=["lastcol"])
            yield

        def stage_Pb(Gi):
            gp = Gi % 2
            r_c, k_c, sgw, a_c, lhid, v_r, sg_t = r_c2[gp], k_c2[gp], sgw2[gp], a_c2[gp], lhid2[gp], v_r2[gp], sg_t2[gp]
            hkeys = [("hx", tt) for tt in range(NCH)] + ["hx0"]

            def proj_cm(wa, wb, col0, ncols, ps_):
                out_ps = psP[ps_][0:ncols, 0:GT]
                for c in range(8):
                    s.mm(out_ps, wa[:, c, col0:col0 + ncols], hx[:, c, 4:GT + 4], start=(c == 0), stop=False,
                         reads=hkeys + ["wa_sb", "wla_sb"], writes=[("psP", ps_)])
                for c in range(8):
                    s.mm(out_ps, wb[:, c, col0:col0 + ncols], hx[:, c, 3:GT + 3], start=False, stop=(c == 7),
                         reads=hkeys + ["wb_sb", "wlb_sb"], writes=[("psP", ps_)])
                return out_ps

            ps_ = nextp()
            proj_cm(wla_sb, wlb_sb, 0, 128, ps_)
            s.act(lhid[:, 0, :], psP[ps_][0:64, 0:GT], AF.Tanh, reads=[("psP", ps_)], writes=[("lhid", 0, gp)])
            s.copy("act", lhid[:, 1, :], psP[ps_][64:128, 0:GT], reads=[("psP", ps_)], writes=[("lhid", 1, gp)])
            yield
            for h in range(4):
                ps_ = nextp()
                o_ = psP[ps_][0:64, 0:GT]
                s.mm(o_, w2a2_sb[:, 0, h * 64:(h + 1) * 64], lhid[:, 0, :], reads=["w2a2", ("lhid", 0, gp)], writes=[("psP", ps_)])
                s.act(sgw[:, h, :], o_, AF.Sigmoid, bias=cp[:, 0, h:h + 1], reads=[("psP", ps_), "cp"], writes=[("sgw", h, gp)])
                yield
                ps_ = nextp()
                o_ = psP[ps_][0:64, 0:GT]
                s.mm(o_, w2a2_sb[:, 1, h * 64:(h + 1) * 64], lhid[:, 1, :], reads=["w2a2", ("lhid", 1, gp)], writes=[("psP", ps_)])
                s.act(a_c[:, h, :], o_, AF.Sigmoid, bias=cp[:, 1, h:h + 1], reads=[("psP", ps_), "cp"], writes=[("a_c", h, gp)])
                yield
            for hp in range(2):
                ps_ = nextp()
                proj_cm(wa_sb, wb_sb, hp * 128, 128, ps_)
                s.copy("dve", r_c[:, 2 * hp, :], psP[ps_][0:64, 0:GT], reads=[("psP", ps_)], writes=[("r_c", 2 * hp, gp)])
                s.copy("dve", r_c[:, 2 * hp + 1, :], psP[ps_][64:128, 0:GT], reads=[("psP", ps_)], writes=[("r_c", 2 * hp + 1, gp)])
                yield
                ps_ = nextp()
                proj_cm(wa_sb, wb_sb, 256 + hp * 128, 128, ps_)
                s.copy("act", k_c[:, 2 * hp, :], psP[ps_][0:64, 0:GT], reads=[("psP", ps_)], writes=[("k_c", 2 * hp, gp)])
                s.copy("act", k_c[:, 2 * hp + 1, :], psP[ps_][64:128, 0:GT], reads=[("psP", ps_)], writes=[("k_c", 2 * hp + 1, gp)])
                yield
            for tt in range(NCH):
                ps_ = nextp()
                for c in range(8):
                    s.mm(psP[ps_][:, :], hx[:, c, 4 + tt * 128:4 + (tt + 1) * 128], wa_sb[:, c, 512:1024], start=(c == 0), stop=False,
                         reads=hkeys + ["wa_sb"], writes=[("psP", ps_)])
                for c in range(8):
                    s.mm(psP[ps_][:, :], hx[:, c, 3 + tt * 128:3 + (tt + 1) * 128], wb_sb[:, c, 512:1024], start=False, stop=(c == 7),
                         reads=hkeys + ["wb_sb"], writes=[("psP", ps_)])
                s.copy("dve", v_r[:, tt, :], psP[ps_][:, 0:256], reads=[("psP", ps_)], writes=[("v_r", tt, gp)])
                s.act(sg_t[:, tt, :], psP[ps_][:, 256:512], AF.Sigmoid, reads=[("psP", ps_)], writes=[("sg_t", tt, gp)])
                s.tt("dve", sg_t[:, tt, :], sg_t[:, tt, :], psP[ps_][:, 256:512], ALU.mult, reads=[("psP", ps_), ("sg_t", tt, gp)], writes=[("sg_t", tt, gp)])
                yield

        def chunk_elem(j, gp):
            r_c, k_c, sgw, a_c, lhid, v_r, sg_t = r_c2[gp], k_c2[gp], sgw2[gp], a_c2[gp], lhid2[gp], v_r2[gp], sg_t2[gp]
            cs = slice(j * L, (j + 1) * L)
            bc = lambda i: cp[:, i, :].unsqueeze(2).to_broadcast([64, 4, L])
            s.tt("dve", kk[:], k_c[:, :, cs], bc(2), ALU.mult, reads=allh("k_c", gp) + ["cp"], writes=["kk"])
            yield
            s.tt("dve", tmp2[:], kk[:], kk[:], ALU.mult, reads=["kk"], writes=["tmp2"])
            yield
            for h in range(4):
                ps_ = nextp()
                o_ = psP[ps_][0:64, 0:L]
                s.mm(o_, ones_r[:, :], tmp2[:, h, :], reads=["ones_r", "tmp2"], writes=[("psP", ps_)])
                yield
                s.act(tmp1[:, h, :], o_, AF.Ln, bias=eps_t[0:64, 2:3], reads=[("psP", ps_), "eps"], writes=["tmp1"])
                yield
            s.act(tmp1[:], tmp1[:], AF.Exp, scale=-0.5, reads=["tmp1"], writes=["tmp1"])
            yield
            s.tt("dve", kk[:], kk[:], tmp1[:], ALU.mult, reads=["kk", "tmp1"], writes=["kk"])
            yield
            for h in range(4):
                s.op("dve", lambda e, h=h: e.tensor_tensor_scan(
                    out=cum[:, h, :], data0=ones_f[:, 0:L], data1=sgw[:, h, cs],
                    initial=0.0, op0=ALU.mult, op1=ALU.add), reads=[("sgw", h, gp), "ones_f"], writes=["cum"])
                yield
            s.tt("pool", ex[:], cum[:], sgw[:, :, cs], ALU.subtract, reads=["cum"] + allh("sgw", gp), writes=["ex"])
            yield
            s.tt("dve", tmp1[:], cum[:], cum[:, :, L - 1:L].to_broadcast([64, 4, L]), ALU.subtract, reads=["cum", "tmp1"], writes=["tmp1"])
            yield
            s.act(g_rem[:], tmp1[:], AF.Exp, scale=CDEC, reads=["tmp1"], writes=["g_rem"])
            yield
            s.act(g_incl[:], cum[:], AF.Exp, scale=-CDEC, reads=["cum"], writes=["g_incl"])
            yield
            s.act(g_inv[:], cum[:], AF.Exp, scale=CDEC, reads=["cum"], writes=["g_inv"])
            yield
            s.act(g_excl[:], ex[:], AF.Exp, scale=-CDEC, reads=["ex"], writes=["g_excl"])
            yield
            s.copy("pool", gLt[:, :, 0:1], g_incl[:, :, L - 1:L], reads=["g_incl"], writes=["gLt"])
            yield
            s.stt(AR[:, :, 0, :], kk[:], -1.0, g_excl[:], ALU.mult, ALU.mult, reads=["kk", "g_excl"], writes=["AR_at"])
            yield
            s.tt("pool", b_c[:], kk[:], a_c[:, :, cs], ALU.mult, reads=["kk"] + allh("a_c", gp), writes=["b_c"])
            yield
            s.tt("dve", bh[:], b_c[:], g_inv[:], ALU.mult, reads=["b_c", "g_inv"], writes=["bh"])
            yield
            s.tt("pool", Bt[:], b_c[:], g_rem[:], ALU.mult, reads=["b_c", "g_rem"], writes=["Bt"])
            yield
            s.tt("dve", tmp1[:], a_c[:, :, cs], bc(3), ALU.mult, reads=allh("a_c", gp) + ["cp", "tmp1"], writes=["tmp1"])
            yield
            s.tt("pool", tmp1[:], tmp1[:], onemka[:].unsqueeze(2).to_broadcast([64, 4, L]), ALU.add, reads=["tmp1", "onemka"], writes=["tmp1"])
            yield
            s.tt("dve", km[:], k_c[:, :, cs], tmp1[:], ALU.mult, reads=allh("k_c", gp) + ["tmp1"], writes=["km"])
            yield
            s.tt("dve", kh[:], km[:], g_inv[:], ALU.mult, reads=["km", "g_inv"], writes=["kh"])
            yield
            s.tt("pool", Kt[:], km[:], g_rem[:], ALU.mult, reads=["km", "g_rem"], writes=["Kt"])
            yield
            s.tt("dve", AR[:, :, 1, :], r_c[:, :, cs], g_incl[:], ALU.mult, reads=allh("r_c", gp) + ["g_incl"], writes=["AR_rt"])
            yield
            s.tt("dve", rkp[:], r_c[:, :, cs], km[:], ALU.mult, reads=allh("r_c", gp) + ["km"], writes=["rkp"])
            yield

        def unit_pre(j, h, u, gp):
            r_c, k_c, sgw, a_c, lhid, v_r, sg_t = r_c2[gp], k_c2[gp], sgw2[gp], a_c2[gp], lhid2[gp], v_r2[gp], sg_t2[gp]
            AR2 = AR[:, h, :, :].rearrange("c a t -> c (a t)")
            pa = u % 2
            psA_ = psAs[pa]
            ka = ("psA", pa)
            kn = ("psN", u)
            s.mm(psA_[:, 0:256], bh[:, h, :], AR2, reads=["bh", "AR_at", "AR_rt"], writes=[ka])
            s.mm(psA_[:, 256:512], kh[:, h, :], AR2, reads=["kh", "AR_at", "AR_rt"], writes=[ka])
            s.mm(psN[u][:, 256:384], AR[:, h, 0, :], bh[:, h, :], reads=["bh", "AR_at"], writes=[kn])
            yield
            mUI = mk[:, 0:2, :].rearrange("p a t -> p (a t)")
            s.tt("dve", A1sb[u][:], psA_[:, 0:256], mUI, ALU.mult, reads=[ka, "mk"], writes=[("A1", u)])
            s.tt("dve", QMP[u][0][:, 0:128], psA_[:, 0:128], mk[:, 0, :], ALU.mult, reads=[ka, "mk"], writes=[("QMP", u, 0)])
            s.tt("dve", QMP[u][0][:, 256:384], psN[u][:, 256:384], mk[:, 2, :], ALU.mult, reads=[kn, "mk"], writes=[("QMP", u, 0)])
            s.tt("dve", A2sb[u][:], psA_[:, 256:512], mUI, ALU.mult, reads=[ka, "mk"], writes=[("A2", u)])
            s.tt("dve", QMP[u][0][:, 128:256], A1sb[u][:, 0:128].bitcast(F32), idf[:], ALU.add, reads=[("A1", u), "idf"], writes=[("QMP", u, 0)])
            s.tt("dve", QMP[u][1][:, 128:256], A1sb[u][:, 0:128].bitcast(F32), idf[:], ALU.add, reads=[("A1", u), "idf"], writes=[("QMP", u, 1)])
            yield
            cur = 0
            for it in range(7):
                nxt = 1 - cur
                last = (it == 6)
                T_ = QMP[u][cur]
                Qc = T_[:, 0:128]
                Pc = T_[:, 256:384]
                kc_ = ("QMP", u, cur)
                if it == 0:
                    s.mm(psN[u][:, 0:128], Pc, Qc, reads=[kc_], writes=[kn])
                elif not last:
                    s.mm(psN[u][:, 0:256], Pc, T_[:, 0:256], reads=[kc_], writes=[kn])
                else:
                    s.mm(psN[u][:, 128:256], Pc, T_[:, 128:256], reads=[kc_], writes=[kn])
                if not last:
                    s.mm(psN[u][:, 256:384], Qc, Pc, reads=[kc_], writes=[kn])
                yield
                if not last:
                    s.copy("act", QMP[u][nxt][:].rearrange("p (a t) -> p a t", a=3)[:, 0:3:2, :],
                           psN[u][:, 0:384].rearrange("p (a t) -> p a t", a=3)[:, 0:3:2, :], reads=[kn], writes=[("QMP", u, nxt)])
                if it >= 1:
                    s.tt("dve", QMP[u][nxt][:, 128:256], psN[u][:, 128:256], T_[:, 128:256].bitcast(F32), ALU.add,
                         reads=[kn, kc_], writes=[("QMP", u, nxt)])
                yield
                cur = nxt
            M = QMP[u][cur][:, 128:256]
            Mkey = ("QMP", u, cur)
            psX = psN[u]
            s.tr(psX[:, 0:64], AR[:, h, 0, :].bitcast(F32), idf[0:64, 0:64], reads=["AR_at", "idf"], writes=[kn])
            s.tr(psX[:, 64:128], Bt[:, h, :], idf[0:64, 0:64], reads=["Bt", "idf"], writes=[kn])
            s.tr(psX[:, 128:192], Kt[:, h, :], idf[0:64, 0:64], reads=["Kt", "idf"], writes=[kn])
            s.mm(psX[:, 320:384], A2sb[u][:, 0:128], v_r[:, j, h * 64:(h + 1) * 64], reads=[("A2", u), ("v_r", j, gp)], writes=[kn])
            yield
            s.copy("act", tok3[u][:].rearrange("p a k -> p (a k)"), psX[:, 0:192], reads=[kn], writes=[("tok3", u)])
            s.copy("act", AVsb[u][:], psX[:, 320:384], reads=[kn], writes=[("AV", u)])
            yield
            s.mm(psX[0:64, 192:320], tok3[u][:, 0, :], M, reads=[("tok3", u), Mkey], writes=[kn])
            s.mm(psX[:, 384:448], M, AVsb[u][:], reads=[Mkey, ("AV", u)], writes=[kn])
            yield
            s.copy("act", G1sb[u][:], psX[0:64, 192:320], reads=[kn], writes=[("G1", u)])
            s.copy("act", P2sb[u][:], psX[:, 384:448], reads=[kn], writes=[("P2", u)])
            yield

        def unit_chain(Gi, j, h, u):
            gp = Gi % 2
            r_c, k_c, sgw, a_c, lhid, v_r, sg_t = r_c2[gp], k_c2[gp], sgw2[gp], a_c2[gp], lhid2[gp], v_r2[gp], sg_t2[gp]
            cidx = Gi * NCH + j
            so = cidx % 2
            sn = 1 - so
            vv = v_r[:, j, h * 64:(h + 1) * 64]
            psC = psN[u]
            Ups = psC[:, 448:512]
            Sps = psC[0:64, 0:64]
            Yps = psC[:, 64:128]
            Rps = psC[:, 128:130]
            key = ("psN", u)
            s.mm(Ups, G1sb[u][:], Ssb[h][so][:], reads=[("G1", u), ("S", h, so)], writes=[key])
            yield
            s.tt("dve", Usb[u][:], Ups, P2sb[u][:], ALU.add, reads=[key, ("P2", u)], writes=[("U", u)])
            yield
            s.mm(Sps, tok3[u][:, 2, :], vv, start=True, stop=False, reads=[("tok3", u), ("v_r", j, gp)], writes=[key])
            s.mm(Sps, tok3[u][:, 1, :], Usb[u][:], start=False, stop=True, reads=[("tok3", u), ("U", u)], writes=[key])
            s.mm(Yps, AR[:, h, 1, :], Ssb[h][so][:], start=True, stop=False, reads=["AR_rt", ("S", h, so)], writes=[key])
            s.mm(Yps, A1sb[u][:, 128:256], Usb[u][:], start=False, stop=False, reads=[("A1", u), ("U", u)], writes=[key])
            s.mm(Yps, A2sb[u][:, 128:256], vv, start=False, stop=True, reads=[("A2", u), ("v_r", j, gp)], writes=[key])
            s.mm(Rps, rkp[:, h, :], rk2[:, h, :], reads=["rkp", "rk2"], writes=[key])
            yield
            s.stt(Ssb[h][sn][:], Ssb[h][so][:].bitcast(F32), gLt[:, h, 0:1], Sps, ALU.mult, ALU.add,
                  reads=[("S", h, so), "gLt", key], writes=[("S", h, sn)])
            s.copy("act", ysb[:, h, :], Yps, reads=[key], writes=[("ysb", h)])
            s.copy("act", rks[:, h:h + 1], Rps[:, 0:1], reads=[key], writes=[("rks", h)])
            yield
            s.op("dve", lambda e, h=h: e.bn_stats(out=bst[:, h, :], in_=ysb[:, h, :]), reads=[("ysb", h)], writes=[("bst", h)])
            s.op("dve", lambda e, h=h: e.bn_aggr(out=mv[:, h, :], in_=bst[:, h, :]), reads=[("bst", h)], writes=[("mv", h)])
            yield

        def rr(gens):
            gens = list(gens)
            while gens:
                for g_ in list(gens):
                    try:
                        next(g_)
                    except StopIteration:
                        gens.remove(g_)

        def chunk_out(Gi, j):
            gp = Gi % 2
            r_c, k_c, sgw, a_c, lhid, v_r, sg_t = r_c2[gp], k_c2[gp], sgw2[gp], a_c2[gp], lhid2[gp], v_r2[gp], sg_t2[gp]
            t0 = Gi * GT + j * L
            oo = cnt["o"] % 2
            cnt["o"] += 1
            s.act(rs4[:], mv[:, :, 1], AF.Ln, bias=eps_t[:, 1:2], reads=allh("mv") + ["eps"], writes=["rs4"])
            yield
            s.act(rs4[:], rs4[:], AF.Exp, scale=-0.5, reads=["rs4"], writes=["rs4"])
            yield
            for h in range(4):
                s.ts("dve", yn[:, h * 64:(h + 1) * 64], ysb[:, h, :], mv[:, h, 0:1], rs4[:, h:h + 1], ALU.subtract, ALU.mult,
                     reads=[("ysb", h), ("mv", h), "rs4"], writes=["yn"])
                yield
            s.tt("pool", yn[:], yn[:], lnx_sb[:, 0, :], ALU.mult, reads=["yn", "lnx"], writes=["yn"])
            yield
            s.tt("pool", yn[:], yn[:], lnx_sb[:, 1, :], ALU.add, reads=["yn", "lnx"], writes=["yn"])
            yield
            for h in range(4):
                s.stt(ogo[oo][:, h * 64:(h + 1) * 64], v_r[:, j, h * 64:(h + 1) * 64].bitcast(F32), rks[:, h:h + 1], yn[:, h * 64:(h + 1) * 64],
                      ALU.mult, ALU.add, reads=[("v_r", j, gp), ("rks", h), "yn"], writes=["ogo"])
                yield
            s.tt("dve", ogo[oo][:], ogo[oo][:], sg_t[:, j, :], ALU.mult, reads=["ogo", ("sg_t", j, gp)], writes=["ogo"])
            yield
            for jj in range(4):
                s.ts("pool", ogo4[oo][:, jj, :], ogo[oo][:], sel_sb[:, jj:jj + 1], 1.0, ALU.mult, ALU.mult,
                     reads=["ogo", "sel"], writes=["ogo4"])
                yield
            s.dma("sp", og1buf[t0 // 1024, t0 % 1024:t0 % 1024 + 128, :].rearrange("t (j f) -> t j f", j=4), ogo4[oo][:], reads=["ogo4"], writes=[("og1buf", t0 // 1024)])
            yield
            if (t0 + 128) % 1024 == 0:
                ck_ = t0 // 1024
                s.cc(lambda e, ck_=ck_: e.collective_compute("AllReduce", ALU.add, replica_groups=GROUPS,
                                                             ins=[og1buf[ck_].opt()], outs=[og1all[ck_].opt()]),
                     reads=[("og1buf", ck_)])
                yield

        def chain_gens(gs):
            for g_ in gs:
                yield from g_

        def rr2(gens, bg):
            gens = list(gens)
            while gens:
                for g_ in list(gens):
                    try:
                        next(g_)
                    except StopIteration:
                        gens.remove(g_)
                if bg[0] is not None:
                    try:
                        next(bg[0])
                    except StopIteration:
                        bg[0] = None

        rr([stage_Pa(0)])
        rr([stage_Pb(0)])
        prev_out = []
        for Gi in range(n_groups):
            gp = Gi % 2
            bg = [chain_gens([stage_Pa(Gi + 1), stage_Pb(Gi + 1)])] if Gi + 1 < n_groups else [None]
            for j in range(NCH):
                rr([chunk_elem(j, gp)] + prev_out)
                prev_out = []
                gens = [unit_pre(j, h, h, gp) for h in range(4)]
                for pair in ((0, 1), (2, 3)):
                    for _ in range(2):
                        for u_ in pair:
                            next(gens[u_])
                rr2(gens, bg)
                rr2([unit_chain(Gi, j, h, h) for h in range(4)], bg)
                prev_out = [chunk_out(Gi, j)]
            if bg[0] is not None:
                rr([bg[0]])
        rr(prev_out)
        s.emit(nc, limit=limit, semstack=semstack, tag='B')


def consts_B():
    r = np.arange(128)[:, None]
    c = np.arange(128)[None, :]
    masks = np.stack([(r < c), (r <= c), (r > c)], axis=1).astype(np.float32)
    return dict(masks=np.ascontiguousarray(masks), identf=np.eye(128, dtype=np.float32), identb=np.eye(128).astype(bf))


def inputs_B(inp, b, g):
    cs = slice(g * 256, (g + 1) * 256)
    w_in = inp["rwkv_w_in"]
    w4 = np.concatenate([w_in[n][:, cs] for n in range(4)], axis=1)
    mixT = np.ascontiguousarray(inp["rwkv_mix"].reshape(6, 8, 128).transpose(2, 1, 0))
    hd = lambda p: np.asarray(p).reshape(-1)[cs].reshape(4, 64).T
    cpar = np.stack([hd(inp["rwkv_w0"]), hd(inp["rwkv_a0"]), hd(inp["rwkv_k_k"]), hd(inp["rwkv_k_a"]), hd(inp["rwkv_r_k"])], axis=1)
    lnx = np.stack([np.broadcast_to(inp["rwkv_lnx_g"][cs][None, :], (128, 256)),
                    np.broadcast_to(inp["rwkv_lnx_b"][cs][None, :], (128, 256))], axis=1)
    m = dict(wout0=np.ascontiguousarray(inp["moba_w_out"]),
             gbc=np.ascontiguousarray(np.broadcast_to(inp["rwkv_norm_g"][None, :], (128, 1024))),
             mixT=mixT, w4=np.ascontiguousarray(w4),
             wl=np.ascontiguousarray(np.concatenate([inp["rwkv_w1"], inp["rwkv_a1"]], axis=1)),
             w2a2=np.ascontiguousarray(np.stack([inp["rwkv_w2"][:, cs], inp["rwkv_a2"][:, cs]], axis=1)),
             cpar=np.ascontiguousarray(cpar.astype(np.float32)), lnx=np.ascontiguousarray(lnx.astype(np.float32)))
    m.update(consts_B())
    return m


D = 1024
NT = 64


def phase_C1(nc, semstack, ogall, og1all, ssbuf, x2dram):
    dram = lambda n, sh, dt, kind="ExternalInput": nc.dram_tensor("c_" + n, sh, dt, kind=kind).ap()
    xc = dram("xc", [S, 256], F32)
    w0c = dram("w0c", [D, 256], F32)
    w1c = dram("w1c", [D, 256], F32)
    identb = dram("identb", [128, 128], BF16)
    s = Sched()
    with ExitStack() as st:
        sb = lambda name, shape, dt: st.enter_context(nc.sbuf_tensor("C_" + name, shape, dt))
        xin = [sb("xin%d" % i, [128, 256], F32) for i in range(3)]
        o1in = [sb("o1in%d" % i, [128, D], F32) for i in range(3)]
        ogf = [sb("ogf%d" % i, [128, 8, 128], F32) for i in range(3)]
        ogin = [sb("ogin%d" % i, [128, 8, 128], BF16) for i in range(2)]
        o1b = sb("o1b", [128, D], BF16)
        o1T = [sb("o1T%d" % i, [128, 8, 128], BF16) for i in range(2)]
        x2t = [sb("x2t%d" % i, [128, 256], F32) for i in range(2)]
        junk = sb("junk", [128, 256], BF16)
        wo0 = sb("wo0", [128, 8, 256], BF16)
        wo1 = sb("wo1", [128, 8, 256], BF16)
        wst = sb("wst", [128, 256], F32)
        idb = sb("idb", [128, 128], BF16)
        ssq = sb("ssq", [128, NT], F32)
        psb = [st.enter_context(nc.psum_tensor("C_ps%d" % i, [128, 512], F32)) for i in range(4)]
        psP = psb[0:2]
        psT = psb[2]
        psT_bf = psT[:].bitcast(BF16)
        s.dma("sp", idb[:], identb[:], writes=["idb"])
        cstg = [(wst, "wst"), (xin[0], ("xin", 0)), (xin[1], ("xin", 1)), (xin[2], ("xin", 2))]
        for c in range(8):
            w_, k_ = cstg[c % 4]
            s.dma("sp", w_[:], w0c[c * 128:(c + 1) * 128, :], writes=[k_])
            s.copy("act", wo0[:, c, :], w_[:], reads=[k_], writes=["wo0"])
        for c in range(8):
            w_, k_ = cstg[c % 4]
            s.dma("sp", w_[:], w1c[c * 128:(c + 1) * 128, :], writes=[k_])
            s.copy("dve", wo1[:, c, :], w_[:], reads=[k_], writes=["wo1"])
        def c_stage1(ti):
            xs = ti % 3
            tok = ti * 128
            ck, c0 = tok // 1024, tok % 1024
            s.dma("sp", xin[xs][:], xc[tok:tok + 128, :], writes=[("xin", xs)])
            s.dma("sp", o1in[xs][:], og1all[ck, c0:c0 + 128, :], writes=[("o1in", xs)])
            s.dma("pool", ogf[xs][:], ogall[ck, :, c0:c0 + 128].rearrange("(c p) t -> p c t", p=128), writes=[("ogf", xs)])

        def c_stage2(ti):
            xs = ti % 3
            b2 = ti % 2
            s.copy("dve", ogin[b2][:], ogf[xs][:], reads=[("ogf", xs)], writes=[("ogin", b2)])
            s.copy("act", o1b[:], o1in[xs][:], reads=[("o1in", xs)], writes=["o1b"])
            for c in range(8):
                s.tr(psT_bf[:, c * 128:(c + 1) * 128], o1b[:, c * 128:(c + 1) * 128], idb[:], reads=["o1b", "idb"], writes=["psT"])
            s.copy("act", o1T[b2][:], psT_bf.rearrange("p (c t) -> p c t", c=8), reads=["psT"], writes=[("o1T", b2)])

        def c_stage3(ti):
            xs = ti % 3
            b2 = ti % 2
            tok = ti * 128
            ps = ti % 2
            for c in range(8):
                s.mm(psP[ps][:, 0:256], ogin[b2][:, c, :], wo0[:, c, :], start=(c == 0), stop=False,
                     reads=[("ogin", b2), "wo0"], writes=[("psP", ps)])
            for c in range(8):
                s.mm(psP[ps][:, 0:256], o1T[b2][:, c, :], wo1[:, c, :], start=False, stop=(c == 7),
                     reads=[("o1T", b2), "wo1"], writes=[("psP", ps)])
            s.tt("dve", x2t[b2][:], psP[ps][:, 0:256], xin[xs][:], ALU.add,
                 reads=[("psP", ps), ("xin", xs)], writes=[("x2t", b2)])
            s.op("dve", lambda e, b2=b2, ti=ti: e.scalar_tensor_tensor(
                out=junk[:], in0=x2t[b2][:], scalar=1.0, in1=x2t[b2][:], op0=ALU.mult, op1=ALU.mult, accum_out=ssq[:, ti:ti + 1]),
                reads=[("x2t", b2)], writes=["junk", "ssq"])
            s.dma("sp", x2dram[tok:tok + 128, :], x2t[b2][:], reads=[("x2t", b2)])

        for ti in range(NT + 2):
            if ti < NT:
                c_stage1(ti)
            if 1 <= ti <= NT:
                c_stage2(ti - 1)
            if ti >= 2:
                c_stage3(ti - 2)
        s.dma("sp", ssbuf[:, :], ssq[:], reads=["ssq"])
        s.emit(nc, semstack=semstack, tag='C')


def phase_C2(nc, semstack, ssall, x2dram):
    dram = lambda n, sh, dt, kind="ExternalInput": nc.dram_tensor("c_" + n, sh, dt, kind=kind).ap()
    gbc = dram("gbc", [128, 256], F32)
    out = dram("out", [S, 256], F32, kind="ExternalOutput")
    s = Sched()
    with ExitStack() as st:
        sb = lambda name, shape, dt: st.enter_context(nc.sbuf_tensor("E_" + name, shape, dt))
        x2t = [sb("x2t%d" % i, [128, 256], F32) for i in range(3)]
        res = [sb("res%d" % i, [128, 256], F32) for i in range(3)]
        gbc_sb = sb("gbc_sb", [128, 256], F32)
        ssq = sb("ssq", [128, NT], F32)
        rstd = sb("rstd", [128, NT], F32)
        eps_t = sb("eps_t", [128, 1], F32)
        s.dma("sp", gbc_sb[:], gbc[:], writes=["gbc"])
        s.dma("sp", ssq[:], ssall[:, :], writes=["ssq"])
        s.memset("pool", eps_t[:], 1e-6, writes=["eps"])
        s.act(rstd[:], ssq[:], AF.Ln, scale=1.0 / D, bias=eps_t[:, 0:1], reads=["ssq", "eps"], writes=["rstd"])
        s.act(rstd[:], rstd[:], AF.Exp, scale=-0.5, reads=["rstd"], writes=["rstd"])
        for ti in range(NT):
            k = ti % 3
            tok = ti * 128
            s.dma("sp", x2t[k][:], x2dram[tok:tok + 128, :], writes=[("x2t", k)])
            s.stt(res[k][:], x2t[k][:], rstd[:, ti:ti + 1], gbc_sb[:], ALU.mult, ALU.mult,
                  reads=[("x2t", k), "rstd", "gbc"], writes=[("res", k)])
            s.dma("pool", out[tok:tok + 128, :], res[k][:], reads=[("res", k)])
        s.emit(nc, semstack=semstack, tag='E')


GROUPS = [[0, 1, 2, 3], [4, 5, 6, 7]]


def allreduce_block(nc, semstack, pairs, tag):
    sem = semstack.enter_context(nc.semaphore(tag + "_cc"))
    with nc.Block() as block:
        @block.gpsimd
        def _(g):
            for i, (src, dst) in enumerate(pairs):
                g.collective_compute("AllReduce", ALU.add, replica_groups=GROUPS,
                                     ins=[src.opt()], outs=[dst.opt()]).then_inc(sem)
                g.wait_ge(sem, i + 1)


def build_fused():
    nc = bass.Bass("TRN2", target_bir_lowering=False)
    x = nc.dram_tensor("x", [S, D], F32, kind="ExternalInput").ap()
    ogbuf = nc.dram_tensor("ogbuf", [8, 1024, 1024], F32).ap()
    ogall = nc.dram_tensor("ogall", [8, 1024, 1024], F32).ap()
    og1buf = nc.dram_tensor("og1buf", [8, 1024, 1024], F32).ap()
    og1all = nc.dram_tensor("og1all", [8, 1024, 1024], F32).ap()
    ssbuf = nc.dram_tensor("ssbuf", [128, 64], F32).ap()
    ssall = nc.dram_tensor("ssall", [128, 64], F32).ap()
    x2dram = nc.dram_tensor("x2dram", [S, 256], F32).ap()
    with ExitStack() as semstack:
        phase_A(nc, semstack, x, ogbuf, ogall)
        phase_B(nc, semstack, x, ogall, og1buf, og1all)
        phase_C1(nc, semstack, ogall, og1all, ssbuf, x2dram)
        allreduce_block(nc, semstack, [(ssbuf, ssall)], "x3")
        phase_C2(nc, semstack, ssall, x2dram)
    return nc


def kernel(**inp):
    inp = {k: np.asarray(v) for k, v in inp.items()}
    x = inp["x"]
    w_in = inp["moba_w_in"]
    nc = build_fused()
    gbcA = np.ascontiguousarray(np.broadcast_to(inp["moba_norm_g"][None, :], (128, 1024)))
    identb = np.eye(128).astype(bf)
    maps = []
    for c in range(8):
        b, g = c // 4, c % 4
        cs = slice(g * 256, (g + 1) * 256)
        m = dict(x=np.ascontiguousarray(x[b]))
        w4 = np.concatenate([w_in[:, k * 1024 + g * 256: k * 1024 + (g + 1) * 256] for k in range(4)], axis=1)
        sel = np.zeros((128, 4), np.float32)
        sel[:, g] = 1.0
        am = dict(gbc=gbcA, w4=np.ascontiguousarray(w4), sel=sel)
        am.update(consts_A(g))
        for k, v in am.items():
            m["a_" + k] = v
        bm = inputs_B(inp, b, g)
        bm["sel"] = sel
        for k, v in bm.items():
            m["b_" + k] = v
        m["c_xc"] = np.ascontiguousarray(x[b][:, cs])
        m["c_w0c"] = np.ascontiguousarray(inp["moba_w_out"][:, cs])
        m["c_w1c"] = np.ascontiguousarray(inp["rwkv_w_out"][:, cs])
        m["c_identb"] = identb
        m["c_gbc"] = np.ascontiguousarray(np.broadcast_to(inp["final_norm_g"][cs][None, :], (128, 256)))
        maps.append(m)
    res = run_bass_kernel_spmd(nc, maps, core_ids=list(range(8))).results
    out = np.zeros((2, 8192, 1024), np.float32)
    for c in range(8):
        b, g = c // 4, c % 4
        out[b, :, g * 256:(g + 1) * 256] = np.asarray(res[c]["c_out"])
    return out
```

```python
import numpy as np
import ml_dtypes
from concourse.bass_utils import run_bass_kernel_spmd
import concourse.bass as bass
import concourse.mybir as mybir
from contextlib import ExitStack

F32 = mybir.dt.float32
BF16 = mybir.dt.bfloat16
AF = mybir.ActivationFunctionType
ALU = mybir.AluOpType
AX = mybir.AxisListType

ENGS = ("pe", "act", "dve", "pool", "sp")
DMA_POOLS = {"sp": (0, 8), "pool": (8, 6)}
N_DMA_SEMS = 14


class Op:
    __slots__ = ("eng", "fn", "deps", "signal", "sig_idx", "is_dma", "dma_sem", "dma_val", "idx", "cc")

    def __init__(self, eng, fn, is_dma):
        self.eng = eng
        self.fn = fn
        self.deps = []
        self.signal = False
        self.sig_idx = 0
        self.is_dma = is_dma
        self.dma_sem = -1
        self.dma_val = 0
        self.cc = False


class Sched:
    def __init__(self):
        self.ops = []
        self.last_w = {}
        self.readers = {}
        self.n_dma_q = {}
        self.n_cc = 0

    def op(self, eng, fn, reads=(), writes=(), dma=False):
        o = Op(eng, fn, dma)
        o.idx = len(self.ops)
        _isps = lambda b: (isinstance(b, str) and b.startswith("ps")) or (isinstance(b, tuple) and isinstance(b[0], str) and b[0].startswith("ps"))
        writes = list(writes) + [b for b in reads if _isps(b)]
        reads = [b for b in reads if not _isps(b)]
        deps = {}
        for b in reads:
            w = self.last_w.get(b)
            if w is not None:
                deps[w.idx] = w
        for b in writes:
            w = self.last_w.get(b)
            if w is not None:
                deps[w.idx] = w
            for r in self.readers.get(b, ()):
                deps[r.idx] = r
        best = {}
        for d in deps.values():
            if d is o:
                continue
            if d.is_dma:
                o.deps.append(d)
                continue
            if d.eng == eng and eng == "pe" and not dma:
                continue
            if d.eng not in best or best[d.eng].idx < d.idx:
                best[d.eng] = d
        for d in best.values():
            o.deps.append(d)
            d.signal = True
        for b in reads:
            self.readers.setdefault(b, []).append(o)
        for b in writes:
            self.last_w[b] = o
            self.readers[b] = []
        if dma:
            base, n = DMA_POOLS[eng]
            k = self.n_dma_q.get(eng, 0)
            self.n_dma_q[eng] = k + 1
            o.dma_sem = base + k % n
            o.dma_val = 16 * (k // n + 1)
        self.ops.append(o)
        return o

    def cc(self, fn, reads=(), writes=()):
        o = self.op("pool", fn, reads, writes, dma=True)
        self.n_dma_q["pool"] -= 1
        o.cc = True
        o.dma_sem = N_DMA_SEMS + self.n_cc
        o.dma_val = 1
        self.n_cc += 1
        return o

    def mm(self, out, lhsT, rhs, start=True, stop=True, reads=(), writes=()):
        return self.op("pe", lambda e: e.matmul(out, lhsT=lhsT, rhs=rhs, start=start, stop=stop), reads, writes)

    def tr(self, out, in_, ident, reads=(), writes=()):
        return self.op("pe", lambda e: e.transpose(out, in_, ident), reads, writes)

    def act(self, out, in_, func, scale=None, bias=None, accum_out=None, reads=(), writes=()):
        kw = {}
        if scale is not None:
            kw["scale"] = scale
        if bias is not None:
            kw["bias"] = bias
        if accum_out is not None:
            kw["accum_out"] = accum_out
        return self.op("act", lambda e: e.activation(out=out, in_=in_, func=func, **kw), reads, writes)

    def dma(self, q, out, in_, reads=(), writes=()):
        return self.op(q, lambda e: e.dma_start(out=out, in_=in_), reads, writes, dma=True)

    def copy(self, eng, out, in_, reads=(), writes=()):
        if eng == "act":
            return self.op("act", lambda e: e.activation(out=out, in_=in_, func=AF.Copy), reads, writes)
        return self.op(eng, lambda e: e.tensor_copy(out=out, in_=in_), reads, writes)

    def memset(self, eng, ap, val, writes=()):
        return self.op(eng, lambda e: e.memset(ap, val), (), writes)

    def tt(self, eng, out, in0, in1, op, reads=(), writes=()):
        return self.op(eng, lambda e: e.tensor_tensor(out=out, in0=in0, in1=in1, op=op), reads, writes)

    def ts(self, eng, out, in0, s1, s2, op0, op1=None, reads=(), writes=(), accum_out=None):
        kw = {}
        if op1 is not None:
            kw["op1"] = op1
        if accum_out is not None:
            kw["accum_out"] = accum_out
        return self.op(eng, lambda e: e.tensor_scalar(out=out, in0=in0, scalar1=s1, scalar2=s2, op0=op0, **kw), reads, writes)

    def stt(self, out, in0, scalar, in1, op0, op1, reads=(), writes=(), eng="dve"):
        return self.op(eng, lambda e: e.scalar_tensor_tensor(out=out, in0=in0, scalar=scalar, in1=in1, op0=op0, op1=op1), reads, writes)

    def emit(self, nc, final_waits=True, limit=None, semstack=None, tag=''):
        if limit is not None:
            self.ops = self.ops[:limit]
            for o in self.ops:
                o.signal = False
            for o in self.ops:
                for d in o.deps:
                    if not d.is_dma:
                        d.signal = True
        cnt = {e: 0 for e in ENGS}
        for o in self.ops:
            if o.is_dma:
                continue
            if o.signal:
                cnt[o.eng] += 1
                o.sig_idx = cnt[o.eng]
        per_eng = {e: [o for o in self.ops if o.eng == e] for e in ENGS}
        with ExitStack() as st:
            sst = semstack if semstack is not None else st
            sems = {e: sst.enter_context(nc.semaphore(tag + "s_" + e)) for e in ENGS}
            dsems = [sst.enter_context(nc.semaphore(tag + "d%d" % i)) for i in range(N_DMA_SEMS + self.n_cc)]
            block = st.enter_context(nc.Block())
            all_dma = [o for o in self.ops if o.is_dma]

            def body(ename):
                def f(engine):
                    waited = {}

                    def wait(sem_key, sem, val):
                        if waited.get(sem_key, 0) >= val:
                            return
                        waited[sem_key] = val
                        engine.wait_ge(sem, val)

                    for o in per_eng[ename]:
                        for d in o.deps:
                            if d.is_dma:
                                wait(("d", d.dma_sem), dsems[d.dma_sem], d.dma_val)
                            else:
                                wait(("e", d.eng), sems[d.eng], d.sig_idx)
                        if o.is_dma:
                            if o.dma_val > 16:
                                wait(("d", o.dma_sem), dsems[o.dma_sem], o.dma_val - 16)
                            ins = o.fn(engine)
                            if o.cc:
                                ins.then_inc(dsems[o.dma_sem])
                            else:
                                ins.then_inc(dsems[o.dma_sem], 16)
                        else:
                            ins = o.fn(engine)
                            if o.signal:
                                ins.then_inc(sems[ename], 1)
                    if final_waits and ename == "sp":
                        last = {}
                        for o in all_dma:
                            last[o.dma_sem] = max(last.get(o.dma_sem, 0), o.dma_val)
                        for s, v in last.items():
                            wait(("d", s), dsems[s], v)
                        for e in ENGS:
                            if cnt[e] > 0:
                                wait(("e", e), sems[e], cnt[e])
                return f

            block.tensor(body("pe"))
            block.scalar(body("act"))
            block.vector(body("dve"))
            block.gpsimd(body("pool"))
            block.sync(body("sp"))


S = 8192
D = 1024
NG_A = S // 512
bf = ml_dtypes.bfloat16
NEG = -1.0e30


def phase_A(nc, semstack, x, ogbuf, ogall, n_groups=NG_A, limit=None):
    dram = lambda n, sh, dt, kind="ExternalInput": nc.dram_tensor("a_" + n, sh, dt, kind=kind).ap()
    gbc = dram("gbc", [128, D], F32)
    w4 = dram("w4", [D, 1024], F32)
    qc = dram("qc", [4, 8, S], BF16)
    kc = dram("kc", [8, S], BF16)
    oh = dram("oh", [32, S], BF16)
    caus = dram("caus", [128, 2, 256], BF16)
    ident = dram("ident", [128, 128], BF16)
    sel = dram("sel", [128, 4], F32)
    s = Sched()
    with ExitStack() as st:
        sb = lambda name, shape, dt: st.enter_context(nc.sbuf_tensor("A_" + name, shape, dt))
        NX = 3
        xin = [sb("xin%d" % i, [128, D], F32) for i in range(NX)]
        gbc_sb = sb("gbc_sb", [128, D], F32)
        junk = sb("junk", [128, D], BF16)
        hn = [sb("hn%d" % i, [128, D], BF16) for i in range(2)]
        hT = sb("hT", [128, 8, 512], BF16)
        w_sb = sb("w_sb", [128, 8, 1024], BF16)
        kaug = sb("kaug", [104, 4, S], BF16)
        vaug = sb("vaug", [128, 64, 4, 65], BF16)
        qaug = [sb("qaug%d" % i, [104, 4, 512], BF16) for i in range(2)]
        sg = [sb("sg%d" % i, [64, 4, 512], BF16) for i in range(2)]
        gs = sb("gs", [128, 16, 32], F32)
        m8 = sb("m8", [128, 16, 8], F32)
        mk01 = sb("mk01", [128, 16, 32], F32)
        mkb = sb("mkb", [128, 16, 32], BF16)
        NP = 4
        pT = [sb("pT%d" % i, [128, 512], BF16) for i in range(NP)]
        kmean = sb("kmean", [64, 4, 32], BF16)
        ksum = sb("ksum", [64, 4, 2], F32)
        ss = sb("ss", [128, 4], F32)
        rstd = sb("rstd", [128, 4], F32)
        ones32 = sb("ones32", [128, 64], F32)
        rden = sb("rden", [128, 512], F32)
        t1 = [sb("t1_%d" % i, [64, 512], F32) for i in range(2)]
        og = [sb("og%d" % i, [64, 512], F32) for i in range(2)]
        og4 = [sb("og4_%d" % i, [64, 4, 512], F32) for i in range(2)]
        sel_sb = sb("sel_sb", [128, 4], F32)
        id_sb = sb("id_sb", [128, 128], BF16)
        caus_sb = sb("caus_sb", [128, 2, 256], BF16)
        psb = [st.enter_context(nc.psum_tensor("A_ps%d" % i, [128, 512], F32)) for i in range(8)]
        psS = psb[0:3]
        psO = psb[3:5]
        psP = psb[5:7]
        psM = psb[7]
        psM_bf = psM[:].bitcast(BF16)

        s.dma("sp", gbc_sb[:], gbc[:], writes=["gbc"])
        s.dma("sp", sel_sb[:], sel[:], writes=["sel"])
        s.dma("sp", id_sb[:], ident[:], writes=["ident"])
        s.dma("sp", caus_sb[:], caus[:], writes=["caus"])
        for c in range(8):
            slot = c % NX
            s.dma("sp", xin[slot][:], w4[c * 128:(c + 1) * 128, :], writes=[("xin", slot)])
            s.copy("pool", w_sb[:, c, :], xin[slot][:], reads=[("xin", slot)], writes=["w_sb"])
        for h in range(4):
            s.dma("pool", kaug[64:96, h, :], oh[:, :], writes=[("kaug_c", h, 0)])
            s.dma("pool", kaug[96:104, h, :], kc[:, :], writes=[("kaug_c", h, 1)])
        s.memset("pool", vaug[:, :, :, 64:65], 1.0, writes=["vaug_ones"])
        s.memset("pool", gs[:], NEG, writes=["gs"])
        s.memset("pool", ones32[:], 1.0, writes=["ones32"])
        for h in range(4):
            s.memset("pool", kmean[:, h, :], 0.0, writes=[("kmean", h)])

        xcount = [0]
        pcount = [0]
        scount = [0]
        ocount = [0]

        def stage_P(G):
            qs = G % 2
            t0 = G * 512
            s.dma("sp", qaug[qs][96:104, :, :], qc[:, :, t0:t0 + 512].rearrange("h r t -> r h t"),
                  writes=[("qaug_c", qs)])
            yield
            for tt in range(4):
                xs = xcount[0] % NX
                xcount[0] += 1
                hs = tt % 2
                tok = t0 + tt * 128
                s.dma("sp", xin[xs][:], x[tok:tok + 128, :], writes=[("xin", xs)])
                s.op("dve", lambda e, xs=xs, tt=tt: e.scalar_tensor_tensor(
                    out=junk[:], in0=xin[xs][:], scalar=1.0, in1=xin[xs][:],
                    op0=ALU.mult, op1=ALU.mult, accum_out=ss[:, tt:tt + 1]),
                    reads=[("xin", xs)], writes=["junk", ("ss", tt)])
                s.act(rstd[:, tt:tt + 1], ss[:, tt:tt + 1], AF.Ln, scale=1.0 / D, bias=eps_t[:, 0:1],
                      reads=[("ss", tt), "eps"], writes=[("rstd", tt)])
                s.act(rstd[:, tt:tt + 1], rstd[:, tt:tt + 1], AF.Exp, scale=-0.5,
                      reads=[("rstd", tt)], writes=[("rstd", tt)])
                s.stt(hn[hs][:], xin[xs][:], rstd[:, tt:tt + 1], gbc_sb[:], ALU.mult, ALU.mult,
                      reads=[("xin", xs), ("rstd", tt), "gbc"], writes=[("hn", hs)])
                yield
                for c in range(8):
                    s.tr(psM_bf[:, c * 128:(c + 1) * 128], hn[hs][:, c * 128:(c + 1) * 128], id_sb[:],
                         reads=[("hn", hs), "ident"], writes=["psM"])
                s.copy("dve", hT[:, :, tt * 128:(tt + 1) * 128],
                       psM_bf.rearrange("p (c t) -> p c t", c=8), reads=["psM"], writes=[("hT", tt)])
                yield
            hT_keys = [("hT", tt) for tt in range(4)]

            def proj(col0):
                ps = pcount[0] % 2
                pcount[0] += 1
                for c in range(8):
                    s.mm(psP[ps][:, :], w_sb[:, c, col0:col0 + 128], hT[:, c, :], start=(c == 0), stop=(c == 7),
                         reads=hT_keys + ["w_sb"], writes=[("psP", ps)])
                return ps

            for hp in range(2):
                ps = proj(hp * 128)
                for hh in range(2):
                    h = 2 * hp + hh
                    s.copy("dve", qaug[qs][0:64, h, :], psP[ps][hh * 64:(hh + 1) * 64, :], reads=[("psP", ps)],
                           writes=[("qaug_q", qs, h)])
                    yield
                ps = proj(256 + hp * 128)
                for hh in range(2):
                    h = 2 * hp + hh
                    for b2 in range(2):
                        s.act(kaug[0:64, h, t0 + b2 * 256:t0 + (b2 + 1) * 256],
                              psP[ps][hh * 64:(hh + 1) * 64, b2 * 256:(b2 + 1) * 256],
                              AF.Copy, accum_out=ksum[:, h, b2:b2 + 1], reads=[("psP", ps)],
                              writes=[("kaug_k", h, G), ("ksum", h)])
                    s.ts("dve", kmean[:, h, 2 * G:2 * G + 2], ksum[:, h, :], 1.0 / 256.0, None, ALU.mult, None,
                         reads=[("ksum", h)], writes=[("kmean", h)])
                    yield
            for hp in range(2):
                ps = proj(768 + hp * 128)
                for hh in range(2):
                    h = 2 * hp + hh
                    s.act(sg[qs][:, h, :], psP[ps][hh * 64:(hh + 1) * 64, :], AF.Silu, reads=[("psP", ps)],
                          writes=[("sg", qs, h)])
            yield
            for tt in range(4):
                ps = pcount[0] % 2
                pcount[0] += 1
                for c in range(8):
                    s.mm(psP[ps][:, 0:256], hT[:, c, tt * 128:(tt + 1) * 128], w_sb[:, c, 512:768],
                         start=(c == 0), stop=(c == 7), reads=[("hT", tt), "w_sb"], writes=[("psP", ps)])
                s.copy("dve", vaug[:, 4 * G + tt, :, 0:64], psP[ps][:, 0:256].rearrange("p (h d) -> p h d", h=4),
                       reads=[("psP", ps)], writes=[("vaug", 4 * G + tt)])
                yield
            psM3 = psM[:].rearrange("p (i n) -> p i n", n=32)
            for tt in range(4):
                for h in range(4):
                    s.mm(psM3[:, tt * 4 + h, :], qaug[qs][0:64, h, tt * 128:(tt + 1) * 128], kmean[:, h, :],
                         reads=[("qaug_q", qs, h), ("kmean", h)], writes=["psM"])
            own0, own1 = 2 * G, 2 * G + 1
            if own0 > 0:
                s.copy("dve", gs[:, 0:8, 0:own0], psM3[:, 0:8, 0:own0], reads=["psM"], writes=["gs"])
            s.copy("dve", gs[:, 8:16, 0:own1], psM3[:, 8:16, 0:own1], reads=["psM"], writes=["gs"])
            yield
            for i in range(16):
                s.op("dve", lambda e, i=i: e.max(out=m8[:, i, :], in_=gs[:, i, :]), reads=["gs"], writes=["m8"])
                if i % 4 == 3:
                    yield
            s.tt("dve", mk01[:], gs[:], m8[:, :, 2:3].to_broadcast([128, 16, 32]), ALU.is_ge,
                 reads=["gs", "m8"], writes=["mk01"])
            s.ts("dve", mkb[:], mk01[:], -1.0, 30000.0, ALU.add, ALU.mult, reads=["mk01"], writes=["mkb"])
            s.memset("dve", mkb[:, 0:8, own0:own0 + 1], 0.0, writes=["mkb"])
            s.memset("dve", mkb[:, 8:16, own1:own1 + 1], 0.0, writes=["mkb"])
            yield
            psM4 = psM_bf.rearrange("p (h t) -> p h t", h=2)
            for hp in range(2):
                for hh in range(2):
                    h = hp * 2 + hh
                    for tt in range(4):
                        s.tr(psM4[64:96, hh, tt * 128:(tt + 1) * 128], mkb[:, tt * 4 + h, :], id_sb[:],
                             reads=["mkb", "ident"], writes=["psM"])
                s.copy("dve", qaug[qs][64:96, hp * 2:hp * 2 + 2, :], psM4[64:96, :, :], reads=["psM"],
                       writes=[("qaug_m", qs, hp)])
                yield

        def stage_A(G, bg):
            qs = G % 2
            t0 = G * 512
            for h in range(4):
                os_ = ocount[0] % 2
                ocount[0] += 1
                chunks = []
                for c in range(4 * G):
                    chunks.append((c, 0, 512, None))
                for cc in range(2):
                    chunks.append((4 * G + cc, 0, 512, cc))
                for cc in range(2):
                    chunks.append((4 * G + 2 + cc, 256, 512, cc))
                qreads = [("qaug_q", qs, h), ("qaug_m", qs, h // 2), ("qaug_c", qs)]
                pend = []
                DLY = 2

                def emit_pv(item):
                    ci_, c_, c0_, c1_, pl_ = item
                    s.mm(psO[os_][0:65, c0_:c1_], vaug[:, c_, h, 0:65], pT[pl_][:, c0_:c1_],
                         start=(ci_ == 0), stop=(ci_ == len(chunks) - 1),
                         reads=[("pT", pl_), ("vaug", c_), "vaug_ones"], writes=[("psO", os_)])

                for ci, (c, c0, c1, diag) in enumerate(chunks):
                    sl = scount[0] % 3
                    pl = scount[0] % NP
                    scount[0] += 1
                    s.mm(psS[sl][:, c0:c1], kaug[0:104, h, c * 128:(c + 1) * 128], qaug[qs][0:104, h, c0:c1],
                         start=True, stop=(diag is None),
                         reads=qreads + [("kaug_k", h, c // 4), ("kaug_c", h, 0), ("kaug_c", h, 1)], writes=[("psS", sl)])
                    if diag is not None:
                        d0 = 0 if c0 == 0 else 256
                        s.mm(psS[sl][:, d0:d0 + 256], id_sb[:], caus_sb[:, diag, :], start=False, stop=True,
                             reads=["ident", "caus"], writes=[("psS", sl)])
                    s.act(pT[pl][:, c0:c1], psS[sl][:, c0:c1], AF.Exp, scale=0.125,
                          reads=[("psS", sl)], writes=[("pT", pl)])
                    pend.append((ci, c, c0, c1, pl))
                    if len(pend) > DLY:
                        emit_pv(pend.pop(0))
                    if bg[0] is not None:
                        try:
                            next(bg[0])
                        except StopIteration:
                            bg[0] = None
                while pend:
                    emit_pv(pend.pop(0))
                ts_ = os_
                s.op("dve", lambda e, os_=os_: e.reciprocal(out=rden[64:65, :], in_=psO[os_][64:65, :]),
                     reads=[("psO", os_)], writes=["rden"])
                s.mm(psM[0:64, :], ones32[64:65, 0:64], rden[64:65, :], reads=["ones32", "rden"], writes=["psM"])
                s.tt("dve", t1[ts_][:], psO[os_][0:64, :], sg[qs][:, h, :], ALU.mult,
                     reads=[("psO", os_), ("sg", qs, h)], writes=[("t1", ts_)])
                s.tt("dve", og[ts_][:], t1[ts_][:], psM[0:64, :], ALU.mult,
                     reads=[("t1", ts_), "psM"], writes=[("og", ts_)])
                for jj in range(4):
                    s.ts("dve", og4[ts_][:, jj, :], og[ts_][:], sel_sb[0:64, jj:jj + 1], 1.0, ALU.mult, ALU.mult,
                         reads=[("og", ts_), "sel"], writes=[("og4", ts_)])
                ck, c0 = t0 // 1024, t0 % 1024
                s.dma("pool", ogbuf[ck, :, c0:c0 + 512].rearrange("(j r) t -> r j t", j=4)[h * 64:(h + 1) * 64, :, :], og4[ts_][:],
                      reads=[("og4", ts_)], writes=[("ogbuf", ck)])

        eps_t = sb("eps_t", [128, 1], F32)
        s.memset("pool", eps_t[:], 1e-6, writes=["eps"])
        for _ in stage_P(0):
            pass
        for G in range(1, n_groups + 1):
            bg = [stage_P(G)] if G < n_groups else [None]
            stage_A(G - 1, bg)
            if bg[0] is not None:
                for _ in bg[0]:
                    pass
            if True:
                if (G - 1) % 2 == 1:
                    ck_ = (G - 1) // 2
                    s.cc(lambda e, ck_=ck_: e.collective_compute("AllReduce", ALU.add, replica_groups=GROUPS,
                                                                 ins=[ogbuf[ck_].opt()], outs=[ogall[ck_].opt()]),
                         reads=[("ogbuf", ck_)])
        s.emit(nc, limit=limit, semstack=semstack, tag='A')


def consts_A(g):
    pos = np.arange(S)
    qc = np.zeros((4, 8, S), dtype=bf)
    for hl in range(4):
        hg = 4 * g + hl
        slope = 2.0 ** (-8.0 * (hg + 1) / 16)
        s8 = np.float64(8.0 * slope)
        p1 = np.float64(bf(s8)); p2 = np.float64(bf(s8 - p1)); p3 = np.float64(bf(s8 - p1 - p2))
        s8p = p1 + p2 + p3
        T = -s8p * pos.astype(np.float64)
        Thi = T.astype(bf); Tlo = (T - Thi.astype(np.float64)).astype(bf)
        qc[hl, 0] = p1; qc[hl, 1] = p2; qc[hl, 2] = p3
        qc[hl, 3] = p1; qc[hl, 4] = p2; qc[hl, 5] = p3
        qc[hl, 6] = Thi; qc[hl, 7] = Tlo
    kc = np.zeros((8, S), dtype=bf)
    kc[0:3] = (pos % 256).astype(bf)
    kc[3:6] = (256 * (pos // 256)).astype(bf)
    kc[6:8] = 1.0
    oh = np.zeros((32, S), dtype=bf)
    oh[pos // 256, pos] = 1.0
    caus = np.zeros((128, 2, 256), dtype=bf)
    j = np.arange(128)[:, None]
    i = np.arange(256)[None, :]
    for c in range(2):
        caus[:, c, :] = np.where(c * 128 + j <= i, 0.0, -30000.0).astype(bf)
    ident = np.eye(128).astype(bf)
    return dict(qc=qc, kc=kc, oh=oh, caus=caus, ident=ident)


F32R = mybir.dt.float32r
S = 8192
D = 1024
bf = ml_dtypes.bfloat16
CDEC = 0.6065306597126334
L = 128


GT = 256
NCH = GT // L


def phase_B(nc, semstack, x, ogall, og1buf, og1all, n_groups=S // GT, limit=None):
    dram = lambda n, sh, dt, kind="ExternalInput": nc.dram_tensor("b_" + n, sh, dt, kind=kind).ap()
    wout0 = dram("wout0", [D, D], F32)
    gbc = dram("gbc", [128, D], F32)
    mixT = dram("mixT", [128, 8, 6], F32)
    w4 = dram("w4", [D, 1024], F32)
    wl = dram("wl", [D, 128], F32)
    w2a2 = dram("w2a2", [64, 2, 256], F32)
    cpar = dram("cpar", [64, 5, 4], F32)
    lnx = dram("lnx", [128, 2, 256], F32)
    masks = dram("masks", [128, 3, 128], F32)
    identf = dram("identf", [128, 128], F32)
    identb = dram("identb", [128, 128], BF16)
    sel = dram("sel", [128, 4], F32)
    s = Sched()
    with ExitStack() as st:
        sb = lambda name, shape, dt: st.enter_context(nc.sbuf_tensor("B_" + name, shape, dt))
        NX = 2
        xin = [sb("xin%d" % i, [128, D], F32) for i in range(NX)]
        ogin = [sb("ogin%d" % i, [128, 8, 128], BF16) for i in range(1)] * 2
        ogf = [sb("ogf%d" % i, [128, 8, 128], F32) for i in range(1)] * 2
        sel_sb = sb("sel_sb", [128, 4], F32)
        ogo4 = [sb("ogo4_%d" % i, [128, 4, 256], F32) for i in range(1)] * 2
        x1t = sb("x1t", [128, D], F32)
        hn = sb("hn", [128, D], BF16)
        junk = hn
        hx = sb("hx", [128, 8, GT + 4], BF16)
        lastcol = sb("lastcol", [128, 8, 2], BF16)
        gbc_sb = sb("gbc_sb", [128, D], F32)
        wo_sb = sb("wo_sb", [128, 8, 1024], BF16)
        wst = x1t
        wa_sb = sb("wa_sb", [128, 8, 1024], BF16)
        wb_sb = sb("wb_sb", [128, 8, 1024], BF16)
        wla_sb = sb("wla_sb", [128, 8, 128], BF16)
        wlb_sb = sb("wlb_sb", [128, 8, 128], BF16)
        mix_sb = sb("mix_sb", [128, 8, 6], F32)
        onem_sb = sb("onem_sb", [128, 8, 6], F32)
        w2a2_sb = sb("w2a2_sb", [64, 2, 256], BF16)
        w2st = sb("w2st", [64, 2, 256], F32)
        cp = sb("cp", [64, 5, 4], F32)
        onemka = sb("onemka", [64, 4], F32)
        rk2 = sb("rk2", [64, 4, 2], F32R)
        lnx_sb = sb("lnx_sb", [128, 2, 256], F32)
        mk = sb("mk", [128, 3, 128], F32)
        idf = sb("idf", [128, 128], F32)
        idb = sb("idb", [128, 128], BF16)
        ones_r = sb("ones_r", [64, 64], F32R)
        ones_f = sb("ones_f", [64, 128], F32)
        eps_t = sb("eps_t", [128, 3], F32)
        ss = sb("ss", [128, 2], F32)
        rstd = sb("rstd", [128, 2], F32)
        Gt = lambda name, dt=F32: sb(name, [64, 4, GT], dt)
        r_c2 = [Gt("r_c%d" % i) for i in range(2)]; k_c2 = [Gt("k_c%d" % i) for i in range(2)]
        sgw2 = [Gt("sgw%d" % i) for i in range(2)]; a_c2 = [Gt("a_c%d" % i) for i in range(2)]
        lhid2 = [sb("lhid%d" % i, [64, 2, GT], BF16) for i in range(2)]
        v_r2 = [sb("v_r%d" % i, [128, NCH, 256], F32R) for i in range(2)]
        sg_t2 = [sb("sg_t%d" % i, [128, NCH, 256], F32) for i in range(2)]
        Ct = lambda name, dt=F32: sb(name, [64, 4, L], dt)
        kk = Ct("kk"); tmp1 = Ct("tmp1"); tmp2 = Ct("tmp2", F32R); cum = Ct("cum"); ex = Ct("ex")
        g_incl = Ct("g_incl"); g_inv = Ct("g_inv"); g_excl = Ct("g_excl"); g_rem = Ct("g_rem")
        b_c = Ct("b_c"); km = Ct("km")
        AR = sb("AR", [64, 4, 2, L], F32R)
        bh = Ct("bh", F32R); kh = Ct("kh", F32R); rkp = Ct("rkp", F32R)
        Bt = Ct("Bt"); Kt = Ct("Kt")
        gLt = sb("gLt", [64, 4, 2], F32)
        NU = 4
        A1sb = [sb("A1sb%d" % i, [128, 256], F32R) for i in range(NU)]
        A2sb = [sb("A2sb%d" % i, [128, 256], F32R) for i in range(NU)]
        QMP = [[sb("QMP%d_%d" % (i, j), [128, 384], F32R) for j in range(2)] for i in range(NU)]
        tok3 = [sb("tok3_%d" % i, [128, 3, 64], F32R) for i in range(NU)]
        G1sb = [sb("G1sb%d" % i, [64, 128], F32R) for i in range(NU)]
        AVsb = [sb("AVsb%d" % i, [128, 64], F32R) for i in range(NU)]
        P2sb = [sb("P2sb%d" % i, [128, 64], F32) for i in range(NU)]
        Usb = [sb("Usb%d" % i, [128, 64], F32R) for i in range(NU)]
        Ssb = [[sb("Ssb%d_%d" % (h, j), [64, 64], F32R) for j in range(2)] for h in range(4)]
        zer = sb("zer", [64, 64], F32)
        ysb = sb("ysb", [128, 4, 64], F32)
        bst = sb("bst", [128, 4, 6], F32)
        mv = sb("mv", [128, 4, 2], F32)
        rs4 = sb("rs4", [128, 4], F32)
        yn = sb("yn", [128, 256], F32)
        rks = sb("rks", [128, 4], F32)
        ogo = [sb("ogo%d" % i, [128, 256], F32) for i in range(1)] * 2
        psb = [st.enter_context(nc.psum_tensor("B_ps%d" % i, [128, 512], F32)) for i in range(8)]
        psP = psb[0:2]
        psT = psb[1]
        psT_bf = psT[:].bitcast(BF16)
        psN = psb[2:6]
        psAs = psb[6:8]

        s.dma("sp", gbc_sb[:], gbc[:], writes=["gbc"])
        s.dma("sp", sel_sb[:], sel[:], writes=["sel"])
        s.dma("sp", idb[:], identb[:], writes=["idb"])
        s.dma("sp", idf[:], identf[:], writes=["idf"])
        s.dma("sp", mk[:], masks[:], writes=["mk"])
        s.dma("sp", mix_sb[:], mixT[:], writes=["mix"])
        s.dma("sp", cp[:], cpar[:], writes=["cp"])
        s.dma("sp", lnx_sb[:], lnx[:], writes=["lnx"])
        s.dma("sp", w2st[:], w2a2[:], writes=["w2st"])
        s.copy("pool", w2a2_sb[:], w2st[:], reads=["w2st"], writes=["w2a2"])
        s.ts("dve", onem_sb[:], mix_sb[:], -1.0, 1.0, ALU.mult, ALU.add, reads=["mix"], writes=["onem"])
        s.ts("dve", onemka[:], cp[:, 3, :], -1.0, 1.0, ALU.mult, ALU.add, reads=["cp"], writes=["onemka"])
        s.copy("dve", rk2[:], cp[:, 4, :].unsqueeze(2).to_broadcast([64, 4, 2]), reads=["cp"], writes=["rk2"])
        s.memset("pool", ones_f[:], 1.0, writes=["ones_f"])
        s.copy("dve", ones_r[:], ones_f[:, 0:64], reads=["ones_f"], writes=["ones_r"])
        s.memset("pool", eps_t[:, 0:1], 1e-6, writes=["eps"])
        s.memset("pool", eps_t[:, 1:2], 64e-5, writes=["eps"])
        s.memset("pool", eps_t[:, 2:3], 1e-30, writes=["eps"])
        s.memset("pool", hx[:], 0.0, writes=[("hx", 0), ("hx", 1), "hx0"])
        s.memset("pool", zer[:], 0.0, writes=["zer"])
        stg = [(x1t, "wst"), (xin[0], ("xin", 0)), (xin[1], ("xin", 1))]
        sk = [0]

        def nstg():
            t_, k_ = stg[sk[0] % 3]
            sk[0] += 1
            return t_, k_

        for c in range(8):
            w_, k_ = nstg()
            s.dma("sp", w_[:], wout0[c * 128:(c + 1) * 128, :], writes=[k_])
            s.copy("pool", wo_sb[:, c, :], w_[:], reads=[k_], writes=["wo_sb"])
        for c in range(8):
            w_, k_ = nstg()
            s.dma("sp", w_[:], w4[c * 128:(c + 1) * 128, :], writes=[k_])
            for n in range(4):
                s.ts("dve", wa_sb[:, c, n * 256:(n + 1) * 256], w_[:, n * 256:(n + 1) * 256], onem_sb[:, c, n:n + 1], None,
                     ALU.mult, reads=[k_, "onem"], writes=["wa_sb"])
                s.ts("pool", wb_sb[:, c, n * 256:(n + 1) * 256], w_[:, n * 256:(n + 1) * 256], mix_sb[:, c, n:n + 1], 1.0,
                     ALU.mult, ALU.mult, reads=[k_, "mix"], writes=["wb_sb"])
        for c in range(8):
            w_, k_ = nstg()
            s.dma("sp", w_[:, 0:128], wl[c * 128:(c + 1) * 128, :], writes=[k_])
            for n in range(2):
                s.ts("dve", wla_sb[:, c, n * 64:(n + 1) * 64], w_[:, n * 64:(n + 1) * 64], onem_sb[:, c, 4 + n:5 + n], None,
                     ALU.mult, reads=[k_, "onem"], writes=["wla_sb"])
                s.ts("pool", wlb_sb[:, c, n * 64:(n + 1) * 64], w_[:, n * 64:(n + 1) * 64], mix_sb[:, c, 4 + n:5 + n], 1.0,
                     ALU.mult, ALU.mult, reads=[k_, "mix"], writes=["wlb_sb"])
        for h in range(4):
            s.copy("dve", Ssb[h][0][:], zer[:], reads=["zer"], writes=[("S", h, 0)])

        cnt = {"x": 0, "p": 0, "u": 0, "o": 0}
        allh = lambda n, gp=None: [((n, h) if gp is None else (n, h, gp)) for h in range(4)]

        def nextp():
            p = cnt["p"] % 2
            cnt["p"] += 1
            return p

        def stage_Pa(Gi):
            t0 = Gi * GT
            if Gi > 0:
                s.copy("pool", hx[:, :, 3:4], lastcol[:, :, 0:1], reads=["lastcol"], writes=["hx0"])
            for tt in range(NCH):
                xs = cnt["x"] % NX
                cnt["x"] += 1
                tok = t0 + tt * 128
                s.dma("sp", xin[xs][:], x[tok:tok + 128, :], writes=[("xin", xs)])
                s.dma("pool", ogf[xs][:], ogall[tok // 1024, :, tok % 1024:tok % 1024 + 128].rearrange("(c p) t -> p c t", p=128), writes=["ogf"])
                s.copy("act", ogin[xs][:], ogf[xs][:], reads=["ogf"], writes=["ogin"])
                yield
                for half in range(2):
                    ps = nextp()
                    for c in range(8):
                        s.mm(psP[ps][:, :], ogin[xs][:, c, :], wo_sb[:, c, half * 512:(half + 1) * 512], start=(c == 0), stop=(c == 7),
                             reads=["ogin", "wo_sb"], writes=[("psP", ps)])
                    yield
                    s.tt("dve", x1t[:, half * 512:(half + 1) * 512], psP[ps][:, :], xin[xs][:, half * 512:(half + 1) * 512], ALU.add,
                         reads=[("psP", ps), ("xin", xs)], writes=[("x1t", half), "wst"])
                    yield
                s.op("dve", lambda e, tt=tt: e.scalar_tensor_tensor(
                    out=junk[:], in0=x1t[:], scalar=1.0, in1=x1t[:],
                    op0=ALU.mult, op1=ALU.mult, accum_out=ss[:, tt:tt + 1]),
                    reads=[("x1t", 0), ("x1t", 1)], writes=["hn", ("ss", tt)])
                s.act(rstd[:, tt:tt + 1], ss[:, tt:tt + 1], AF.Ln, scale=1.0 / D, bias=eps_t[:, 0:1],
                      reads=[("ss", tt), "eps"], writes=[("rstd", tt)])
                s.act(rstd[:, tt:tt + 1], rstd[:, tt:tt + 1], AF.Exp, scale=-0.5,
                      reads=[("rstd", tt)], writes=[("rstd", tt)])
                s.stt(hn[:], x1t[:], rstd[:, tt:tt + 1], gbc_sb[:], ALU.mult, ALU.mult,
                      reads=[("x1t", 0), ("x1t", 1), ("rstd", tt), "gbc"], writes=["hn"])
                yield
                for c in range(8):
                    s.tr(psT_bf[:, c * 128:(c + 1) * 128], hn[:, c * 128:(c + 1) * 128], idb[:],
                         reads=["hn", "idb"], writes=[("psP", 1)])
                yield
                s.copy("act", hx[:, :, 4 + tt * 128:4 + (tt + 1) * 128],
                       psT_bf.rearrange("p (c t) -> p c t", c=8), reads=[("psP", 1)], writes=[("hx", tt)])
                yield
            s.copy("pool", lastcol[:, :, 0:1], hx[:, :, GT + 3:GT + 4], reads=[("hx", NCH - 1)], writes=["lastcol"])
            yield

        def stage_Pb(Gi):
            gp = Gi % 2
            r_c, k_c, sgw, a_c, lhid, v_r, sg_t = r_c2[gp], k_c2[gp], sgw2[gp], a_c2[gp], lhid2[gp], v_r2[gp], sg_t2[gp]
            hkeys = [("hx", tt) for tt in range(NCH)] + ["hx0"]

            def proj_cm(wa, wb, col0, ncols, ps_):
                out_ps = psP[ps_][0:ncols, 0:GT]
                for c in range(8):
                    s.mm(out_ps, wa[:, c, col0:col0 + ncols], hx[:, c, 4:GT + 4], start=(c == 0), stop=False,
                         reads=hkeys + ["wa_sb", "wla_sb"], writes=[("psP", ps_)])
                for c in range(8):
                    s.mm(out_ps, wb[:, c, col0:col0 + ncols], hx[:, c, 3:GT + 3], start=False, stop=(c == 7),
                         reads=hkeys + ["wb_sb", "wlb_sb"], writes=[("psP", ps_)])
                return out_ps

            ps_ = nextp()
            proj_cm(wla_sb, wlb_sb, 0, 128, ps_)
            s.act(lhid[:, 0, :], psP[ps_][0:64, 0:GT], AF.Tanh, reads=[("psP", ps_)], writes=[("lhid", 0, gp)])
            s.copy("act", lhid[:, 1, :], psP[ps_][64:128, 0:GT], reads=[("psP", ps_)], writes=[("lhid", 1, gp)])
            yield
            for h in range(4):
                ps_ = nextp()
                o_ = psP[ps_][0:64, 0:GT]
                s.mm(o_, w2a2_sb[:, 0, h * 64:(h + 1) * 64], lhid[:, 0, :], reads=["w2a2", ("lhid", 0, gp)], writes=[("psP", ps_)])
                s.act(sgw[:, h, :], o_, AF.Sigmoid, bias=cp[:, 0, h:h + 1], reads=[("psP", ps_), "cp"], writes=[("sgw", h, gp)])
                yield
                ps_ = nextp()
                o_ = psP[ps_][0:64, 0:GT]
                s.mm(o_, w2a2_sb[:, 1, h * 64:(h + 1) * 64], lhid[:, 1, :], reads=["w2a2", ("lhid", 1, gp)], writes=[("psP", ps_)])
                s.act(a_c[:, h, :], o_, AF.Sigmoid, bias=cp[:, 1, h:h + 1], reads=[("psP", ps_), "cp"], writes=[("a_c", h, gp)])
                yield
            for hp in range(2):
                ps_ = nextp()
                proj_cm(wa_sb, wb_sb, hp * 128, 128, ps_)
                s.copy("dve", r_c[:, 2 * hp, :], psP[ps_][0:64, 0:GT], reads=[("psP", ps_)], writes=[("r_c", 2 * hp, gp)])
                s.copy("dve", r_c[:, 2 * hp + 1, :], psP[ps_][64:128, 0:GT], reads=[("psP", ps_)], writes=[("r_c", 2 * hp + 1, gp)])
                yield
                ps_ = nextp()
                proj_cm(wa_sb, wb_sb, 256 + hp * 128, 128, ps_)
                s.copy("act", k_c[:, 2 * hp, :], psP[ps_][0:64, 0:GT], reads=[("psP", ps_)], writes=[("k_c", 2 * hp, gp)])
                s.copy("act", k_c[:, 2 * hp + 1, :], psP[ps_][64:128, 0:GT], reads=[("psP", ps_)], writes=[("k_c", 2 * hp + 1, gp)])
                yield
            for tt in range(NCH):
                ps_ = nextp()
                for c in range(8):
                    s.mm(psP[ps_][:, :], hx[:, c, 4 + tt * 128:4 + (tt + 1) * 128], wa_sb[:, c, 512:1024], start=(c == 0), stop=False,
                         reads=hkeys + ["wa_sb"], writes=[("psP", ps_)])
                for c in range(8):
                    s.mm(psP[ps_][:, :], hx[:, c, 3 + tt * 128:3 + (tt + 1) * 128], wb_sb[:, c, 512:1024], start=False, stop=(c == 7),
                         reads=hkeys + ["wb_sb"], writes=[("psP", ps_)])
                s.copy("dve", v_r[:, tt, :], psP[ps_][:, 0:256], reads=[("psP", ps_)], writes=[("v_r", tt, gp)])
                s.act(sg_t[:, tt, :], psP[ps_][:, 256:512], AF.Sigmoid, reads=[("psP", ps_)], writes=[("sg_t", tt, gp)])
                s.tt("dve", sg_t[:, tt, :], sg_t[:, tt, :], psP[ps_][:, 256:512], ALU.mult, reads=[("psP", ps_), ("sg_t", tt, gp)], writes=[("sg_t", tt, gp)])
                yield

        def chunk_elem(j, gp):
            r_c, k_c, sgw, a_c, lhid, v_r, sg_t = r_c2[gp], k_c2[gp], sgw2[gp], a_c2[gp], lhid2[gp], v_r2[gp], sg_t2[gp]
            cs = slice(j * L, (j + 1) * L)
            bc = lambda i: cp[:, i, :].unsqueeze(2).to_broadcast([64, 4, L])
            s.tt("dve", kk[:], k_c[:, :, cs], bc(2), ALU.mult, reads=allh("k_c", gp) + ["cp"], writes=["kk"])
            yield
            s.tt("dve", tmp2[:], kk[:], kk[:], ALU.mult, reads=["kk"], writes=["tmp2"])
            yield
            for h in range(4):
                ps_ = nextp()
                o_ = psP[ps_][0:64, 0:L]
                s.mm(o_, ones_r[:, :], tmp2[:, h, :], reads=["ones_r", "tmp2"], writes=[("psP", ps_)])
                yield
                s.act(tmp1[:, h, :], o_, AF.Ln, bias=eps_t[0:64, 2:3], reads=[("psP", ps_), "eps"], writes=["tmp1"])
                yield
            s.act(tmp1[:], tmp1[:], AF.Exp, scale=-0.5, reads=["tmp1"], writes=["tmp1"])
            yield
            s.tt("dve", kk[:], kk[:], tmp1[:], ALU.mult, reads=["kk", "tmp1"], writes=["kk"])
            yield
            for h in range(4):
                s.op("dve", lambda e, h=h: e.tensor_tensor_scan(
                    out=cum[:, h, :], data0=ones_f[:, 0:L], data1=sgw[:, h, cs],
                    initial=0.0, op0=ALU.mult, op1=ALU.add), reads=[("sgw", h, gp), "ones_f"], writes=["cum"])
                yield
            s.tt("dve", ex[:], cum[:], sgw[:, :, cs], ALU.subtract, reads=["cum"] + allh("sgw", gp), writes=["ex"])
            yield
            s.tt("dve", tmp1[:], cum[:], cum[:, :, L - 1:L].to_broadcast([64, 4, L]), ALU.subtract, reads=["cum", "tmp1"], writes=["tmp1"])
            yield
            s.act(g_rem[:], tmp1[:], AF.Exp, scale=CDEC, reads=["tmp1"], writes=["g_rem"])
            yield
            s.act(g_incl[:], cum[:], AF.Exp, scale=-CDEC, reads=["cum"], writes=["g_incl"])
            yield
            s.act(g_inv[:], cum[:], AF.Exp, scale=CDEC, reads=["cum"], writes=["g_inv"])
            yield
            s.act(g_excl[:], ex[:], AF.Exp, scale=-CDEC, reads=["ex"], writes=["g_excl"])
            yield
            s.copy("act", gLt[:, :, 0:1], g_incl[:, :, L - 1:L], reads=["g_incl"], writes=["gLt"])
            yield
            s.stt(AR[:, :, 0, :], kk[:], -1.0, g_excl[:], ALU.mult, ALU.mult, reads=["kk", "g_excl"], writes=["AR_at"])
            yield
            s.tt("dve", b_c[:], kk[:], a_c[:, :, cs], ALU.mult, reads=["kk"] + allh("a_c", gp), writes=["b_c"])
            yield
            s.tt("dve", bh[:], b_c[:], g_inv[:], ALU.mult, reads=["b_c", "g_inv"], writes=["bh"])
            yield
            s.tt("dve", Bt[:], b_c[:], g_rem[:], ALU.mult, reads=["b_c", "g_rem"], writes=["Bt"])
            yield
            s.tt("dve", tmp1[:], a_c[:, :, cs], bc(3), ALU.mult, reads=allh("a_c", gp) + ["cp", "tmp1"], writes=["tmp1"])
            yield
            s.tt("dve", tmp1[:], tmp1[:], onemka[:].unsqueeze(2).to_broadcast([64, 4, L]), ALU.add, reads=["tmp1", "onemka"], writes=["tmp1"])
            yield
            s.tt("dve", km[:], k_c[:, :, cs], tmp1[:], ALU.mult, reads=allh("k_c", gp) + ["tmp1"], writes=["km"])
            yield
            s.tt("dve", kh[:], km[:], g_inv[:], ALU.mult, reads=["km", "g_inv"], writes=["kh"])
            yield
            s.tt("dve", Kt[:], km[:], g_rem[:], ALU.mult, reads=["km", "g_rem"], writes=["Kt"])
            yield
            s.tt("dve", AR[:, :, 1, :], r_c[:, :, cs], g_incl[:], ALU.mult, reads=allh("r_c", gp) + ["g_incl"], writes=["AR_rt"])
            yield
            s.tt("dve", rkp[:], r_c[:, :, cs], km[:], ALU.mult, reads=allh("r_c", gp) + ["km"], writes=["rkp"])
            yield

        def unit_pre(j, h, u, gp):
            r_c, k_c, sgw, a_c, lhid, v_r, sg_t = r_c2[gp], k_c2[gp], sgw2[gp], a_c2[gp], lhid2[gp], v_r2[gp], sg_t2[gp]
            AR2 = AR[:, h, :, :].rearrange("c a t -> c (a t)")
            pa = u % 2
            psA_ = psAs[pa]
            ka = ("psA", pa)
            kn = ("psN", u)
            s.mm(psA_[:, 0:256], bh[:, h, :], AR2, reads=["bh", "AR_at", "AR_rt"], writes=[ka])
            s.mm(psA_[:, 256:512], kh[:, h, :], AR2, reads=["kh", "AR_at", "AR_rt"], writes=[ka])
            s.mm(psN[u][:, 256:384], AR[:, h, 0, :], bh[:, h, :], reads=["bh", "AR_at"], writes=[kn])
            yield
            mUI = mk[:, 0:2, :].rearrange("p a t -> p (a t)")
            s.tt("dve", A1sb[u][:], psA_[:, 0:256], mUI, ALU.mult, reads=[ka, "mk"], writes=[("A1", u)])
            s.tt("dve", QMP[u][0][:, 0:128], psA_[:, 0:128], mk[:, 0, :], ALU.mult, reads=[ka, "mk"], writes=[("QMP", u, 0)])
            s.tt("dve", QMP[u][0][:, 256:384], psN[u][:, 256:384], mk[:, 2, :], ALU.mult, reads=[kn, "mk"], writes=[("QMP", u, 0)])
            s.tt("dve", A2sb[u][:], psA_[:, 256:512], mUI, ALU.mult, reads=[ka, "mk"], writes=[("A2", u)])
            s.tt("dve", QMP[u][0][:, 128:256], A1sb[u][:, 0:128].bitcast(F32), idf[:], ALU.add, reads=[("A1", u), "idf"], writes=[("QMP", u, 0)])
            s.tt("dve", QMP[u][1][:, 128:256], A1sb[u][:, 0:128].bitcast(F32), idf[:], ALU.add, reads=[("A1", u), "idf"], writes=[("QMP", u, 1)])
            yield
            cur = 0
            for it in range(7):
                nxt = 1 - cur
                last = (it == 6)
                T_ = QMP[u][cur]
                Qc = T_[:, 0:128]
                Pc = T_[:, 256:384]
                kc_ = ("QMP", u, cur)
                if it == 0:
                    s.mm(psN[u][:, 0:128], Pc, Qc, reads=[kc_], writes=[kn])
                elif not last:
                    s.mm(psN[u][:, 0:256], Pc, T_[:, 0:256], reads=[kc_], writes=[kn])
                else:
                    s.mm(psN[u][:, 128:256], Pc, T_[:, 128:256], reads=[kc_], writes=[kn])
                if not last:
                    s.mm(psN[u][:, 256:384], Qc, Pc, reads=[kc_], writes=[kn])
                yield
                if not last:
                    s.copy("act", QMP[u][nxt][:].rearrange("p (a t) -> p a t", a=3)[:, 0:3:2, :],
                           psN[u][:, 0:384].rearrange("p (a t) -> p a t", a=3)[:, 0:3:2, :], reads=[kn], writes=[("QMP", u, nxt)])
                if it >= 1:
                    s.tt("dve", QMP[u][nxt][:, 128:256], psN[u][:, 128:256], T_[:, 128:256].bitcast(F32), ALU.add,
                         reads=[kn, kc_], writes=[("QMP", u, nxt)])
                yield
                cur = nxt
            M = QMP[u][cur][:, 128:256]
            Mkey = ("QMP", u, cur)
            psX = psN[u]
            s.tr(psX[:, 0:64], AR[:, h, 0, :].bitcast(F32), idf[0:64, 0:64], reads=["AR_at", "idf"], writes=[kn])
            s.tr(psX[:, 64:128], Bt[:, h, :], idf[0:64, 0:64], reads=["Bt", "idf"], writes=[kn])
            s.tr(psX[:, 128:192], Kt[:, h, :], idf[0:64, 0:64], reads=["Kt", "idf"], writes=[kn])
            s.mm(psX[:, 320:384], A2sb[u][:, 0:128], v_r[:, j, h * 64:(h + 1) * 64], reads=[("A2", u), ("v_r", j, gp)], writes=[kn])
            yield
            s.copy("act", tok3[u][:].rearrange("p a k -> p (a k)"), psX[:, 0:192], reads=[kn], writes=[("tok3", u)])
            s.copy("act", AVsb[u][:], psX[:, 320:384], reads=[kn], writes=[("AV", u)])
            yield
            s.mm(psX[0:64, 192:320], tok3[u][:, 0, :], M, reads=[("tok3", u), Mkey], writes=[kn])
            s.mm(psX[:, 384:448], M, AVsb[u][:], reads=[Mkey, ("AV", u)], writes=[kn])
            yield
            s.copy("act", G1sb[u][:], psX[0:64, 192:320], reads=[kn], writes=[("G1", u)])
            s.copy("act", P2sb[u][:], psX[:, 384:448], reads=[kn], writes=[("P2", u)])
            yield

        def unit_chain(Gi, j, h, u):
            gp = Gi % 2
            r_c, k_c, sgw, a_c, lhid, v_r, sg_t = r_c2[gp], k_c2[gp], sgw2[gp], a_c2[gp], lhid2[gp], v_r2[gp], sg_t2[gp]
            cidx = Gi * NCH + j
            so = cidx % 2
            sn = 1 - so
            vv = v_r[:, j, h * 64:(h + 1) * 64]
            psC = psN[u]
            Ups = psC[:, 448:512]
            Sps = psC[0:64, 0:64]
            Yps = psC[:, 64:128]
            Rps = psC[:, 128:130]
            key = ("psN", u)
            s.mm(Ups, G1sb[u][:], Ssb[h][so][:], reads=[("G1", u), ("S", h, so)], writes=[key])
            yield
            s.tt("dve", Usb[u][:], Ups, P2sb[u][:], ALU.add, reads=[key, ("P2", u)], writes=[("U", u)])
            yield
            s.mm(Sps, tok3[u][:, 2, :], vv, start=True, stop=False, reads=[("tok3", u), ("v_r", j, gp)], writes=[key])
            s.mm(Sps, tok3[u][:, 1, :], Usb[u][:], start=False, stop=True, reads=[("tok3", u), ("U", u)], writes=[key])
            s.mm(Yps, AR[:, h, 1, :], Ssb[h][so][:], start=True, stop=False, reads=["AR_rt", ("S", h, so)], writes=[key])
            s.mm(Yps, A1sb[u][:, 128:256], Usb[u][:], start=False, stop=False, reads=[("A1", u), ("U", u)], writes=[key])
            s.mm(Yps, A2sb[u][:, 128:256], vv, start=False, stop=True, reads=[("A2", u), ("v_r", j, gp)], writes=[key])
            s.mm(Rps, rkp[:, h, :], rk2[:, h, :], reads=["rkp", "rk2"], writes=[key])
            yield
            s.stt(Ssb[h][sn][:], Ssb[h][so][:].bitcast(F32), gLt[:, h, 0:1], Sps, ALU.mult, ALU.add,
                  reads=[("S", h, so), "gLt", key], writes=[("S", h, sn)])
            s.copy("act", ysb[:, h, :], Yps, reads=[key], writes=[("ysb", h)])
            s.copy("act", rks[:, h:h + 1], Rps[:, 0:1], reads=[key], writes=[("rks", h)])
            yield
            s.op("dve", lambda e, h=h: e.bn_stats(out=bst[:, h, :], in_=ysb[:, h, :]), reads=[("ysb", h)], writes=[("bst", h)])
            s.op("dve", lambda e, h=h: e.bn_aggr(out=mv[:, h, :], in_=bst[:, h, :]), reads=[("bst", h)], writes=[("mv", h)])
            yield

        def rr(gens):
            gens = list(gens)
            while gens:
                for g_ in list(gens):
                    try:
                        next(g_)
                    except StopIteration:
                        gens.remove(g_)

        def chunk_out(Gi, j):
            gp = Gi % 2
            r_c, k_c, sgw, a_c, lhid, v_r, sg_t = r_c2[gp], k_c2[gp], sgw2[gp], a_c2[gp], lhid2[gp], v_r2[gp], sg_t2[gp]
            t0 = Gi * GT + j * L
            oo = cnt["o"] % 2
            cnt["o"] += 1
            s.act(rs4[:], mv[:, :, 1], AF.Ln, bias=eps_t[:, 1:2], reads=allh("mv") + ["eps"], writes=["rs4"])
            yield
            s.act(rs4[:], rs4[:], AF.Exp, scale=-0.5, reads=["rs4"], writes=["rs4"])
            yield
            for h in range(4):
                s.ts("dve", yn[:, h * 64:(h + 1) * 64], ysb[:, h, :], mv[:, h, 0:1], rs4[:, h:h + 1], ALU.subtract, ALU.mult,
                     reads=[("ysb", h), ("mv", h), "rs4"], writes=["yn"])
                yield
            s.tt("dve", yn[:], yn[:], lnx_sb[:, 0, :], ALU.mult, reads=["yn", "lnx"], writes=["yn"])
            yield
            s.tt("dve", yn[:], yn[:], lnx_sb[:, 1, :], ALU.add, reads=["yn", "lnx"], writes=["yn"])
            yield
            for h in range(4):
                s.stt(ogo[oo][:, h * 64:(h + 1) * 64], v_r[:, j, h * 64:(h + 1) * 64].bitcast(F32), rks[:, h:h + 1], yn[:, h * 64:(h + 1) * 64],
                      ALU.mult, ALU.add, reads=[("v_r", j, gp), ("rks", h), "yn"], writes=["ogo"])
                yield
            s.tt("dve", ogo[oo][:], ogo[oo][:], sg_t[:, j, :], ALU.mult, reads=["ogo", ("sg_t", j, gp)], writes=["ogo"])
            yield
            for jj in range(4):
                s.act(ogo4[oo][:, jj, :], ogo[oo][:], AF.Copy, scale=sel_sb[:, jj:jj + 1],
                      reads=["ogo", "sel"], writes=["ogo4"])
                yield
            s.dma("sp", og1buf[t0 // 1024, t0 % 1024:t0 % 1024 + 128, :].rearrange("t (j f) -> t j f", j=4), ogo4[oo][:], reads=["ogo4"], writes=[("og1buf", t0 // 1024)])
            yield
            if (t0 + 128) % 1024 == 0:
                ck_ = t0 // 1024
                s.cc(lambda e, ck_=ck_: e.collective_compute("AllReduce", ALU.add, replica_groups=GROUPS,
                                                             ins=[og1buf[ck_].opt()], outs=[og1all[ck_].opt()]),
                     reads=[("og1buf", ck_)])
                yield

        def chain_gens(gs):
            for g_ in gs:
                yield from g_

        def rr2(gens, bg):
            gens = list(gens)
            while gens:
                for g_ in list(gens):
                    try:
                        next(g_)
                    except StopIteration:
                        gens.remove(g_)
                if bg[0] is not None:
                    try:
                        next(bg[0])
                    except StopIteration:
                        bg[0] = None

        rr([stage_Pa(0)])
        rr([stage_Pb(0)])
        prev_out = []
        for Gi in range(n_groups):
            gp = Gi % 2
            bg = [chain_gens([stage_Pa(Gi + 1), stage_Pb(Gi + 1)])] if Gi + 1 < n_groups else [None]
            for j in range(NCH):
                rr([chunk_elem(j, gp)] + prev_out)
                prev_out = []
                gens = [unit_pre(j, h, h, gp) for h in range(4)]
                for pair in ((0, 1), (2, 3)):
                    for _ in range(2):
                        for u_ in pair:
                            next(gens[u_])
                rr2(gens, bg)
                rr2([unit_chain(Gi, j, h, h) for h in range(4)], bg)
                prev_out = [chunk_out(Gi, j)]
            if bg[0] is not None:
                rr([bg[0]])
        rr(prev_out)
        s.emit(nc, limit=limit, semstack=semstack, tag='B')


def consts_B():
    r = np.arange(128)[:, None]
    c = np.arange(128)[None, :]
    masks = np.stack([(r < c), (r <= c), (r > c)], axis=1).astype(np.float32)
    return dict(masks=np.ascontiguousarray(masks), identf=np.eye(128, dtype=np.float32), identb=np.eye(128).astype(bf))


def inputs_B(inp, b, g):
    cs = slice(g * 256, (g + 1) * 256)
    w_in = inp["rwkv_w_in"]
    w4 = np.concatenate([w_in[n][:, cs] for n in range(4)], axis=1)
    mixT = np.ascontiguousarray(inp["rwkv_mix"].reshape(6, 8, 128).transpose(2, 1, 0))
    hd = lambda p: np.asarray(p).reshape(-1)[cs].reshape(4, 64).T
    cpar = np.stack([hd(inp["rwkv_w0"]), hd(inp["rwkv_a0"]), hd(inp["rwkv_k_k"]), hd(inp["rwkv_k_a"]), hd(inp["rwkv_r_k"])], axis=1)
    lnx = np.stack([np.broadcast_to(inp["rwkv_lnx_g"][cs][None, :], (128, 256)),
                    np.broadcast_to(inp["rwkv_lnx_b"][cs][None, :], (128, 256))], axis=1)
    m = dict(wout0=np.ascontiguousarray(inp["moba_w_out"]),
             gbc=np.ascontiguousarray(np.broadcast_to(inp["rwkv_norm_g"][None, :], (128, 1024))),
             mixT=mixT, w4=np.ascontiguousarray(w4),
             wl=np.ascontiguousarray(np.concatenate([inp["rwkv_w1"], inp["rwkv_a1"]], axis=1)),
             w2a2=np.ascontiguousarray(np.stack([inp["rwkv_w2"][:, cs], inp["rwkv_a2"][:, cs]], axis=1)),
             cpar=np.ascontiguousarray(cpar.astype(np.float32)), lnx=np.ascontiguousarray(lnx.astype(np.float32)))
    m.update(consts_B())
    return m


D = 1024
NT = 64


def phase_C1(nc, semstack, ogall, og1all, ssbuf, x2dram):
    dram = lambda n, sh, dt, kind="ExternalInput": nc.dram_tensor("c_" + n, sh, dt, kind=kind).ap()
    xc = dram("xc", [S, 256], F32)
    w0c = dram("w0c", [D, 256], F32)
    w1c = dram("w1c", [D, 256], F32)
    identb = dram("identb", [128, 128], BF16)
    s = Sched()
    with ExitStack() as st:
        sb = lambda name, shape, dt: st.enter_context(nc.sbuf_tensor("C_" + name, shape, dt))
        xin = [sb("xin%d" % i, [128, 256], F32) for i in range(3)]
        o1in = [sb("o1in%d" % i, [128, D], F32) for i in range(3)]
        ogf = [sb("ogf%d" % i, [128, 8, 128], F32) for i in range(3)]
        ogin = [sb("ogin%d" % i, [128, 8, 128], BF16) for i in range(2)]
        o1b = sb("o1b", [128, D], BF16)
        o1T = [sb("o1T%d" % i, [128, 8, 128], BF16) for i in range(2)]
        x2t = [sb("x2t%d" % i, [128, 256], F32) for i in range(2)]
        junk = sb("junk", [128, 256], BF16)
        wo0 = sb("wo0", [128, 8, 256], BF16)
        wo1 = sb("wo1", [128, 8, 256], BF16)
        wst = sb("wst", [128, 256], F32)
        idb = sb("idb", [128, 128], BF16)
        ssq = sb("ssq", [128, NT], F32)
        psb = [st.enter_context(nc.psum_tensor("C_ps%d" % i, [128, 512], F32)) for i in range(4)]
        psP = psb[0:2]
        psT = psb[2]
        psT_bf = psT[:].bitcast(BF16)
        s.dma("sp", idb[:], identb[:], writes=["idb"])
        cstg = [(wst, "wst"), (xin[0], ("xin", 0)), (xin[1], ("xin", 1)), (xin[2], ("xin", 2))]
        for c in range(8):
            w_, k_ = cstg[c % 4]
            s.dma("sp", w_[:], w0c[c * 128:(c + 1) * 128, :], writes=[k_])
            s.copy("act", wo0[:, c, :], w_[:], reads=[k_], writes=["wo0"])
        for c in range(8):
            w_, k_ = cstg[c % 4]
            s.dma("sp", w_[:], w1c[c * 128:(c + 1) * 128, :], writes=[k_])
            s.copy("dve", wo1[:, c, :], w_[:], reads=[k_], writes=["wo1"])
        def c_stage1(ti):
            xs = ti % 3
            tok = ti * 128
            ck, c0 = tok // 1024, tok % 1024
            s.dma("sp", xin[xs][:], xc[tok:tok + 128, :], writes=[("xin", xs)])
            s.dma("sp", o1in[xs][:], og1all[ck, c0:c0 + 128, :], writes=[("o1in", xs)])
            s.dma("pool", ogf[xs][:], ogall[ck, :, c0:c0 + 128].rearrange("(c p) t -> p c t", p=128), writes=[("ogf", xs)])

        def c_stage2(ti):
            xs = ti % 3
            b2 = ti % 2
            s.copy("dve", ogin[b2][:], ogf[xs][:], reads=[("ogf", xs)], writes=[("ogin", b2)])
            s.copy("act", o1b[:], o1in[xs][:], reads=[("o1in", xs)], writes=["o1b"])
            for c in range(8):
                s.tr(psT_bf[:, c * 128:(c + 1) * 128], o1b[:, c * 128:(c + 1) * 128], idb[:], reads=["o1b", "idb"], writes=["psT"])
            s.copy("act", o1T[b2][:], psT_bf.rearrange("p (c t) -> p c t", c=8), reads=["psT"], writes=[("o1T", b2)])

        def c_stage3(ti):
            xs = ti % 3
            b2 = ti % 2
            tok = ti * 128
            ps = ti % 2
            for c in range(8):
                s.mm(psP[ps][:, 0:256], ogin[b2][:, c, :], wo0[:, c, :], start=(c == 0), stop=False,
                     reads=[("ogin", b2), "wo0"], writes=[("psP", ps)])
            for c in range(8):
                s.mm(psP[ps][:, 0:256], o1T[b2][:, c, :], wo1[:, c, :], start=False, stop=(c == 7),
                     reads=[("o1T", b2), "wo1"], writes=[("psP", ps)])
            s.tt("dve", x2t[b2][:], psP[ps][:, 0:256], xin[xs][:], ALU.add,
                 reads=[("psP", ps), ("xin", xs)], writes=[("x2t", b2)])
            s.op("dve", lambda e, b2=b2, ti=ti: e.scalar_tensor_tensor(
                out=junk[:], in0=x2t[b2][:], scalar=1.0, in1=x2t[b2][:], op0=ALU.mult, op1=ALU.mult, accum_out=ssq[:, ti:ti + 1]),
                reads=[("x2t", b2)], writes=["junk", "ssq"])
            s.dma("sp", x2dram[tok:tok + 128, :], x2t[b2][:], reads=[("x2t", b2)])

        for ti in range(NT + 2):
            if ti < NT:
                c_stage1(ti)
            if 1 <= ti <= NT:
                c_stage2(ti - 1)
            if ti >= 2:
                c_stage3(ti - 2)
        s.dma("sp", ssbuf[:, :], ssq[:], reads=["ssq"])
        s.emit(nc, semstack=semstack, tag='C')


def phase_C2(nc, semstack, ssall, x2dram):
    dram = lambda n, sh, dt, kind="ExternalInput": nc.dram_tensor("c_" + n, sh, dt, kind=kind).ap()
    gbc = dram("gbc", [128, 256], F32)
    out = dram("out", [S, 256], F32, kind="ExternalOutput")
    s = Sched()
    with ExitStack() as st:
        sb = lambda name, shape, dt: st.enter_context(nc.sbuf_tensor("E_" + name, shape, dt))
        x2t = [sb("x2t%d" % i, [128, 256], F32) for i in range(3)]
        res = [sb("res%d" % i, [128, 256], F32) for i in range(3)]
        gbc_sb = sb("gbc_sb", [128, 256], F32)
        ssq = sb("ssq", [128, NT], F32)
        rstd = sb("rstd", [128, NT], F32)
        eps_t = sb("eps_t", [128, 1], F32)
        s.dma("sp", gbc_sb[:], gbc[:], writes=["gbc"])
        s.dma("sp", ssq[:], ssall[:, :], writes=["ssq"])
        s.memset("pool", eps_t[:], 1e-6, writes=["eps"])
        s.act(rstd[:], ssq[:], AF.Ln, scale=1.0 / D, bias=eps_t[:, 0:1], reads=["ssq", "eps"], writes=["rstd"])
        s.act(rstd[:], rstd[:], AF.Exp, scale=-0.5, reads=["rstd"], writes=["rstd"])
        for ti in range(NT):
            k = ti % 3
            tok = ti * 128
            s.dma("sp", x2t[k][:], x2dram[tok:tok + 128, :], writes=[("x2t", k)])
            s.stt(res[k][:], x2t[k][:], rstd[:, ti:ti + 1], gbc_sb[:], ALU.mult, ALU.mult,
                  reads=[("x2t", k), "rstd", "gbc"], writes=[("res", k)])
            s.dma("pool", out[tok:tok + 128, :], res[k][:], reads=[("res", k)])
        s.emit(nc, semstack=semstack, tag='E')


GROUPS = [[0, 1, 2, 3], [4, 5, 6, 7]]


def allreduce_block(nc, semstack, pairs, tag):
    sem = semstack.enter_context(nc.semaphore(tag + "_cc"))
    with nc.Block() as block:
        @block.gpsimd
        def _(g):
            for i, (src, dst) in enumerate(pairs):
                g.collective_compute("AllReduce", ALU.add, replica_groups=GROUPS,
                                     ins=[src.opt()], outs=[dst.opt()]).then_inc(sem)
                g.wait_ge(sem, i + 1)


def build_fused():
    nc = bass.Bass("TRN2", target_bir_lowering=False)
    x = nc.dram_tensor("x", [S, D], F32, kind="ExternalInput").ap()
    ogbuf = nc.dram_tensor("ogbuf", [8, 1024, 1024], F32).ap()
    ogall = nc.dram_tensor("ogall", [8, 1024, 1024], F32).ap()
    og1buf = nc.dram_tensor("og1buf", [8, 1024, 1024], F32).ap()
    og1all = nc.dram_tensor("og1all", [8, 1024, 1024], F32).ap()
    ssbuf = nc.dram_tensor("ssbuf", [128, 64], F32).ap()
    ssall = nc.dram_tensor("ssall", [128, 64], F32).ap()
    x2dram = nc.dram_tensor("x2dram", [S, 256], F32).ap()
    with ExitStack() as semstack:
        phase_A(nc, semstack, x, ogbuf, ogall)
        phase_B(nc, semstack, x, ogall, og1buf, og1all)
        phase_C1(nc, semstack, ogall, og1all, ssbuf, x2dram)
        allreduce_block(nc, semstack, [(ssbuf, ssall)], "x3")
        phase_C2(nc, semstack, ssall, x2dram)
    return nc


def kernel(**inp):
    inp = {k: np.asarray(v) for k, v in inp.items()}
    x = inp["x"]
    w_in = inp["moba_w_in"]
    nc = build_fused()
    gbcA = np.ascontiguousarray(np.broadcast_to(inp["moba_norm_g"][None, :], (128, 1024)))
    identb = np.eye(128).astype(bf)
    maps = []
    for c in range(8):
        b, g = c // 4, c % 4
        cs = slice(g * 256, (g + 1) * 256)
        m = dict(x=np.ascontiguousarray(x[b]))
        w4 = np.concatenate([w_in[:, k * 1024 + g * 256: k * 1024 + (g + 1) * 256] for k in range(4)], axis=1)
        sel = np.zeros((128, 4), np.float32)
        sel[:, g] = 1.0
        am = dict(gbc=gbcA, w4=np.ascontiguousarray(w4), sel=sel)
        am.update(consts_A(g))
        for k, v in am.items():
            m["a_" + k] = v
        bm = inputs_B(inp, b, g)
        bm["sel"] = sel
        for k, v in bm.items():
            m["b_" + k] = v
        m["c_xc"] = np.ascontiguousarray(x[b][:, cs])
        m["c_w0c"] = np.ascontiguousarray(inp["moba_w_out"][:, cs])
        m["c_w1c"] = np.ascontiguousarray(inp["rwkv_w_out"][:, cs])
        m["c_identb"] = identb
        m["c_gbc"] = np.ascontiguousarray(np.broadcast_to(inp["final_norm_g"][cs][None, :], (128, 256)))
        maps.append(m)
    res = run_bass_kernel_spmd(nc, maps, core_ids=list(range(8))).results
    out = np.zeros((2, 8192, 1024), np.float32)
    for c in range(8):
        b, g = c // 4, c % 4
        out[b, :, g * 256:(g + 1) * 256] = np.asarray(res[c]["c_out"])
    return out
```

```python
import numpy as np
import ml_dtypes
from concourse.bass_utils import run_bass_kernel_spmd
import concourse.bass as bass
import concourse.mybir as mybir
from contextlib import ExitStack

F32 = mybir.dt.float32
BF16 = mybir.dt.bfloat16
AF = mybir.ActivationFunctionType
ALU = mybir.AluOpType
AX = mybir.AxisListType

ENGS = ("pe", "act", "dve", "pool", "sp")
DMA_POOLS = {"sp": (0, 8), "pool": (8, 6)}
N_DMA_SEMS = 14


class Op:
    __slots__ = ("eng", "fn", "deps", "signal", "sig_idx", "is_dma", "dma_sem", "dma_val", "idx", "cc")

    def __init__(self, eng, fn, is_dma):
        self.eng = eng
        self.fn = fn
        self.deps = []
        self.signal = False
        self.sig_idx = 0
        self.is_dma = is_dma
        self.dma_sem = -1
        self.dma_val = 0
        self.cc = False


class Sched:
    def __init__(self):
        self.ops = []
        self.last_w = {}
        self.readers = {}
        self.n_dma_q = {}
        self.n_cc = 0

    def op(self, eng, fn, reads=(), writes=(), dma=False):
        o = Op(eng, fn, dma)
        o.idx = len(self.ops)
        _isps = lambda b: (isinstance(b, str) and b.startswith("ps")) or (isinstance(b, tuple) and isinstance(b[0], str) and b[0].startswith("ps"))
        writes = list(writes) + [b for b in reads if _isps(b)]
        reads = [b for b in reads if not _isps(b)]
        deps = {}
        for b in reads:
            w = self.last_w.get(b)
            if w is not None:
                deps[w.idx] = w
        for b in writes:
            w = self.last_w.get(b)
            if w is not None:
                deps[w.idx] = w
            for r in self.readers.get(b, ()):
                deps[r.idx] = r
        best = {}
        for d in deps.values():
            if d is o:
                continue
            if d.is_dma:
                o.deps.append(d)
                continue
            if d.eng == eng and eng == "pe" and not dma:
                continue
            if d.eng not in best or best[d.eng].idx < d.idx:
                best[d.eng] = d
        for d in best.values():
            o.deps.append(d)
            d.signal = True
        for b in reads:
            self.readers.setdefault(b, []).append(o)
        for b in writes:
            self.last_w[b] = o
            self.readers[b] = []
        if dma:
            base, n = DMA_POOLS[eng]
            k = self.n_dma_q.get(eng, 0)
            self.n_dma_q[eng] = k + 1
            o.dma_sem = base + k % n
            o.dma_val = 16 * (k // n + 1)
        self.ops.append(o)
        return o

    def cc(self, fn, reads=(), writes=()):
        o = self.op("pool", fn, reads, writes, dma=True)
        self.n_dma_q["pool"] -= 1
        o.cc = True
        o.dma_sem = N_DMA_SEMS + self.n_cc
        o.dma_val = 1
        self.n_cc += 1
        return o

    def mm(self, out, lhsT, rhs, start=True, stop=True, reads=(), writes=()):
        return self.op("pe", lambda e: e.matmul(out, lhsT=lhsT, rhs=rhs, start=start, stop=stop), reads, writes)

    def tr(self, out, in_, ident, reads=(), writes=()):
        return self.op("pe", lambda e: e.transpose(out, in_, ident), reads, writes)

    def act(self, out, in_, func, scale=None, bias=None, accum_out=None, reads=(), writes=()):
        kw = {}
        if scale is not None:
            kw["scale"] = scale
        if bias is not None:
            kw["bias"] = bias
        if accum_out is not None:
            kw["accum_out"] = accum_out
        return self.op("act", lambda e: e.activation(out=out, in_=in_, func=func, **kw), reads, writes)

    def dma(self, q, out, in_, reads=(), writes=()):
        return self.op(q, lambda e: e.dma_start(out=out, in_=in_), reads, writes, dma=True)

    def copy(self, eng, out, in_, reads=(), writes=()):
        if eng == "act":
            return self.op("act", lambda e: e.activation(out=out, in_=in_, func=AF.Copy), reads, writes)
        return self.op(eng, lambda e: e.tensor_copy(out=out, in_=in_), reads, writes)

    def memset(self, eng, ap, val, writes=()):
        return self.op(eng, lambda e: e.memset(ap, val), (), writes)

    def tt(self, eng, out, in0, in1, op, reads=(), writes=()):
        return self.op(eng, lambda e: e.tensor_tensor(out=out, in0=in0, in1=in1, op=op), reads, writes)

    def ts(self, eng, out, in0, s1, s2, op0, op1=None, reads=(), writes=(), accum_out=None):
        kw = {}
        if op1 is not None:
            kw["op1"] = op1
        if accum_out is not None:
            kw["accum_out"] = accum_out
        return self.op(eng, lambda e: e.tensor_scalar(out=out, in0=in0, scalar1=s1, scalar2=s2, op0=op0, **kw), reads, writes)

    def stt(self, out, in0, scalar, in1, op0, op1, reads=(), writes=(), eng="dve"):
        return self.op(eng, lambda e: e.scalar_tensor_tensor(out=out, in0=in0, scalar=scalar, in1=in1, op0=op0, op1=op1), reads, writes)

    def emit(self, nc, final_waits=True, limit=None, semstack=None, tag=''):
        if limit is not None:
            self.ops = self.ops[:limit]
            for o in self.ops:
                o.signal = False
            for o in self.ops:
                for d in o.deps:
                    if not d.is_dma:
                        d.signal = True
        cnt = {e: 0 for e in ENGS}
        for o in self.ops:
            if o.is_dma:
                continue
            if o.signal:
                cnt[o.eng] += 1
                o.sig_idx = cnt[o.eng]
        per_eng = {e: [o for o in self.ops if o.eng == e] for e in ENGS}
        with ExitStack() as st:
            sst = semstack if semstack is not None else st
            sems = {e: sst.enter_context(nc.semaphore(tag + "s_" + e)) for e in ENGS}
            dsems = [sst.enter_context(nc.semaphore(tag + "d%d" % i)) for i in range(N_DMA_SEMS + self.n_cc)]
            block = st.enter_context(nc.Block())
            all_dma = [o for o in self.ops if o.is_dma]

            def body(ename):
                def f(engine):
                    waited = {}

                    def wait(sem_key, sem, val):
                        if waited.get(sem_key, 0) >= val:
                            return
                        waited[sem_key] = val
                        engine.wait_ge(sem, val)

                    for o in per_eng[ename]:
                        for d in o.deps:
                            if d.is_dma:
                                wait(("d", d.dma_sem), dsems[d.dma_sem], d.dma_val)
                            else:
                                wait(("e", d.eng), sems[d.eng], d.sig_idx)
                        if o.is_dma:
                            if o.dma_val > 16:
                                wait(("d", o.dma_sem), dsems[o.dma_sem], o.dma_val - 16)
                            ins = o.fn(engine)
                            if o.cc:
                                ins.then_inc(dsems[o.dma_sem])
                            else:
                                ins.then_inc(dsems[o.dma_sem], 16)
                        else:
                            ins = o.fn(engine)
                            if o.signal:
                                ins.then_inc(sems[ename], 1)
                    if final_waits and ename == "sp":
                        last = {}
                        for o in all_dma:
                            last[o.dma_sem] = max(last.get(o.dma_sem, 0), o.dma_val)
                        for s, v in last.items():
                            wait(("d", s), dsems[s], v)
                        for e in ENGS:
                            if cnt[e] > 0:
                                wait(("e", e), sems[e], cnt[e])
                return f

            block.tensor(body("pe"))
            block.scalar(body("act"))
            block.vector(body("dve"))
            block.gpsimd(body("pool"))
            block.sync(body("sp"))


S = 8192
D = 1024
NG_A = S // 512
bf = ml_dtypes.bfloat16
NEG = -1.0e30


def phase_A(nc, semstack, x, ogbuf, ogall, n_groups=NG_A, limit=None):
    dram = lambda n, sh, dt, kind="ExternalInput": nc.dram_tensor("a_" + n, sh, dt, kind=kind).ap()
    gbc = dram("gbc", [128, D], F32)
    w4 = dram("w4", [D, 1024], F32)
    qc = dram("qc", [4, 8, S], BF16)
    kc = dram("kc", [8, S], BF16)
    oh = dram("oh", [32, S], BF16)
    caus = dram("caus", [128, 2, 256], BF16)
    ident = dram("ident", [128, 128], BF16)
    sel = dram("sel", [128, 4], F32)
    s = Sched()
    with ExitStack() as st:
        sb = lambda name, shape, dt: st.enter_context(nc.sbuf_tensor("A_" + name, shape, dt))
        NX = 3
        xin = [sb("xin%d" % i, [128, D], F32) for i in range(NX)]
        gbc_sb = sb("gbc_sb", [128, D], F32)
        junk = sb("junk", [128, D], BF16)
        hn = [sb("hn%d" % i, [128, D], BF16) for i in range(2)]
        hT = sb("hT", [128, 8, 512], BF16)
        w_sb = sb("w_sb", [128, 8, 1024], BF16)
        kaug = sb("kaug", [104, 4, S], BF16)
        vaug = sb("vaug", [128, 64, 4, 65], BF16)
        qaug = [sb("qaug%d" % i, [104, 4, 512], BF16) for i in range(2)]
        sg = [sb("sg%d" % i, [64, 4, 512], BF16) for i in range(2)]
        gs = sb("gs", [128, 16, 32], F32)
        m8 = sb("m8", [128, 16, 8], F32)
        mk01 = sb("mk01", [128, 16, 32], F32)
        mkb = sb("mkb", [128, 16, 32], BF16)
        NP = 4
        pT = [sb("pT%d" % i, [128, 512], BF16) for i in range(NP)]
        kmean = sb("kmean", [64, 4, 32], BF16)
        ksum = sb("ksum", [64, 4, 2], F32)
        ss = sb("ss", [128, 4], F32)
        rstd = sb("rstd", [128, 4], F32)
        ones32 = sb("ones32", [128, 64], F32)
        rden = sb("rden", [128, 512], F32)
        t1 = [sb("t1_%d" % i, [64, 512], F32) for i in range(2)]
        og = [sb("og%d" % i, [64, 512], F32) for i in range(2)]
        og4 = [sb("og4_%d" % i, [64, 4, 512], F32) for i in range(2)]
        sel_sb = sb("sel_sb", [128, 4], F32)
        id_sb = sb("id_sb", [128, 128], BF16)
        caus_sb = sb("caus_sb", [128, 2, 256], BF16)
        psb = [st.enter_context(nc.psum_tensor("A_ps%d" % i, [128, 512], F32)) for i in range(8)]
        psS = psb[0:3]
        psO = psb[3:5]
        psP = psb[5:7]
        psM = psb[7]
        psM_bf = psM[:].bitcast(BF16)

        s.dma("sp", gbc_sb[:], gbc[:], writes=["gbc"])
        s.dma("sp", sel_sb[:], sel[:], writes=["sel"])
        s.dma("sp", id_sb[:], ident[:], writes=["ident"])
        s.dma("sp", caus_sb[:], caus[:], writes=["caus"])
        for c in range(8):
            slot = c % NX
            s.dma("sp", xin[slot][:], w4[c * 128:(c + 1) * 128, :], writes=[("xin", slot)])
            s.copy("pool", w_sb[:, c, :], xin[slot][:], reads=[("xin", slot)], writes=["w_sb"])
        for h in range(4):
            s.dma("pool", kaug[64:96, h, :], oh[:, :], writes=[("kaug_c", h, 0)])
            s.dma("pool", kaug[96:104, h, :], kc[:, :], writes=[("kaug_c", h, 1)])
        s.memset("pool", vaug[:, :, :, 64:65], 1.0, writes=["vaug_ones"])
        s.memset("pool", gs[:], NEG, writes=["gs"])
        s.memset("pool", ones32[:], 1.0, writes=["ones32"])
        for h in range(4):
            s.memset("pool", kmean[:, h, :], 0.0, writes=[("kmean", h)])

        xcount = [0]
        pcount = [0]
        scount = [0]
        ocount = [0]

        def stage_P(G):
            qs = G % 2
            t0 = G * 512
            s.dma("sp", qaug[qs][96:104, :, :], qc[:, :, t0:t0 + 512].rearrange("h r t -> r h t"),
                  writes=[("qaug_c", qs)])
            yield
            for tt in range(4):
                xs = xcount[0] % NX
                xcount[0] += 1
                hs = tt % 2
                tok = t0 + tt * 128
                s.dma("sp", xin[xs][:], x[tok:tok + 128, :], writes=[("xin", xs)])
                s.op("dve", lambda e, xs=xs, tt=tt: e.scalar_tensor_tensor(
                    out=junk[:], in0=xin[xs][:], scalar=1.0, in1=xin[xs][:],
                    op0=ALU.mult, op1=ALU.mult, accum_out=ss[:, tt:tt + 1]),
                    reads=[("xin", xs)], writes=["junk", ("ss", tt)])
                s.act(rstd[:, tt:tt + 1], ss[:, tt:tt + 1], AF.Ln, scale=1.0 / D, bias=eps_t[:, 0:1],
                      reads=[("ss", tt), "eps"], writes=[("rstd", tt)])
                s.act(rstd[:, tt:tt + 1], rstd[:, tt:tt + 1], AF.Exp, scale=-0.5,
                      reads=[("rstd", tt)], writes=[("rstd", tt)])
                s.stt(hn[hs][:], xin[xs][:], rstd[:, tt:tt + 1], gbc_sb[:], ALU.mult, ALU.mult,
                      reads=[("xin", xs), ("rstd", tt), "gbc"], writes=[("hn", hs)])
                yield
                for c in range(8):
                    s.tr(psM_bf[:, c * 128:(c + 1) * 128], hn[hs][:, c * 128:(c + 1) * 128], id_sb[:],
                         reads=[("hn", hs), "ident"], writes=["psM"])
                s.copy("dve", hT[:, :, tt * 128:(tt + 1) * 128],
                       psM_bf.rearrange("p (c t) -> p c t", c=8), reads=["psM"], writes=[("hT", tt)])
                yield
            hT_keys = [("hT", tt) for tt in range(4)]

            def proj(col0):
                ps = pcount[0] % 2
                pcount[0] += 1
                for c in range(8):
                    s.mm(psP[ps][:, :], w_sb[:, c, col0:col0 + 128], hT[:, c, :], start=(c == 0), stop=(c == 7),
                         reads=hT_keys + ["w_sb"], writes=[("psP", ps)])
                return ps

            for hp in range(2):
                ps = proj(hp * 128)
                for hh in range(2):
                    h = 2 * hp + hh
                    s.copy("dve", qaug[qs][0:64, h, :], psP[ps][hh * 64:(hh + 1) * 64, :], reads=[("psP", ps)],
                           writes=[("qaug_q", qs, h)])
                    yield
                ps = proj(256 + hp * 128)
                for hh in range(2):
                    h = 2 * hp + hh
                    for b2 in range(2):
                        s.act(kaug[0:64, h, t0 + b2 * 256:t0 + (b2 + 1) * 256],
                              psP[ps][hh * 64:(hh + 1) * 64, b2 * 256:(b2 + 1) * 256],
                              AF.Copy, accum_out=ksum[:, h, b2:b2 + 1], reads=[("psP", ps)],
                              writes=[("kaug_k", h, G), ("ksum", h)])
                    s.ts("dve", kmean[:, h, 2 * G:2 * G + 2], ksum[:, h, :], 1.0 / 256.0, None, ALU.mult, None,
                         reads=[("ksum", h)], writes=[("kmean", h)])
                    yield
            for hp in range(2):
                ps = proj(768 + hp * 128)
                for hh in range(2):
                    h = 2 * hp + hh
                    s.act(sg[qs][:, h, :], psP[ps][hh * 64:(hh + 1) * 64, :], AF.Silu, reads=[("psP", ps)],
                          writes=[("sg", qs, h)])
            yield
            for tt in range(4):
                ps = pcount[0] % 2
                pcount[0] += 1
                for c in range(8):
                    s.mm(psP[ps][:, 0:256], hT[:, c, tt * 128:(tt + 1) * 128], w_sb[:, c, 512:768],
                         start=(c == 0), stop=(c == 7), reads=[("hT", tt), "w_sb"], writes=[("psP", ps)])
                s.copy("dve", vaug[:, 4 * G + tt, :, 0:64], psP[ps][:, 0:256].rearrange("p (h d) -> p h d", h=4),
                       reads=[("psP", ps)], writes=[("vaug", 4 * G + tt)])
                yield
            psM3 = psM[:].rearrange("p (i n) -> p i n", n=32)
            for tt in range(4):
                for h in range(4):
                    s.mm(psM3[:, tt * 4 + h, :], qaug[qs][0:64, h, tt * 128:(tt + 1) * 128], kmean[:, h, :],
                         reads=[("qaug_q", qs, h), ("kmean", h)], writes=["psM"])
            own0, own1 = 2 * G, 2 * G + 1
            if own0 > 0:
                s.copy("dve", gs[:, 0:8, 0:own0], psM3[:, 0:8, 0:own0], reads=["psM"], writes=["gs"])
            s.copy("dve", gs[:, 8:16, 0:own1], psM3[:, 8:16, 0:own1], reads=["psM"], writes=["gs"])
            yield
            for i in range(16):
                s.op("dve", lambda e, i=i: e.max(out=m8[:, i, :], in_=gs[:, i, :]), reads=["gs"], writes=["m8"])
                if i % 4 == 3:
                    yield
            s.tt("dve", mk01[:], gs[:], m8[:, :, 2:3].to_broadcast([128, 16, 32]), ALU.is_ge,
                 reads=["gs", "m8"], writes=["mk01"])
            s.ts("dve", mkb[:], mk01[:], -1.0, 30000.0, ALU.add, ALU.mult, reads=["mk01"], writes=["mkb"])
            s.memset("dve", mkb[:, 0:8, own0:own0 + 1], 0.0, writes=["mkb"])
            s.memset("dve", mkb[:, 8:16, own1:own1 + 1], 0.0, writes=["mkb"])
            yield
            psM4 = psM_bf.rearrange("p (h t) -> p h t", h=2)
            for hp in range(2):
                for hh in range(2):
                    h = hp * 2 + hh
                    for tt in range(4):
                        s.tr(psM4[64:96, hh, tt * 128:(tt + 1) * 128], mkb[:, tt * 4 + h, :], id_sb[:],
                             reads=["mkb", "ident"], writes=["psM"])
                s.copy("dve", qaug[qs][64:96, hp * 2:hp * 2 + 2, :], psM4[64:96, :, :], reads=["psM"],
                       writes=[("qaug_m", qs, hp)])
                yield

        def stage_A(G, bg):
            qs = G % 2
            t0 = G * 512
            for h in range(4):
                os_ = ocount[0] % 2
                ocount[0] += 1
                chunks = []
                for c in range(4 * G):
                    chunks.append((c, 0, 512, None))
                for cc in range(2):
                    chunks.append((4 * G + cc, 0, 512, cc))
                for cc in range(2):
                    chunks.append((4 * G + 2 + cc, 256, 512, cc))
                qreads = [("qaug_q", qs, h), ("qaug_m", qs, h // 2), ("qaug_c", qs)]
                pend = []
                DLY = 2

                def emit_pv(item):
                    ci_, c_, c0_, c1_, pl_ = item
                    s.mm(psO[os_][0:65, c0_:c1_], vaug[:, c_, h, 0:65], pT[pl_][:, c0_:c1_],
                         start=(ci_ == 0), stop=(ci_ == len(chunks) - 1),
                         reads=[("pT", pl_), ("vaug", c_), "vaug_ones"], writes=[("psO", os_)])

                for ci, (c, c0, c1, diag) in enumerate(chunks):
                    sl = scount[0] % 3
                    pl = scount[0] % NP
                    scount[0] += 1
                    s.mm(psS[sl][:, c0:c1], kaug[0:104, h, c * 128:(c + 1) * 128], qaug[qs][0:104, h, c0:c1],
                         start=True, stop=(diag is None),
                         reads=qreads + [("kaug_k", h, c // 4), ("kaug_c", h, 0), ("kaug_c", h, 1)], writes=[("psS", sl)])
                    if diag is not None:
                        d0 = 0 if c0 == 0 else 256
                        s.mm(psS[sl][:, d0:d0 + 256], id_sb[:], caus_sb[:, diag, :], start=False, stop=True,
                             reads=["ident", "caus"], writes=[("psS", sl)])
                    s.act(pT[pl][:, c0:c1], psS[sl][:, c0:c1], AF.Exp, scale=0.125,
                          reads=[("psS", sl)], writes=[("pT", pl)])
                    pend.append((ci, c, c0, c1, pl))
                    if len(pend) > DLY:
                        emit_pv(pend.pop(0))
                    if bg[0] is not None:
                        try:
                            next(bg[0])
                        except StopIteration:
                            bg[0] = None
                while pend:
                    emit_pv(pend.pop(0))
                ts_ = os_
                s.op("dve", lambda e, os_=os_: e.reciprocal(out=rden[64:65, :], in_=psO[os_][64:65, :]),
                     reads=[("psO", os_)], writes=["rden"])
                s.mm(psM[0:64, :], ones32[64:65, 0:64], rden[64:65, :], reads=["ones32", "rden"], writes=["psM"])
                s.tt("dve", t1[ts_][:], psO[os_][0:64, :], sg[qs][:, h, :], ALU.mult,
                     reads=[("psO", os_), ("sg", qs, h)], writes=[("t1", ts_)])
                s.tt("dve", og[ts_][:], t1[ts_][:], psM[0:64, :], ALU.mult,
                     reads=[("t1", ts_), "psM"], writes=[("og", ts_)])
                for jj in range(4):
                    s.ts("pool", og4[ts_][:, jj, :], og[ts_][:], sel_sb[0:64, jj:jj + 1], 1.0, ALU.mult, ALU.mult,
                         reads=[("og", ts_), "sel"], writes=[("og4", ts_)])
                ck, c0 = t0 // 1024, t0 % 1024
                s.dma("pool", ogbuf[ck, :, c0:c0 + 512].rearrange("(j r) t -> r j t", j=4)[h * 64:(h + 1) * 64, :, :], og4[ts_][:],
                      reads=[("og4", ts_)], writes=[("ogbuf", ck)])

        eps_t = sb("eps_t", [128, 1], F32)
        s.memset("pool", eps_t[:], 1e-6, writes=["eps"])
        for _ in stage_P(0):
            pass
        for G in range(1, n_groups + 1):
            bg = [stage_P(G)] if G < n_groups else [None]
            stage_A(G - 1, bg)
            if bg[0] is not None:
                for _ in bg[0]:
                    pass
            if True:
                if (G - 1) % 2 == 1:
                    ck_ = (G - 1) // 2
                    s.cc(lambda e, ck_=ck_: e.collective_compute("AllReduce", ALU.add, replica_groups=GROUPS,
                                                                 ins=[ogbuf[ck_].opt()], outs=[ogall[ck_].opt()]),
                         reads=[("ogbuf", ck_)])
        s.emit(nc, limit=limit, semstack=semstack, tag='A')


def consts_A(g):
    pos = np.arange(S)
    qc = np.zeros((4, 8, S), dtype=bf)
    for hl in range(4):
        hg = 4 * g + hl
        slope = 2.0 ** (-8.0 * (hg + 1) / 16)
        s8 = np.float64(8.0 * slope)
        p1 = np.float64(bf(s8)); p2 = np.float64(bf(s8 - p1)); p3 = np.float64(bf(s8 - p1 - p2))
        s8p = p1 + p2 + p3
        T = -s8p * pos.astype(np.float64)
        Thi = T.astype(bf); Tlo = (T - Thi.astype(np.float64)).astype(bf)
        qc[hl, 0] = p1; qc[hl, 1] = p2; qc[hl, 2] = p3
        qc[hl, 3] = p1; qc[hl, 4] = p2; qc[hl, 5] = p3
        qc[hl, 6] = Thi; qc[hl, 7] = Tlo
    kc = np.zeros((8, S), dtype=bf)
    kc[0:3] = (pos % 256).astype(bf)
    kc[3:6] = (256 * (pos // 256)).astype(bf)
    kc[6:8] = 1.0
    oh = np.zeros((32, S), dtype=bf)
    oh[pos // 256, pos] = 1.0
    caus = np.zeros((128, 2, 256), dtype=bf)
    j = np.arange(128)[:, None]
    i = np.arange(256)[None, :]
    for c in range(2):
        caus[:, c, :] = np.where(c * 128 + j <= i, 0.0, -30000.0).astype(bf)
    ident = np.eye(128).astype(bf)
    return dict(qc=qc, kc=kc, oh=oh, caus=caus, ident=ident)


F32R = mybir.dt.float32r
S = 8192
D = 1024
bf = ml_dtypes.bfloat16
CDEC = 0.6065306597126334
L = 128


GT = 256
NCH = GT // L


def phase_B(nc, semstack, x, ogall, og1buf, og1all, n_groups=S // GT, limit=None):
    dram = lambda n, sh, dt, kind="ExternalInput": nc.dram_tensor("b_" + n, sh, dt, kind=kind).ap()
    wout0 = dram("wout0", [D, D], F32)
    gbc = dram("gbc", [128, D], F32)
    mixT = dram("mixT", [128, 8, 6], F32)
    w4 = dram("w4", [D, 1024], F32)
    wl = dram("wl", [D, 128], F32)
    w2a2 = dram("w2a2", [64, 2, 256], F32)
    cpar = dram("cpar", [64, 5, 4], F32)
    lnx = dram("lnx", [128, 2, 256], F32)
    masks = dram("masks", [128, 3, 128], F32)
    identf = dram("identf", [128, 128], F32)
    identb = dram("identb", [128, 128], BF16)
    sel = dram("sel", [128, 4], F32)
    s = Sched()
    with ExitStack() as st:
        sb = lambda name, shape, dt: st.enter_context(nc.sbuf_tensor("B_" + name, shape, dt))
        NX = 2
        xin = [sb("xin%d" % i, [128, D], F32) for i in range(NX)]
        ogin = [sb("ogin%d" % i, [128, 8, 128], BF16) for i in range(1)] * 2
        ogf = [sb("ogf%d" % i, [128, 8, 128], F32) for i in range(1)] * 2
        sel_sb = sb("sel_sb", [128, 4], F32)
        ogo4 = [sb("ogo4_%d" % i, [128, 4, 256], F32) for i in range(1)] * 2
        x1t = sb("x1t", [128, D], F32)
        hn = sb("hn", [128, D], BF16)
        junk = hn
        hx = sb("hx", [128, 8, GT + 4], BF16)
        lastcol = sb("lastcol", [128, 8, 2], BF16)
        gbc_sb = sb("gbc_sb", [128, D], F32)
        wo_sb = sb("wo_sb", [128, 8, 1024], BF16)
        wst = x1t
        wa_sb = sb("wa_sb", [128, 8, 1024], BF16)
        wb_sb = sb("wb_sb", [128, 8, 1024], BF16)
        wla_sb = sb("wla_sb", [128, 8, 128], BF16)
        wlb_sb = sb("wlb_sb", [128, 8, 128], BF16)
        mix_sb = sb("mix_sb", [128, 8, 6], F32)
        onem_sb = sb("onem_sb", [128, 8, 6], F32)
        w2a2_sb = sb("w2a2_sb", [64, 2, 256], BF16)
        w2st = sb("w2st", [64, 2, 256], F32)
        cp = sb("cp", [64, 5, 4], F32)
        onemka = sb("onemka", [64, 4], F32)
        rk2 = sb("rk2", [64, 4, 2], F32R)
        lnx_sb = sb("lnx_sb", [128, 2, 256], F32)
        mk = sb("mk", [128, 3, 128], F32)
        idf = sb("idf", [128, 128], F32)
        idb = sb("idb", [128, 128], BF16)
        ones_r = sb("ones_r", [64, 64], F32R)
        ones_f = sb("ones_f", [64, 128], F32)
        eps_t = sb("eps_t", [128, 3], F32)
        ss = sb("ss", [128, 2], F32)
        rstd = sb("rstd", [128, 2], F32)
        Gt = lambda name, dt=F32: sb(name, [64, 4, GT], dt)
        r_c2 = [Gt("r_c%d" % i) for i in range(2)]; k_c2 = [Gt("k_c%d" % i) for i in range(2)]
        sgw2 = [Gt("sgw%d" % i) for i in range(2)]; a_c2 = [Gt("a_c%d" % i) for i in range(2)]
        lhid2 = [sb("lhid%d" % i, [64, 2, GT], BF16) for i in range(2)]
        v_r2 = [sb("v_r%d" % i, [128, NCH, 256], F32R) for i in range(2)]
        sg_t2 = [sb("sg_t%d" % i, [128, NCH, 256], F32) for i in range(2)]
        Ct = lambda name, dt=F32: sb(name, [64, 4, L], dt)
        kk = Ct("kk"); tmp1 = Ct("tmp1"); tmp2 = Ct("tmp2", F32R); cum = Ct("cum"); ex = Ct("ex")
        g_incl = Ct("g_incl"); g_inv = Ct("g_inv"); g_excl = Ct("g_excl"); g_rem = Ct("g_rem")
        b_c = Ct("b_c"); km = Ct("km")
        AR = sb("AR", [64, 4, 2, L], F32R)
        bh = Ct("bh", F32R); kh = Ct("kh", F32R); rkp = Ct("rkp", F32R)
        Bt = Ct("Bt"); Kt = Ct("Kt")
        gLt = sb("gLt", [64, 4, 2], F32)
        NU = 4
        A1sb = [sb("A1sb%d" % i, [128, 256], F32R) for i in range(NU)]
        A2sb = [sb("A2sb%d" % i, [128, 256], F32R) for i in range(NU)]
        QMP = [[sb("QMP%d_%d" % (i, j), [128, 384], F32R) for j in range(2)] for i in range(NU)]
        tok3 = [sb("tok3_%d" % i, [128, 3, 64], F32R) for i in range(NU)]
        G1sb = [sb("G1sb%d" % i, [64, 128], F32R) for i in range(NU)]
        AVsb = [sb("AVsb%d" % i, [128, 64], F32R) for i in range(NU)]
        P2sb = [sb("P2sb%d" % i, [128, 64], F32) for i in range(NU)]
        Usb = [sb("Usb%d" % i, [128, 64], F32R) for i in range(NU)]
        Ssb = [[sb("Ssb%d_%d" % (h, j), [64, 64], F32R) for j in range(2)] for h in range(4)]
        zer = sb("zer", [64, 64], F32)
        ysb = sb("ysb", [128, 4, 64], F32)
        bst = sb("bst", [128, 4, 6], F32)
        mv = sb("mv", [128, 4, 2], F32)
        rs4 = sb("rs4", [128, 4], F32)
        yn = sb("yn", [128, 256], F32)
        rks = sb("rks", [128, 4], F32)
        ogo = [sb("ogo%d" % i, [128, 256], F32) for i in range(1)] * 2
        psb = [st.enter_context(nc.psum_tensor("B_ps%d" % i, [128, 512], F32)) for i in range(8)]
        psP = psb[0:2]
        psT = psb[1]
        psT_bf = psT[:].bitcast(BF16)
        psN = psb[2:6]
        psAs = psb[6:8]

        s.dma("sp", gbc_sb[:], gbc[:], writes=["gbc"])
        s.dma("sp", sel_sb[:], sel[:], writes=["sel"])
        s.dma("sp", idb[:], identb[:], writes=["idb"])
        s.dma("sp", idf[:], identf[:], writes=["idf"])
        s.dma("sp", mk[:], masks[:], writes=["mk"])
        s.dma("sp", mix_sb[:], mixT[:], writes=["mix"])
        s.dma("sp", cp[:], cpar[:], writes=["cp"])
        s.dma("sp", lnx_sb[:], lnx[:], writes=["lnx"])
        s.dma("sp", w2st[:], w2a2[:], writes=["w2st"])
        s.copy("pool", w2a2_sb[:], w2st[:], reads=["w2st"], writes=["w2a2"])
        s.ts("dve", onem_sb[:], mix_sb[:], -1.0, 1.0, ALU.mult, ALU.add, reads=["mix"], writes=["onem"])
        s.ts("dve", onemka[:], cp[:, 3, :], -1.0, 1.0, ALU.mult, ALU.add, reads=["cp"], writes=["onemka"])
        s.copy("dve", rk2[:], cp[:, 4, :].unsqueeze(2).to_broadcast([64, 4, 2]), reads=["cp"], writes=["rk2"])
        s.memset("pool", ones_f[:], 1.0, writes=["ones_f"])
        s.copy("dve", ones_r[:], ones_f[:, 0:64], reads=["ones_f"], writes=["ones_r"])
        s.memset("pool", eps_t[:, 0:1], 1e-6, writes=["eps"])
        s.memset("pool", eps_t[:, 1:2], 64e-5, writes=["eps"])
        s.memset("pool", eps_t[:, 2:3], 1e-30, writes=["eps"])
        s.memset("pool", hx[:], 0.0, writes=[("hx", 0), ("hx", 1), "hx0"])
        s.memset("pool", zer[:], 0.0, writes=["zer"])
        stg = [(x1t, "wst"), (xin[0], ("xin", 0)), (xin[1], ("xin", 1))]
        sk = [0]

        def nstg():
            t_, k_ = stg[sk[0] % 3]
            sk[0] += 1
            return t_, k_

        for c in range(8):
            w_, k_ = nstg()
            s.dma("sp", w_[:], wout0[c * 128:(c + 1) * 128, :], writes=[k_])
            s.copy("pool", wo_sb[:, c, :], w_[:], reads=[k_], writes=["wo_sb"])
        for c in range(8):
            w_, k_ = nstg()
            s.dma("sp", w_[:], w4[c * 128:(c + 1) * 128, :], writes=[k_])
            for n in range(4):
                s.ts("dve", wa_sb[:, c, n * 256:(n + 1) * 256], w_[:, n * 256:(n + 1) * 256], onem_sb[:, c, n:n + 1], None,
                     ALU.mult, reads=[k_, "onem"], writes=["wa_sb"])
                s.ts("pool", wb_sb[:, c, n * 256:(n + 1) * 256], w_[:, n * 256:(n + 1) * 256], mix_sb[:, c, n:n + 1], 1.0,
                     ALU.mult, ALU.mult, reads=[k_, "mix"], writes=["wb_sb"])
        for c in range(8):
            w_, k_ = nstg()
            s.dma("sp", w_[:, 0:128], wl[c * 128:(c + 1) * 128, :], writes=[k_])
            for n in range(2):
                s.ts("dve", wla_sb[:, c, n * 64:(n + 1) * 64], w_[:, n * 64:(n + 1) * 64], onem_sb[:, c, 4 + n:5 + n], None,
                     ALU.mult, reads=[k_, "onem"], writes=["wla_sb"])
                s.ts("pool", wlb_sb[:, c, n * 64:(n + 1) * 64], w_[:, n * 64:(n + 1) * 64], mix_sb[:, c, 4 + n:5 + n], 1.0,
                     ALU.mult, ALU.mult, reads=[k_, "mix"], writes=["wlb_sb"])
        for h in range(4):
            s.copy("dve", Ssb[h][0][:], zer[:], reads=["zer"], writes=[("S", h, 0)])

        cnt = {"x": 0, "p": 0, "u": 0, "o": 0}
        allh = lambda n, gp=None: [((n, h) if gp is None else (n, h, gp)) for h in range(4)]

        def nextp():
            p = cnt["p"] % 2
            cnt["p"] += 1
            return p

        def stage_Pa(Gi):
            t0 = Gi * GT
            if Gi > 0:
                s.copy("pool", hx[:, :, 3:4], lastcol[:, :, 0:1], reads=["lastcol"], writes=["hx0"])
            for tt in range(NCH):
                xs = cnt["x"] % NX
                cnt["x"] += 1
                tok = t0 + tt * 128
                s.dma("sp", xin[xs][:], x[tok:tok + 128, :], writes=[("xin", xs)])
                s.dma("pool", ogf[xs][:], ogall[tok // 1024, :, tok % 1024:tok % 1024 + 128].rearrange("(c p) t -> p c t", p=128), writes=["ogf"])
                s.copy("act", ogin[xs][:], ogf[xs][:], reads=["ogf"], writes=["ogin"])
                yield
                for half in range(2):
                    ps = nextp()
                    for c in range(8):
                        s.mm(psP[ps][:, :], ogin[xs][:, c, :], wo_sb[:, c, half * 512:(half + 1) * 512], start=(c == 0), stop=(c == 7),
                             reads=["ogin", "wo_sb"], writes=[("psP", ps)])
                    yield
                    s.tt("dve", x1t[:, half * 512:(half + 1) * 512], psP[ps][:, :], xin[xs][:, half * 512:(half + 1) * 512], ALU.add,
                         reads=[("psP", ps), ("xin", xs)], writes=[("x1t", half), "wst"])
                    yield
                s.op("dve", lambda e, tt=tt: e.scalar_tensor_tensor(
                    out=junk[:], in0=x1t[:], scalar=1.0, in1=x1t[:],
                    op0=ALU.mult, op1=ALU.mult, accum_out=ss[:, tt:tt + 1]),
                    reads=[("x1t", 0), ("x1t", 1)], writes=["hn", ("ss", tt)])
                s.act(rstd[:, tt:tt + 1], ss[:, tt:tt + 1], AF.Ln, scale=1.0 / D, bias=eps_t[:, 0:1],
                      reads=[("ss", tt), "eps"], writes=[("rstd", tt)])
                s.act(rstd[:, tt:tt + 1], rstd[:, tt:tt + 1], AF.Exp, scale=-0.5,
                      reads=[("rstd", tt)], writes=[("rstd", tt)])
                s.stt(hn[:], x1t[:], rstd[:, tt:tt + 1], gbc_sb[:], ALU.mult, ALU.mult,
                      reads=[("x1t", 0), ("x1t", 1), ("rstd", tt), "gbc"], writes=["hn"])
                yield
                for c in range(8):
                    s.tr(psT_bf[:, c * 128:(c + 1) * 128], hn[:, c * 128:(c + 1) * 128], idb[:],
                         reads=["hn", "idb"], writes=[("psP", 1)])
                yield
                s.copy("act", hx[:, :, 4 + tt * 128:4 + (tt + 1) * 128],
                       psT_bf.rearrange("p (c t) -> p c t", c=8), reads=[("psP", 1)], writes=[("hx", tt)])
                yield
            s.copy("pool", lastcol[:, :, 0:1], hx[:, :, GT + 3:GT + 4], reads=[("hx", NCH - 1)], writes=["lastcol"])
            yield

        def stage_Pb(Gi):
            gp = Gi % 2
            r_c, k_c, sgw, a_c, lhid, v_r, sg_t = r_c2[gp], k_c2[gp], sgw2[gp], a_c2[gp], lhid2[gp], v_r2[gp], sg_t2[gp]
            hkeys = [("hx", tt) for tt in range(NCH)] + ["hx0"]

            def proj_cm(wa, wb, col0, ncols, ps_):
                out_ps = psP[ps_][0:ncols, 0:GT]
                for c in range(8):
                    s.mm(out_ps, wa[:, c, col0:col0 + ncols], hx[:, c, 4:GT + 4], start=(c == 0), stop=False,
                         reads=hkeys + ["wa_sb", "wla_sb"], writes=[("psP", ps_)])
                for c in range(8):
                    s.mm(out_ps, wb[:, c, col0:col0 + ncols], hx[:, c, 3:GT + 3], start=False, stop=(c == 7),
                         reads=hkeys + ["wb_sb", "wlb_sb"], writes=[("psP", ps_)])
                return out_ps

            ps_ = nextp()
            proj_cm(wla_sb, wlb_sb, 0, 128, ps_)
            s.act(lhid[:, 0, :], psP[ps_][0:64, 0:GT], AF.Tanh, reads=[("psP", ps_)], writes=[("lhid", 0, gp)])
            s.copy("act", lhid[:, 1, :], psP[ps_][64:128, 0:GT], reads=[("psP", ps_)], writes=[("lhid", 1, gp)])
            yield
            for h in range(4):
                ps_ = nextp()
                o_ = psP[ps_][0:64, 0:GT]
                s.mm(o_, w2a2_sb[:, 0, h * 64:(h + 1) * 64], lhid[:, 0, :], reads=["w2a2", ("lhid", 0, gp)], writes=[("psP", ps_)])
                s.act(sgw[:, h, :], o_, AF.Sigmoid, bias=cp[:, 0, h:h + 1], reads=[("psP", ps_), "cp"], writes=[("sgw", h, gp)])
                yield
                ps_ = nextp()
                o_ = psP[ps_][0:64, 0:GT]
                s.mm(o_, w2a2_sb[:, 1, h * 64:(h + 1) * 64], lhid[:, 1, :], reads=["w2a2", ("lhid", 1, gp)], writes=[("psP", ps_)])
                s.act(a_c[:, h, :], o_, AF.Sigmoid, bias=cp[:, 1, h:h + 1], reads=[("psP", ps_), "cp"], writes=[("a_c", h, gp)])
                yield
            for hp in range(2):
                ps_ = nextp()
                proj_cm(wa_sb, wb_sb, hp * 128, 128, ps_)
                s.copy("dve", r_c[:, 2 * hp, :], psP[ps_][0:64, 0:GT], reads=[("psP", ps_)], writes=[("r_c", 2 * hp, gp)])
                s.copy("dve", r_c[:, 2 * hp + 1, :], psP[ps_][64:128, 0:GT], reads=[("psP", ps_)], writes=[("r_c", 2 * hp + 1, gp)])
                yield
                ps_ = nextp()
                proj_cm(wa_sb, wb_sb, 256 + hp * 128, 128, ps_)
                s.copy("act", k_c[:, 2 * hp, :], psP[ps_][0:64, 0:GT], reads=[("psP", ps_)], writes=[("k_c", 2 * hp, gp)])
                s.copy("act", k_c[:, 2 * hp + 1, :], psP[ps_][64:128, 0:GT], reads=[("psP", ps_)], writes=[("k_c", 2 * hp + 1, gp)])
                yield
            for tt in range(NCH):
                ps_ = nextp()
                for c in range(8):
                    s.mm(psP[ps_][:, :], hx[:, c, 4 + tt * 128:4 + (tt + 1) * 128], wa_sb[:, c, 512:1024], start=(c == 0), stop=False,
                         reads=hkeys + ["wa_sb"], writes=[("psP", ps_)])
                for c in range(8):
                    s.mm(psP[ps_][:, :], hx[:, c, 3 + tt * 128:3 + (tt + 1) * 128], wb_sb[:, c, 512:1024], start=False, stop=(c == 7),
                         reads=hkeys + ["wb_sb"], writes=[("psP", ps_)])
                s.copy("dve", v_r[:, tt, :], psP[ps_][:, 0:256], reads=[("psP", ps_)], writes=[("v_r", tt, gp)])
                s.act(sg_t[:, tt, :], psP[ps_][:, 256:512], AF.Sigmoid, reads=[("psP", ps_)], writes=[("sg_t", tt, gp)])
                s.tt("dve", sg_t[:, tt, :], sg_t[:, tt, :], psP[ps_][:, 256:512], ALU.mult, reads=[("psP", ps_), ("sg_t", tt, gp)], writes=[("sg_t", tt, gp)])
                yield

        def chunk_elem(j, gp):
            r_c, k_c, sgw, a_c, lhid, v_r, sg_t = r_c2[gp], k_c2[gp], sgw2[gp], a_c2[gp], lhid2[gp], v_r2[gp], sg_t2[gp]
            cs = slice(j * L, (j + 1) * L)
            bc = lambda i: cp[:, i, :].unsqueeze(2).to_broadcast([64, 4, L])
            s.tt("dve", kk[:], k_c[:, :, cs], bc(2), ALU.mult, reads=allh("k_c", gp) + ["cp"], writes=["kk"])
            yield
            s.tt("dve", tmp2[:], kk[:], kk[:], ALU.mult, reads=["kk"], writes=["tmp2"])
            yield
            for h in range(4):
                ps_ = nextp()
                o_ = psP[ps_][0:64, 0:L]
                s.mm(o_, ones_r[:, :], tmp2[:, h, :], reads=["ones_r", "tmp2"], writes=[("psP", ps_)])
                yield
                s.act(tmp1[:, h, :], o_, AF.Ln, bias=eps_t[0:64, 2:3], reads=[("psP", ps_), "eps"], writes=["tmp1"])
                yield
            s.act(tmp1[:], tmp1[:], AF.Exp, scale=-0.5, reads=["tmp1"], writes=["tmp1"])
            yield
            s.tt("dve", kk[:], kk[:], tmp1[:], ALU.mult, reads=["kk", "tmp1"], writes=["kk"])
            yield
            for h in range(4):
                s.op("dve", lambda e, h=h: e.tensor_tensor_scan(
                    out=cum[:, h, :], data0=ones_f[:, 0:L], data1=sgw[:, h, cs],
                    initial=0.0, op0=ALU.mult, op1=ALU.add), reads=[("sgw", h, gp), "ones_f"], writes=["cum"])
                yield
            s.tt("dve", ex[:], cum[:], sgw[:, :, cs], ALU.subtract, reads=["cum"] + allh("sgw", gp), writes=["ex"])
            yield
            s.tt("dve", tmp1[:], cum[:], cum[:, :, L - 1:L].to_broadcast([64, 4, L]), ALU.subtract, reads=["cum", "tmp1"], writes=["tmp1"])
            yield
            s.act(g_rem[:], tmp1[:], AF.Exp, scale=CDEC, reads=["tmp1"], writes=["g_rem"])
            yield
            s.act(g_incl[:], cum[:], AF.Exp, scale=-CDEC, reads=["cum"], writes=["g_incl"])
            yield
            s.act(g_inv[:], cum[:], AF.Exp, scale=CDEC, reads=["cum"], writes=["g_inv"])
            yield
            s.act(g_excl[:], ex[:], AF.Exp, scale=-CDEC, reads=["ex"], writes=["g_excl"])
            yield
            s.copy("pool", gLt[:, :, 0:1], g_incl[:, :, L - 1:L], reads=["g_incl"], writes=["gLt"])
            yield
            s.stt(AR[:, :, 0, :], kk[:], -1.0, g_excl[:], ALU.mult, ALU.mult, reads=["kk", "g_excl"], writes=["AR_at"])
            yield
            s.tt("dve", b_c[:], kk[:], a_c[:, :, cs], ALU.mult, reads=["kk"] + allh("a_c", gp), writes=["b_c"])
            yield
            s.tt("dve", bh[:], b_c[:], g_inv[:], ALU.mult, reads=["b_c", "g_inv"], writes=["bh"])
            yield
            s.tt("dve", Bt[:], b_c[:], g_rem[:], ALU.mult, reads=["b_c", "g_rem"], writes=["Bt"])
            yield
            s.tt("dve", tmp1[:], a_c[:, :, cs], bc(3), ALU.mult, reads=allh("a_c", gp) + ["cp", "tmp1"], writes=["tmp1"])
            yield
            s.tt("dve", tmp1[:], tmp1[:], onemka[:].unsqueeze(2).to_broadcast([64, 4, L]), ALU.add, reads=["tmp1", "onemka"], writes=["tmp1"])
            yield
            s.tt("dve", km[:], k_c[:, :, cs], tmp1[:], ALU.mult, reads=allh("k_c", gp) + ["tmp1"], writes=["km"])
            yield
            s.tt("dve", kh[:], km[:], g_inv[:], ALU.mult, reads=["km", "g_inv"], writes=["kh"])
            yield
            s.tt("dve", Kt[:], km[:], g_rem[:], ALU.mult, reads=["km", "g_rem"], writes=["Kt"])
            yield
            s.tt("dve", AR[:, :, 1, :], r_c[:, :, cs], g_incl[:], ALU.mult, reads=allh("r_c", gp) + ["g_incl"], writes=["AR_rt"])
            yield
            s.tt("dve", rkp[:], r_c[:, :, cs], km[:], ALU.mult, reads=allh("r_c", gp) + ["km"], writes=["rkp"])
            yield

        def unit_pre(j, h, u, gp):
            r_c, k_c, sgw, a_c, lhid, v_r, sg_t = r_c2[gp], k_c2[gp], sgw2[gp], a_c2[gp], lhid2[gp], v_r2[gp], sg_t2[gp]
            AR2 = AR[:, h, :, :].rearrange("c a t -> c (a t)")
            pa = u % 2
            psA_ = psAs[pa]
            ka = ("psA", pa)
            kn = ("psN", u)
            s.mm(psA_[:, 0:256], bh[:, h, :], AR2, reads=["bh", "AR_at", "AR_rt"], writes=[ka])
            s.mm(psA_[:, 256:512], kh[:, h, :], AR2, reads=["kh", "AR_at", "AR_rt"], writes=[ka])
            s.mm(psN[u][:, 256:384], AR[:, h, 0, :], bh[:, h, :], reads=["bh", "AR_at"], writes=[kn])
            yield
            mUI = mk[:, 0:2, :].rearrange("p a t -> p (a t)")
            s.tt("dve", A1sb[u][:], psA_[:, 0:256], mUI, ALU.mult, reads=[ka, "mk"], writes=[("A1", u)])
            s.tt("dve", QMP[u][0][:, 0:128], psA_[:, 0:128], mk[:, 0, :], ALU.mult, reads=[ka, "mk"], writes=[("QMP", u, 0)])
            s.tt("dve", QMP[u][0][:, 256:384], psN[u][:, 256:384], mk[:, 2, :], ALU.mult, reads=[kn, "mk"], writes=[("QMP", u, 0)])
            s.tt("dve", A2sb[u][:], psA_[:, 256:512], mUI, ALU.mult, reads=[ka, "mk"], writes=[("A2", u)])
            s.tt("dve", QMP[u][0][:, 128:256], A1sb[u][:, 0:128].bitcast(F32), idf[:], ALU.add, reads=[("A1", u), "idf"], writes=[("QMP", u, 0)])
            s.tt("dve", QMP[u][1][:, 128:256], A1sb[u][:, 0:128].bitcast(F32), idf[:], ALU.add, reads=[("A1", u), "idf"], writes=[("QMP", u, 1)])
            yield
            cur = 0
            for it in range(7):
                nxt = 1 - cur
                last = (it == 6)
                T_ = QMP[u][cur]
                Qc = T_[:, 0:128]
                Pc = T_[:, 256:384]
                kc_ = ("QMP", u, cur)
                if it == 0:
                    s.mm(psN[u][:, 0:128], Pc, Qc, reads=[kc_], writes=[kn])
                elif not last:
                    s.mm(psN[u][:, 0:256], Pc, T_[:, 0:256], reads=[kc_], writes=[kn])
                else:
                    s.mm(psN[u][:, 128:256], Pc, T_[:, 128:256], reads=[kc_], writes=[kn])
                if not last:
                    s.mm(psN[u][:, 256:384], Qc, Pc, reads=[kc_], writes=[kn])
                yield
                if not last:
                    s.copy("act", QMP[u][nxt][:].rearrange("p (a t) -> p a t", a=3)[:, 0:3:2, :],
                           psN[u][:, 0:384].rearrange("p (a t) -> p a t", a=3)[:, 0:3:2, :], reads=[kn], writes=[("QMP", u, nxt)])
                if it >= 1:
                    s.tt("dve", QMP[u][nxt][:, 128:256], psN[u][:, 128:256], T_[:, 128:256].bitcast(F32), ALU.add,
                         reads=[kn, kc_], writes=[("QMP", u, nxt)])
                yield
                cur = nxt
            M = QMP[u][cur][:, 128:256]
            Mkey = ("QMP", u, cur)
            psX = psN[u]
            s.tr(psX[:, 0:64], AR[:, h, 0, :].bitcast(F32), idf[0:64, 0:64], reads=["AR_at", "idf"], writes=[kn])
            s.tr(psX[:, 64:128], Bt[:, h, :], idf[0:64, 0:64], reads=["Bt", "idf"], writes=[kn])
            s.tr(psX[:, 128:192], Kt[:, h, :], idf[0:64, 0:64], reads=["Kt", "idf"], writes=[kn])
            s.mm(psX[:, 320:384], A2sb[u][:, 0:128], v_r[:, j, h * 64:(h + 1) * 64], reads=[("A2", u), ("v_r", j, gp)], writes=[kn])
            yield
            s.copy("act", tok3[u][:].rearrange("p a k -> p (a k)"), psX[:, 0:192], reads=[kn], writes=[("tok3", u)])
            s.copy("act", AVsb[u][:], psX[:, 320:384], reads=[kn], writes=[("AV", u)])
            yield
            s.mm(psX[0:64, 192:320], tok3[u][:, 0, :], M, reads=[("tok3", u), Mkey], writes=[kn])
            s.mm(psX[:, 384:448], M, AVsb[u][:], reads=[Mkey, ("AV", u)], writes=[kn])
            yield
            s.copy("act", G1sb[u][:], psX[0:64, 192:320], reads=[kn], writes=[("G1", u)])
            s.copy("act", P2sb[u][:], psX[:, 384:448], reads=[kn], writes=[("P2", u)])
            yield

        def unit_chain(Gi, j, h, u):
            gp = Gi % 2
            r_c, k_c, sgw, a_c, lhid, v_r, sg_t = r_c2[gp], k_c2[gp], sgw2[gp], a_c2[gp], lhid2[gp], v_r2[gp], sg_t2[gp]
            cidx = Gi * NCH + j
            so = cidx % 2
            sn = 1 - so
            vv = v_r[:, j, h * 64:(h + 1) * 64]
            psC = psN[u]
            Ups = psC[:, 448:512]
            Sps = psC[0:64, 0:64]
            Yps = psC[:, 64:128]
            Rps = psC[:, 128:130]
            key = ("psN", u)
            s.mm(Ups, G1sb[u][:], Ssb[h][so][:], reads=[("G1", u), ("S", h, so)], writes=[key])
            yield
            s.tt("dve", Usb[u][:], Ups, P2sb[u][:], ALU.add, reads=[key, ("P2", u)], writes=[("U", u)])
            yield
            s.mm(Sps, tok3[u][:, 2, :], vv, start=True, stop=False, reads=[("tok3", u), ("v_r", j, gp)], writes=[key])
            s.mm(Sps, tok3[u][:, 1, :], Usb[u][:], start=False, stop=True, reads=[("tok3", u), ("U", u)], writes=[key])
            s.mm(Yps, AR[:, h, 1, :], Ssb[h][so][:], start=True, stop=False, reads=["AR_rt", ("S", h, so)], writes=[key])
            s.mm(Yps, A1sb[u][:, 128:256], Usb[u][:], start=False, stop=False, reads=[("A1", u), ("U", u)], writes=[key])
            s.mm(Yps, A2sb[u][:, 128:256], vv, start=False, stop=True, reads=[("A2", u), ("v_r", j, gp)], writes=[key])
            s.mm(Rps, rkp[:, h, :], rk2[:, h, :], reads=["rkp", "rk2"], writes=[key])
            yield
            s.stt(Ssb[h][sn][:], Ssb[h][so][:].bitcast(F32), gLt[:, h, 0:1], Sps, ALU.mult, ALU.add,
                  reads=[("S", h, so), "gLt", key], writes=[("S", h, sn)])
            s.copy("act", ysb[:, h, :], Yps, reads=[key], writes=[("ysb", h)])
            s.copy("act", rks[:, h:h + 1], Rps[:, 0:1], reads=[key], writes=[("rks", h)])
            yield
            s.op("dve", lambda e, h=h: e.bn_stats(out=bst[:, h, :], in_=ysb[:, h, :]), reads=[("ysb", h)], writes=[("bst", h)])
            s.op("dve", lambda e, h=h: e.bn_aggr(out=mv[:, h, :], in_=bst[:, h, :]), reads=[("bst", h)], writes=[("mv", h)])
            yield

        def rr(gens):
            gens = list(gens)
            while gens:
                for g_ in list(gens):
                    try:
                        next(g_)
                    except StopIteration:
                        gens.remove(g_)

        def chunk_out(Gi, j):
            gp = Gi % 2
            r_c, k_c, sgw, a_c, lhid, v_r, sg_t = r_c2[gp], k_c2[gp], sgw2[gp], a_c2[gp], lhid2[gp], v_r2[gp], sg_t2[gp]
            t0 = Gi * GT + j * L
            oo = cnt["o"] % 2
            cnt["o"] += 1
            s.act(rs4[:], mv[:, :, 1], AF.Ln, bias=eps_t[:, 1:2], reads=allh("mv") + ["eps"], writes=["rs4"])
            yield
            s.act(rs4[:], rs4[:], AF.Exp, scale=-0.5, reads=["rs4"], writes=["rs4"])
            yield
            for h in range(4):
                s.ts("dve", yn[:, h * 64:(h + 1) * 64], ysb[:, h, :], mv[:, h, 0:1], rs4[:, h:h + 1], ALU.subtract, ALU.mult,
                     reads=[("ysb", h), ("mv", h), "rs4"], writes=["yn"])
                yield
            s.tt("dve", yn[:], yn[:], lnx_sb[:, 0, :], ALU.mult, reads=["yn", "lnx"], writes=["yn"])
            yield
            s.tt("dve", yn[:], yn[:], lnx_sb[:, 1, :], ALU.add, reads=["yn", "lnx"], writes=["yn"])
            yield
            for h in range(4):
                s.stt(ogo[oo][:, h * 64:(h + 1) * 64], v_r[:, j, h * 64:(h + 1) * 64].bitcast(F32), rks[:, h:h + 1], yn[:, h * 64:(h + 1) * 64],
                      ALU.mult, ALU.add, reads=[("v_r", j, gp), ("rks", h), "yn"], writes=["ogo"])
                yield
            s.tt("dve", ogo[oo][:], ogo[oo][:], sg_t[:, j, :], ALU.mult, reads=["ogo", ("sg_t", j, gp)], writes=["ogo"])
            yield
            for jj in range(4):
                s.ts("pool", ogo4[oo][:, jj, :], ogo[oo][:], sel_sb[:, jj:jj + 1], 1.0, ALU.mult, ALU.mult,
                     reads=["ogo", "sel"], writes=["ogo4"])
                yield
            s.dma("sp", og1buf[t0 // 1024, t0 % 1024:t0 % 1024 + 128, :].rearrange("t (j f) -> t j f", j=4), ogo4[oo][:], reads=["ogo4"], writes=[("og1buf", t0 // 1024)])
            yield
            if (t0 + 128) % 1024 == 0:
                ck_ = t0 // 1024
                s.cc(lambda e, ck_=ck_: e.collective_compute("AllReduce", ALU.add, replica_groups=GROUPS,
                                                             ins=[og1buf[ck_].opt()], outs=[og1all[ck_].opt()]),
                     reads=[("og1buf", ck_)])
                yield

        def chain_gens(gs):
            for g_ in gs:
                yield from g_

        def rr2(gens, bg):
            gens = list(gens)
            while gens:
                for g_ in list(gens):
                    try:
                        next(g_)
                    except StopIteration:
                        gens.remove(g_)
                if bg[0] is not None:
                    try:
                        next(bg[0])
                    except StopIteration:
                        bg[0] = None

        rr([stage_Pa(0)])
        rr([stage_Pb(0)])
        prev_out = []
        for Gi in range(n_groups):
            gp = Gi % 2
            bg = [chain_gens([stage_Pa(Gi + 1), stage_Pb(Gi + 1)])] if Gi + 1 < n_groups else [None]
            for j in range(NCH):
                rr([chunk_elem(j, gp)] + prev_out)
                prev_out = []
                gens = [unit_pre(j, h, h, gp) for h in range(4)]
                for pair in ((0, 1), (2, 3)):
                    for _ in range(2):
                        for u_ in pair:
                            next(gens[u_])
                rr2(gens, bg)
                rr2([unit_chain(Gi, j, h, h) for h in range(4)], bg)
                prev_out = [chunk_out(Gi, j)]
            if bg[0] is not None:
                rr([bg[0]])
        rr(prev_out)
        s.emit(nc, limit=limit, semstack=semstack, tag='B')


def consts_B():
    r = np.arange(128)[:, None]
    c = np.arange(128)[None, :]
    masks = np.stack([(r < c), (r <= c), (r > c)], axis=1).astype(np.float32)
    return dict(masks=np.ascontiguousarray(masks), identf=np.eye(128, dtype=np.float32), identb=np.eye(128).astype(bf))


def inputs_B(inp, b, g):
    cs = slice(g * 256, (g + 1) * 256)
    w_in = inp["rwkv_w_in"]
    w4 = np.concatenate([w_in[n][:, cs] for n in range(4)], axis=1)
    mixT = np.ascontiguousarray(inp["rwkv_mix"].reshape(6, 8, 128).transpose(2, 1, 0))
    hd = lambda p: np.asarray(p).reshape(-1)[cs].reshape(4, 64).T
    cpar = np.stack([hd(inp["rwkv_w0"]), hd(inp["rwkv_a0"]), hd(inp["rwkv_k_k"]), hd(inp["rwkv_k_a"]), hd(inp["rwkv_r_k"])], axis=1)
    lnx = np.stack([np.broadcast_to(inp["rwkv_lnx_g"][cs][None, :], (128, 256)),
                    np.broadcast_to(inp["rwkv_lnx_b"][cs][None, :], (128, 256))], axis=1)
    m = dict(wout0=np.ascontiguousarray(inp["moba_w_out"]),
             gbc=np.ascontiguousarray(np.broadcast_to(inp["rwkv_norm_g"][None, :], (128, 1024))),
             mixT=mixT, w4=np.ascontiguousarray(w4),
             wl=np.ascontiguousarray(np.concatenate([inp["rwkv_w1"], inp["rwkv_a1"]], axis=1)),
             w2a2=np.ascontiguousarray(np.stack([inp["rwkv_w2"][:, cs], inp["rwkv_a2"][:, cs]], axis=1)),
             cpar=np.ascontiguousarray(cpar.astype(np.float32)), lnx=np.ascontiguousarray(lnx.astype(np.float32)))
    m.update(consts_B())
    return m


D = 1024
NT = 64


def phase_C1(nc, semstack, ogall, og1all, ssbuf, x2dram):
    dram = lambda n, sh, dt, kind="ExternalInput": nc.dram_tensor("c_" + n, sh, dt, kind=kind).ap()
    xc = dram("xc", [S, 256], F32)
    w0c = dram("w0c", [D, 256], F32)
    w1c = dram("w1c", [D, 256], F32)
    identb = dram("identb", [128, 128], BF16)
    s = Sched()
    with ExitStack() as st:
        sb = lambda name, shape, dt: st.enter_context(nc.sbuf_tensor("C_" + name, shape, dt))
        xin = [sb("xin%d" % i, [128, 256], F32) for i in range(3)]
        o1in = [sb("o1in%d" % i, [128, D], F32) for i in range(3)]
        ogf = [sb("ogf%d" % i, [128, 8, 128], F32) for i in range(3)]
        ogin = [sb("ogin%d" % i, [128, 8, 128], BF16) for i in range(2)]
        o1b = sb("o1b", [128, D], BF16)
        o1T = [sb("o1T%d" % i, [128, 8, 128], BF16) for i in range(2)]
        x2t = [sb("x2t%d" % i, [128, 256], F32) for i in range(2)]
        junk = sb("junk", [128, 256], BF16)
        wo0 = sb("wo0", [128, 8, 256], BF16)
        wo1 = sb("wo1", [128, 8, 256], BF16)
        wst = sb("wst", [128, 256], F32)
        idb = sb("idb", [128, 128], BF16)
        ssq = sb("ssq", [128, NT], F32)
        psb = [st.enter_context(nc.psum_tensor("C_ps%d" % i, [128, 512], F32)) for i in range(4)]
        psP = psb[0:2]
        psT = psb[2]
        psT_bf = psT[:].bitcast(BF16)
        s.dma("sp", idb[:], identb[:], writes=["idb"])
        cstg = [(wst, "wst"), (xin[0], ("xin", 0)), (xin[1], ("xin", 1)), (xin[2], ("xin", 2))]
        for c in range(8):
            w_, k_ = cstg[c % 4]
            s.dma("sp", w_[:], w0c[c * 128:(c + 1) * 128, :], writes=[k_])
            s.copy("act", wo0[:, c, :], w_[:], reads=[k_], writes=["wo0"])
        for c in range(8):
            w_, k_ = cstg[c % 4]
            s.dma("sp", w_[:], w1c[c * 128:(c + 1) * 128, :], writes=[k_])
            s.copy("dve", wo1[:, c, :], w_[:], reads=[k_], writes=["wo1"])
        def c_stage1(ti):
            xs = ti % 3
            tok = ti * 128
            ck, c0 = tok // 1024, tok % 1024
            s.dma("sp", xin[xs][:], xc[tok:tok + 128, :], writes=[("xin", xs)])
            s.dma("sp", o1in[xs][:], og1all[ck, c0:c0 + 128, :], writes=[("o1in", xs)])
            s.dma("pool", ogf[xs][:], ogall[ck, :, c0:c0 + 128].rearrange("(c p) t -> p c t", p=128), writes=[("ogf", xs)])

        def c_stage2(ti):
            xs = ti % 3
            b2 = ti % 2
            s.copy("dve", ogin[b2][:], ogf[xs][:], reads=[("ogf", xs)], writes=[("ogin", b2)])
            s.copy("act", o1b[:], o1in[xs][:], reads=[("o1in", xs)], writes=["o1b"])
            for c in range(8):
                s.tr(psT_bf[:, c * 128:(c + 1) * 128], o1b[:, c * 128:(c + 1) * 128], idb[:], reads=["o1b", "idb"], writes=["psT"])
            s.copy("act", o1T[b2][:], psT_bf.rearrange("p (c t) -> p c t", c=8), reads=["psT"], writes=[("o1T", b2)])

        def c_stage3(ti):
            xs = ti % 3
            b2 = ti % 2
            tok = ti * 128
            ps = ti % 2
            for c in range(8):
                s.mm(psP[ps][:, 0:256], ogin[b2][:, c, :], wo0[:, c, :], start=(c == 0), stop=False,
                     reads=[("ogin", b2), "wo0"], writes=[("psP", ps)])
            for c in range(8):
                s.mm(psP[ps][:, 0:256], o1T[b2][:, c, :], wo1[:, c, :], start=False, stop=(c == 7),
                     reads=[("o1T", b2), "wo1"], writes=[("psP", ps)])
            s.tt("dve", x2t[b2][:], psP[ps][:, 0:256], xin[xs][:], ALU.add,
                 reads=[("psP", ps), ("xin", xs)], writes=[("x2t", b2)])
            s.op("dve", lambda e, b2=b2, ti=ti: e.scalar_tensor_tensor(
                out=junk[:], in0=x2t[b2][:], scalar=1.0, in1=x2t[b2][:], op0=ALU.mult, op1=ALU.mult, accum_out=ssq[:, ti:ti + 1]),
                reads=[("x2t", b2)], writes=["junk", "ssq"])
            s.dma("sp", x2dram[tok:tok + 128, :], x2t[b2][:], reads=[("x2t", b2)])

        for ti in range(NT + 2):
            if ti < NT:
                c_stage1(ti)
            if 1 <= ti <= NT:
                c_stage2(ti - 1)
            if ti >= 2:
                c_stage3(ti - 2)
        s.dma("sp", ssbuf[:, :], ssq[:], reads=["ssq"])
        s.emit(nc, semstack=semstack, tag='C')


def phase_C2(nc, semstack, ssall, x2dram):
    dram = lambda n, sh, dt, kind="ExternalInput": nc.dram_tensor("c_" + n, sh, dt, kind=kind).ap()
    gbc = dram("gbc", [128, 256], F32)
    out = dram("out", [S, 256], F32, kind="ExternalOutput")
    s = Sched()
    with ExitStack() as st:
        sb = lambda name, shape, dt: st.enter_context(nc.sbuf_tensor("E_" + name, shape, dt))
        x2t = [sb("x2t%d" % i, [128, 256], F32) for i in range(3)]
        res = [sb("res%d" % i, [128, 256], F32) for i in range(3)]
        gbc_sb = sb("gbc_sb", [128, 256], F32)
        ssq = sb("ssq", [128, NT], F32)
        rstd = sb("rstd", [128, NT], F32)
        eps_t = sb("eps_t", [128, 1], F32)
        s.dma("sp", gbc_sb[:], gbc[:], writes=["gbc"])
        s.dma("sp", ssq[:], ssall[:, :], writes=["ssq"])
        s.memset("pool", eps_t[:], 1e-6, writes=["eps"])
        s.act(rstd[:], ssq[:], AF.Ln, scale=1.0 / D, bias=eps_t[:, 0:1], reads=["ssq", "eps"], writes=["rstd"])
        s.act(rstd[:], rstd[:], AF.Exp, scale=-0.5, reads=["rstd"], writes=["rstd"])
        for ti in range(NT):
            k = ti % 3
            tok = ti * 128
            s.dma("sp", x2t[k][:], x2dram[tok:tok + 128, :], writes=[("x2t", k)])
            s.stt(res[k][:], x2t[k][:], rstd[:, ti:ti + 1], gbc_sb[:], ALU.mult, ALU.mult,
                  reads=[("x2t", k), "rstd", "gbc"], writes=[("res", k)])
            s.dma("pool", out[tok:tok + 128, :], res[k][:], reads=[("res", k)])
        s.emit(nc, semstack=semstack, tag='E')


GROUPS = [[0, 1, 2, 3], [4, 5, 6, 7]]


def allreduce_block(nc, semstack, pairs, tag):
    sem = semstack.enter_context(nc.semaphore(tag + "_cc"))
    with nc.Block() as block:
        @block.gpsimd
        def _(g):
            for i, (src, dst) in enumerate(pairs):
                g.collective_compute("AllReduce", ALU.add, replica_groups=GROUPS,
                                     ins=[src.opt()], outs=[dst.opt()]).then_inc(sem)
                g.wait_ge(sem, i + 1)


def build_fused():
    nc = bass.Bass("TRN2", target_bir_lowering=False)
    x = nc.dram_tensor("x", [S, D], F32, kind="ExternalInput").ap()
    ogbuf = nc.dram_tensor("ogbuf", [8, 1024, 1024], F32).ap()
    ogall = nc.dram_tensor("ogall", [8, 1024, 1024], F32).ap()
    og1buf = nc.dram_tensor("og1buf", [8, 1024, 1024], F32).ap()
    og1all = nc.dram_tensor("og1all", [8, 1024, 1024], F32).ap()
    ssbuf = nc.dram_tensor("ssbuf", [128, 64], F32).ap()
    ssall = nc.dram_tensor("ssall", [128, 64], F32).ap()
    x2dram = nc.dram_tensor("x2dram", [S, 256], F32).ap()
    with ExitStack() as semstack:
        phase_A(nc, semstack, x, ogbuf, ogall)
        phase_B(nc, semstack, x, ogall, og1buf, og1all)
        phase_C1(nc, semstack, ogall, og1all, ssbuf, x2dram)
        allreduce_block(nc, semstack, [(ssbuf, ssall)], "x3")
        phase_C2(nc, semstack, ssall, x2dram)
    return nc


def kernel(**inp):
    inp = {k: np.asarray(v) for k, v in inp.items()}
    x = inp["x"]
    w_in = inp["moba_w_in"]
    nc = build_fused()
    gbcA = np.ascontiguousarray(np.broadcast_to(inp["moba_norm_g"][None, :], (128, 1024)))
    identb = np.eye(128).astype(bf)
    maps = []
    for c in range(8):
        b, g = c // 4, c % 4
        cs = slice(g * 256, (g + 1) * 256)
        m = dict(x=np.ascontiguousarray(x[b]))
        w4 = np.concatenate([w_in[:, k * 1024 + g * 256: k * 1024 + (g + 1) * 256] for k in range(4)], axis=1)
        sel = np.zeros((128, 4), np.float32)
        sel[:, g] = 1.0
        am = dict(gbc=gbcA, w4=np.ascontiguousarray(w4), sel=sel)
        am.update(consts_A(g))
        for k, v in am.items():
            m["a_" + k] = v
        bm = inputs_B(inp, b, g)
        bm["sel"] = sel
        for k, v in bm.items():
            m["b_" + k] = v
        m["c_xc"] = np.ascontiguousarray(x[b][:, cs])
        m["c_w0c"] = np.ascontiguousarray(inp["moba_w_out"][:, cs])
        m["c_w1c"] = np.ascontiguousarray(inp["rwkv_w_out"][:, cs])
        m["c_identb"] = identb
        m["c_gbc"] = np.ascontiguousarray(np.broadcast_to(inp["final_norm_g"][cs][None, :], (128, 256)))
        maps.append(m)
    res = run_bass_kernel_spmd(nc, maps, core_ids=list(range(8))).results
    out = np.zeros((2, 8192, 1024), np.float32)
    for c in range(8):
        b, g = c // 4, c % 4
        out[b, :, g * 256:(g + 1) * 256] = np.asarray(res[c]["c_out"])
    return out
```
